# Optimizing a Trainium2 kernel written in Bass

```python
import functools
import jax, jax.numpy as jnp
from jax import lax
import numpy as np

D_MODEL = 2048
BATCH = 8
SEQ = 2048
DEPTH = 1
DEC_BATCH = 128
DEC_SEQ = 8
PAST_LEN = 16384
PAGE_SIZE = 128

F32 = jnp.float32
N_META = 16
NORM_EPS = 1e-6
NEG = -1e30
HEAD_A = 64
D_A = D_MODEL // 2
H_A = D_A // HEAD_A
DECAY_LORA = 96
AAA_LORA = 96
GATE_LORA = 256
RWKV_COLS = 3 * D_A + DECAY_LORA + AAA_LORA + GATE_LORA
LNX_EPS = 64e-5
QK_NOPE = 64
QK_ROPE = 32
V_HEAD = 64
H_B = (D_MODEL // 2) // V_HEAD
Q_LORA = 512
KV_LORA = 256
ROPE_THETA = 10000.0
Q_BLOCK = 128
OFF_Q = 0
OFF_KV = OFF_Q + Q_LORA
OFF_KR = OFF_KV + KV_LORA
OFF_RWKV = OFF_KR + QK_ROPE
OFF_GA = OFF_RWKV + RWKV_COLS
OFF_GB = OFF_GA + D_MODEL
IN_COLS = OFF_GB + D_MODEL
N_GROUPS = 4
EXPERTS_PER_GROUP = 8
N_EXPERTS = N_GROUPS * EXPERTS_PER_GROUP
TOP_K = 2
EXPERT_FF = 512
MOE_BLOCK = 1024

kernel_name = 'rwkv7_mla_gated_hier_moe_step'


def rmsnorm(x, g):
    xf = x.astype(F32)
    y = xf * lax.rsqrt(jnp.mean(xf * xf, -1, keepdims=True) + NORM_EPS)
    return (y * g.astype(F32)).astype(x.dtype)


def rope(x, pos):
    half = x.shape[-1] // 2
    inv = ROPE_THETA ** (-jnp.arange(half, dtype=F32) / half)
    ang = pos.astype(F32)[:, None] * inv[None, :]
    ang = ang.reshape(ang.shape[:1] + (1,) * (x.ndim - 3) + ang.shape[1:])
    cos, sin = jnp.cos(ang), jnp.sin(ang)
    xf = x.astype(F32)
    x1, x2 = xf[..., :half], xf[..., half:]
    return jnp.concatenate([x1 * cos - x2 * sin, x1 * sin + x2 * cos], -1).astype(x.dtype)


def rwkv_prepare(p_rw, prev_row, lp):
    b, t = p_rw.shape[:2]
    prev = jnp.concatenate([prev_row[:, None].astype(p_rw.dtype), p_rw[:, :-1]], axis=1)
    xs = p_rw + (prev - p_rw) * lp['rwkv_mu']
    cuts = np.cumsum([D_A, D_A, D_A, DECAY_LORA, AAA_LORA]).tolist()
    r, k, v, wl, al, gl = jnp.split(xs, cuts, axis=-1)
    w_log = -jax.nn.softplus(-(lp['rwkv_w0'] + jnp.tanh(wl) @ lp['rwkv_w2']).astype(F32)) - 0.5
    decay = jnp.exp(-jnp.exp(w_log))
    a = jax.nn.sigmoid((lp['rwkv_a0'] + al @ lp['rwkv_a2']).astype(F32))
    g = jax.nn.sigmoid(gl) @ lp['rwkv_g2']
    heads = lambda z: z.reshape(b, t, H_A, HEAD_A)
    kk = heads((k * lp['rwkv_k_k']).astype(F32))
    kk = kk * lax.rsqrt(jnp.maximum(jnp.sum(kk * kk, -1, keepdims=True), 1e-24))
    k_mod = k.astype(F32) * (1.0 + (a - 1.0) * lp['rwkv_k_a'].astype(F32))
    return heads(r.astype(F32)), heads(decay), heads(k_mod), heads(v.astype(F32)), kk, heads(a), g


def rwkv_scan(r, decay, k, v, kk, a, s0):
    def step(state, inp):
        r_t, w_t, k_t, v_t, kk_t, a_t = inp
        sa = jnp.einsum('bhvk,bhk->bhv', state, -kk_t)
        state = (state * w_t[:, :, None, :] + sa[..., None] * (kk_t * a_t)[:, :, None, :]
                 + v_t[..., None] * k_t[:, :, None, :])
        return state, jnp.einsum('bhvk,bhk->bhv', state, r_t)
    xs = tuple(jnp.moveaxis(z, 1, 0) for z in (r, decay, k, v, kk, a))
    s_last, y = lax.scan(step, s0, xs)
    return jnp.moveaxis(y, 0, 1), s_last


def rwkv_output(y, r, k, v, g, lp):
    b, t = y.shape[:2]
    mu = jnp.mean(y, -1, keepdims=True)
    var = jnp.mean(jnp.square(y - mu), -1, keepdims=True)
    yn = ((y - mu) * lax.rsqrt(var + LNX_EPS)).reshape(b, t, D_A)
    yn = yn * lp['rwkv_ln_g'].astype(F32) + lp['rwkv_ln_b'].astype(F32)
    bonus = (jnp.sum(r * k * lp['rwkv_r_k'].astype(F32), -1, keepdims=True) * v).reshape(b, t, D_A)
    return ((yn + bonus) * g.astype(F32)).astype(g.dtype)


def mla_project(p, pos, lp):
    b, t = p.shape[:2]
    c_q = rmsnorm(p[..., OFF_Q:OFF_KV], lp['mla_q_norm_g'])
    q = (c_q @ lp['mla_w_uq']).reshape(b, t, H_B, QK_NOPE + QK_ROPE)
    q_nope, q_rope = q[..., :QK_NOPE], rope(q[..., QK_NOPE:], pos)
    ckv = rmsnorm(p[..., OFF_KV:OFF_KR], lp['mla_kv_norm_g'])
    krope = rope(p[..., OFF_KR:OFF_RWKV], pos)
    return q_nope, q_rope, ckv, krope


def mla_attend_prompt(q_nope, q_rope, ckv, krope, lp):
    b, t = ckv.shape[:2]
    k_nope = jnp.einsum('btc,chn->bthn', ckv, lp['mla_w_uk'])
    v = jnp.einsum('btc,chv->bthv', ckv, lp['mla_w_uv'])
    k = jnp.concatenate([k_nope, jnp.broadcast_to(krope[:, :, None], (b, t, H_B, QK_ROPE))], -1)
    q = jnp.concatenate([q_nope, q_rope], -1)
    kpos = jnp.arange(t)
    scale = (QK_NOPE + QK_ROPE) ** -0.5

    def block(args):
        qb, qpos = args
        sc = jnp.einsum('bqhd,bkhd->bhqk', qb, k).astype(F32) * scale
        sc = jnp.where(kpos[None, :] <= qpos[:, None], sc, NEG)
        pr = jax.nn.softmax(sc, axis=-1).astype(v.dtype)
        return jnp.einsum('bhqk,bkhv->bqhv', pr, v)

    o_meta = block((q[:, :N_META], jnp.arange(N_META)))
    n_real = t - N_META
    n_blk = n_real // Q_BLOCK
    qb = q[:, N_META:].reshape(b, n_blk, Q_BLOCK, H_B, QK_NOPE + QK_ROPE).swapaxes(0, 1)
    pb = (N_META + jnp.arange(n_real)).reshape(n_blk, Q_BLOCK)
    o_real = lax.map(block, (qb, pb)).swapaxes(0, 1).reshape(b, n_real, H_B, V_HEAD)
    return jnp.concatenate([o_meta, o_real], axis=1).reshape(b, t, H_B * V_HEAD)


def mla_attend_sample(q_nope, q_rope, ckv, krope, cache_ckv, cache_krope, layer, page_table, lp):
    db, ds = ckv.shape[:2]
    scale = (QK_NOPE + QK_ROPE) ** -0.5
    q_lat = jnp.einsum('bshn,chn->bshc', q_nope, lp['mla_w_uk']).astype(F32)
    q_r = q_rope.astype(F32)

    def scores(kc, kr):
        return (jnp.einsum('bshc,bkc->bhsk', q_lat, kc) + jnp.einsum('bshr,bkr->bhsk', q_r, kr)) * scale

    def update(carry, sc, kc):
        m, l, acc = carry
        m_new = jnp.maximum(m, sc.max(-1))
        corr = jnp.exp(m - m_new)
        pr = jnp.exp(sc - m_new[..., None])
        return (m_new, l * corr + pr.sum(-1),
                acc * corr[..., None] + jnp.einsum('bhsk,bkc->bhsc', pr, kc))

    def page_step(carry, phys):
        kc = cache_ckv[layer, phys].astype(F32)
        kr = cache_krope[layer, phys].astype(F32)
        return update(carry, scores(kc, kr), kc), None

    init = (jnp.full((db, H_B, ds), NEG, F32), jnp.zeros((db, H_B, ds), F32),
            jnp.zeros((db, H_B, ds, KV_LORA), F32))
    carry, _ = lax.scan(page_step, init, page_table.T)
    kc_new, kr_new = ckv.astype(F32), krope.astype(F32)
    causal = jnp.arange(ds)[None, :] <= jnp.arange(ds)[:, None]
    sc_new = jnp.where(causal, scores(kc_new, kr_new), NEG)
    _, l, acc = update(carry, sc_new, kc_new)
    o_lat = (acc / l[..., None]).astype(ckv.dtype)
    o = jnp.einsum('bhsc,chv->bshv', o_lat, lp['mla_w_uv'])
    return o.reshape(db, ds, H_B * V_HEAD)


def token_mixer(h, pos, shift_prev, wkv_prev, attend, lp):
    p = h @ lp['w_in']
    p_rw = p[..., OFF_RWKV:OFF_GA]
    r, decay, k, v, kk, a, g = rwkv_prepare(p_rw, shift_prev, lp)
    y_state, wkv_new = rwkv_scan(r, decay, k, v, kk, a, wkv_prev.astype(F32))
    o_a = rwkv_output(y_state, r, k, v, g, lp)
    q_nope, q_rope, ckv, krope = mla_project(p, pos, lp)
    o_b = attend(q_nope, q_rope, ckv, krope)
    gate_a = jax.nn.sigmoid(p[..., OFF_GA:OFF_GB])
    gate_b = jax.nn.sigmoid(p[..., OFF_GB:])
    merged = gate_a * (o_a @ lp['w_up_a']) + gate_b * (o_b @ lp['w_up_b'])
    return merged @ lp['w_o'], (ckv, krope, wkv_new, p_rw[:, -1])


def moe_ffn(h, lp):
    shp = h.shape
    x = h.reshape(-1, D_MODEL)
    n = x.shape[0]
    xf = x.astype(F32)
    g_logit = xf @ lp['router_group_w'].astype(F32)
    g_prob = jax.nn.softmax(g_logit, axis=-1)
    g_sel = jnp.argmax(g_logit + lp['router_group_b'].astype(F32), axis=-1)
    e_logit = (xf @ lp['router_expert_w'].astype(F32)).reshape(n, N_GROUPS, EXPERTS_PER_GROUP)
    e_in = jnp.take_along_axis(e_logit, g_sel[:, None, None], axis=1)[:, 0]
    e_bias = lp['router_expert_b'].astype(F32).reshape(N_GROUPS, EXPERTS_PER_GROUP)[g_sel]
    _, idx = lax.top_k(e_in + e_bias, TOP_K)
    gate = (jax.nn.softmax(jnp.take_along_axis(e_in, idx, axis=1), axis=-1)
            * jnp.take_along_axis(g_prob, g_sel[:, None], axis=1))
    eid = g_sel[:, None] * EXPERTS_PER_GROUP + idx
    comb = jnp.sum(jax.nn.one_hot(eid, N_EXPERTS, dtype=F32) * gate[..., None], axis=1)
    n_blk = -(-n // MOE_BLOCK)
    pad = n_blk * MOE_BLOCK - n
    xb = jnp.pad(x, ((0, pad), (0, 0))).reshape(n_blk, MOE_BLOCK, D_MODEL)
    cb = jnp.pad(comb, ((0, pad), (0, 0))).reshape(n_blk, MOE_BLOCK, N_EXPERTS).astype(x.dtype)

    def block(args):
        xt, ct = args
        hg = jnp.einsum('td,edf->tef', xt, lp['expert_w_gate'])
        hu = jnp.einsum('td,edf->tef', xt, lp['expert_w_up'])
        return jnp.einsum('tef,efd->td', jax.nn.silu(hg) * hu * ct[..., None], lp['expert_w_down'])

    y = lax.map(block, (xb, cb)).reshape(n_blk * MOE_BLOCK, D_MODEL)[:n]
    return y.reshape(shp)


def stack_state(states, i):
    return jnp.stack([st[i] for st in states], axis=0)


def setup_inputs(seed: int = 0) -> dict:
    key = jax.random.key(seed)
    ks = iter(jax.random.split(key, 48))
    nrm = lambda shape, scale: jax.random.normal(next(ks), shape, F32) * scale
    uni = lambda shape, lo, hi: jax.random.uniform(next(ks), shape, F32, lo, hi)
    n_pages = PAST_LEN // PAGE_SIZE
    n_used = DEC_BATCH * n_pages
    n_pool = n_used + max(1, n_used // 4)
    page_table = jax.random.permutation(next(ks), n_pool)[:n_used].reshape(DEC_BATCH, n_pages).astype(jnp.int32)
    L, D = DEPTH, D_MODEL
    return {
        'x_prompt': nrm((BATCH, SEQ, D), 1.0),
        'x_sample': nrm((DEC_BATCH, DEC_SEQ, D), 1.0),
        'cache_ckv': nrm((L, n_pool, PAGE_SIZE, KV_LORA), 1.0),
        'cache_krope': nrm((L, n_pool, PAGE_SIZE, QK_ROPE), 1.0),
        'state_wkv': nrm((L, DEC_BATCH, H_A, HEAD_A, HEAD_A), 0.3),
        'state_shift': nrm((L, DEC_BATCH, RWKV_COLS), 1.0),
        'page_table': page_table,
        'meta_tokens': nrm((N_META, D), 1.0),
        'norm_mix_g': 1.0 + nrm((L, D), 0.02),
        'w_in': nrm((L, D, IN_COLS), D ** -0.5),
        'rwkv_mu': uni((L, RWKV_COLS), 0.0, 1.0),
        'rwkv_w0': uni((L, D_A), -6.0, -1.0),
        'rwkv_w2': nrm((L, DECAY_LORA, D_A), 0.1 * DECAY_LORA ** -0.5),
        'rwkv_a0': nrm((L, D_A), 0.1),
        'rwkv_a2': nrm((L, AAA_LORA, D_A), AAA_LORA ** -0.5),
        'rwkv_g2': nrm((L, GATE_LORA, D_A), GATE_LORA ** -0.5),
        'rwkv_k_k': 0.85 + nrm((L, D_A), 0.02),
        'rwkv_k_a': 1.0 + nrm((L, D_A), 0.02),
        'rwkv_r_k': nrm((L, H_A, HEAD_A), 0.1),
        'rwkv_ln_g': 1.0 + nrm((L, D_A), 0.02),
        'rwkv_ln_b': nrm((L, D_A), 0.02),
        'mla_q_norm_g': 1.0 + nrm((L, Q_LORA), 0.02),
        'mla_w_uq': nrm((L, Q_LORA, H_B * (QK_NOPE + QK_ROPE)), Q_LORA ** -0.5),
        'mla_kv_norm_g': 1.0 + nrm((L, KV_LORA), 0.02),
        'mla_w_uk': nrm((L, KV_LORA, H_B, QK_NOPE), KV_LORA ** -0.5),
        'mla_w_uv': nrm((L, KV_LORA, H_B, V_HEAD), KV_LORA ** -0.5),
        'w_up_a': nrm((L, D_A, D), D_A ** -0.5),
        'w_up_b': nrm((L, H_B * V_HEAD, D), (H_B * V_HEAD) ** -0.5),
        'w_o': nrm((L, D, D), D ** -0.5),
        'norm_ffn_g': 1.0 + nrm((L, D), 0.02),
        'router_group_w': nrm((L, D, N_GROUPS), D ** -0.5),
        'router_group_b': nrm((L, N_GROUPS), 0.01),
        'router_expert_w': nrm((L, D, N_EXPERTS), D ** -0.5),
        'router_expert_b': nrm((L, N_EXPERTS), 0.01),
        'expert_w_gate': nrm((L, N_EXPERTS, D, EXPERT_FF), D ** -0.5),
        'expert_w_up': nrm((L, N_EXPERTS, D, EXPERT_FF), D ** -0.5),
        'expert_w_down': nrm((L, N_EXPERTS, EXPERT_FF, D), EXPERT_FF ** -0.5),
        'norm_final_g': 1.0 + nrm((D,), 0.02),
    }


def reference(x_prompt, x_sample, cache_ckv, cache_krope, state_wkv, state_shift, page_table,
              meta_tokens, norm_mix_g, w_in, rwkv_mu, rwkv_w0, rwkv_w2, rwkv_a0, rwkv_a2,
              rwkv_g2, rwkv_k_k, rwkv_k_a, rwkv_r_k, rwkv_ln_g, rwkv_ln_b, mla_q_norm_g,
              mla_w_uq, mla_kv_norm_g, mla_w_uk, mla_w_uv, w_up_a, w_up_b, w_o, norm_ffn_g,
              router_group_w, router_group_b, router_expert_w, router_expert_b,
              expert_w_gate, expert_w_up, expert_w_down, norm_final_g):
    b, s = x_prompt.shape[:2]
    ds = x_sample.shape[1]
    past = page_table.shape[1] * PAGE_SIZE
    meta = jnp.broadcast_to(meta_tokens.astype(x_prompt.dtype)[None], (b, N_META, D_MODEL))
    xp = jnp.concatenate([meta, x_prompt], axis=1)
    xs = x_sample
    pos_p = jnp.arange(N_META + s)
    pos_s = past + jnp.arange(ds)
    new_p, new_s = [], []
    for l in range(DEPTH):
        lp = dict(w_in=w_in[l], rwkv_mu=rwkv_mu[l], rwkv_w0=rwkv_w0[l], rwkv_w2=rwkv_w2[l],
                  rwkv_a0=rwkv_a0[l], rwkv_a2=rwkv_a2[l], rwkv_g2=rwkv_g2[l],
                  rwkv_k_k=rwkv_k_k[l], rwkv_k_a=rwkv_k_a[l], rwkv_r_k=rwkv_r_k[l],
                  rwkv_ln_g=rwkv_ln_g[l], rwkv_ln_b=rwkv_ln_b[l],
                  mla_q_norm_g=mla_q_norm_g[l], mla_w_uq=mla_w_uq[l],
                  mla_kv_norm_g=mla_kv_norm_g[l], mla_w_uk=mla_w_uk[l], mla_w_uv=mla_w_uv[l],
                  w_up_a=w_up_a[l], w_up_b=w_up_b[l], w_o=w_o[l],
                  router_group_w=router_group_w[l], router_group_b=router_group_b[l],
                  router_expert_w=router_expert_w[l], router_expert_b=router_expert_b[l],
                  expert_w_gate=expert_w_gate[l], expert_w_up=expert_w_up[l],
                  expert_w_down=expert_w_down[l])
        mix_p, st_p = token_mixer(rmsnorm(xp, norm_mix_g[l]), pos_p,
                                  jnp.zeros((b, RWKV_COLS), xp.dtype),
                                  jnp.zeros((b, H_A, HEAD_A, HEAD_A), F32),
                                  functools.partial(mla_attend_prompt, lp=lp), lp)
        xp = xp + mix_p
        xp = xp + moe_ffn(rmsnorm(xp, norm_ffn_g[l]), lp)
        mix_s, st_s = token_mixer(rmsnorm(xs, norm_mix_g[l]), pos_s, state_shift[l], state_wkv[l],
                                  functools.partial(mla_attend_sample, cache_ckv=cache_ckv,
                                                    cache_krope=cache_krope, layer=l,
                                                    page_table=page_table, lp=lp), lp)
        xs = xs + mix_s
        xs = xs + moe_ffn(rmsnorm(xs, norm_ffn_g[l]), lp)
        new_p.append(st_p)
        new_s.append(st_s)
    y_prompt = rmsnorm(xp[:, N_META:], norm_final_g)
    y_sample = rmsnorm(xs, norm_final_g)
    ckv_p, kr_p, wkv_p, sh_p = (stack_state(new_p, 0), stack_state(new_p, 1),
                                stack_state(new_p, 2), stack_state(new_p, 3))
    ckv_s, kr_s, wkv_s, sh_s = (stack_state(new_s, 0), stack_state(new_s, 1),
                                stack_state(new_s, 2), stack_state(new_s, 3))
    return (y_prompt, y_sample, ckv_p, kr_p, wkv_p, sh_p, ckv_s, kr_s, wkv_s, sh_s)
```

```python
import contextlib
import numpy as np
import ml_dtypes
import concourse.bass as bass
import concourse.mybir as mybir
from concourse.bass_utils import run_bass_kernel_spmd

F32 = mybir.dt.float32
BF16 = mybir.dt.bfloat16
I32 = mybir.dt.int32
AF = mybir.ActivationFunctionType
ALU = mybir.AluOpType
AX = mybir.AxisListType

NT = 2192
NPR = 2064
NS = 128
D = 2048
TT = [(0, 16)] + [(16 + 128 * i, 128) for i in range(16)] + [(2064, 128)]
GROUPS = [(0, 512), (512, 512), (1024, 512), (1536, 512), (2048, 144)]
RWKV_COLS = 3520
OFF_RWKV = 800
IN_COLS = 8416
SCALE = 96 ** -0.5


class Buf:
    __slots__ = ("writer", "readers")

    def __init__(self):
        self.writer = None
        self.readers = {}


class T:
    __slots__ = ("t", "b")

    def __init__(self, t, b=None):
        self.t = t
        self.b = b if b is not None else Buf()

    def __getitem__(self, k):
        return self.t[k]


def _bufs(xs):
    out = []
    for x in xs:
        if x is None:
            continue
        out.append(x.b if isinstance(x, T) else x)
    return out


class _Rec:
    def __getattr__(self, name):
        return lambda *a, **k: (name, a, k)


_REC = _Rec()


def _replay(E, call):
    kw = call[2]
    if call[0] == "dma_start" and "allow_slow_non_contiguous" not in kw:
        kw = dict(kw, allow_slow_non_contiguous=True)
    return getattr(E, call[0])(*call[1], **kw)


class Sched:
    ENGS = ("sync", "scalar", "vector", "gpsimd", "tensor")
    EPOCH = 30000

    def __init__(self, nc):
        self.nc = nc
        self.streams = {e: [] for e in self.ENGS}
        self.sems = {}
        self.cnt = {}
        self.cur = {}
        self.waited = {e: {} for e in self.ENGS}
        self.ndma = {e: 0 for e in self.ENGS}
        self.dma_pool = {e: [] for e in self.ENGS}
        self.dma_last = {}
        self.n_dma_sems = 8
        self.nsem = 0
        self.ninst = 0
        for e in self.ENGS:
            self._new_epoch(e)

    def _alloc(self, key):
        h = self.nc.alloc_semaphore(name=f"s{self.nsem}")
        self.nsem += 1
        self.sems[key] = h
        self.cnt[key] = 0
        return key

    def _new_epoch(self, e):
        self.cur[e] = self._alloc(f"{e}_ep{self.nsem}")

    def _wait(self, eng, dep):
        key, val = dep
        if self.waited[eng].get(key, 0) >= val:
            return
        self.waited[eng][key] = val
        h = self.sems[key]
        self.streams[eng].append(lambda E, h=h, val=val: E.wait_ge(h, val))

    def _deps(self, eng, reads, writes):
        for b in reads:
            if b.writer is not None:
                self._wait(eng, b.writer)
        for b in writes:
            if b.writer is not None:
                self._wait(eng, b.writer)
            for d in b.readers.values():
                self._wait(eng, d)

    def _mark(self, dep, reads, writes):
        for b in reads:
            b.readers[dep[0]] = dep
        for b in writes:
            b.writer = dep
            b.readers = {}

    def op(self, eng, fn, reads=(), writes=()):
        reads = _bufs(reads)
        writes = _bufs(writes)
        self._deps(eng, reads, writes)
        key = self.cur[eng]
        if self.cnt[key] >= self.EPOCH:
            self._new_epoch(eng)
            key = self.cur[eng]
        self.cnt[key] += 1
        val = self.cnt[key]
        h = self.sems[key]
        call = fn(_REC)
        self.streams[eng].append(lambda E, call=call, h=h: _replay(E, call).then_inc(h, 1))
        dep = (key, val)
        if eng == "tensor":
            self.waited[eng][key] = val
        self._mark(dep, reads, writes)
        self.ninst += 1
        return dep

    def dma(self, eng, fn, reads=(), writes=()):
        reads = _bufs(reads)
        writes = _bufs(writes)
        self._deps(eng, reads, writes)
        pool = self.dma_pool[eng]
        i = self.ndma[eng]
        self.ndma[eng] += 1
        if len(pool) < self.n_dma_sems:
            pool.append(self._alloc(f"{eng}_dma{len(pool)}"))
        key = pool[i % self.n_dma_sems]
        last = self.dma_last.get(key)
        if last is not None:
            self._wait(eng, last)
        self.cnt[key] += 16
        val = self.cnt[key]
        h = self.sems[key]
        call = fn(_REC)
        self.streams[eng].append(lambda E, call=call, h=h: _replay(E, call).then_inc(h, 16))
        dep = (key, val)
        self.dma_last[key] = dep
        self._mark(dep, reads, writes)
        self.ninst += 1
        return dep

    def barrier(self):
        deps = []
        for e in self.ENGS:
            key = self.cur[e]
            if self.cnt[key] > 0:
                deps.append((key, self.cnt[key]))
        for key, dep in self.dma_last.items():
            deps.append(dep)
        for e in self.ENGS:
            for d in deps:
                self._wait(e, d)

    def finish(self):
        self.barrier()
        nc = self.nc
        st = self.streams
        with nc.Block() as block:
            @block.sync
            def _(E):
                for t in st["sync"]:
                    t(E)

            @block.scalar
            def _(E):
                for t in st["scalar"]:
                    t(E)

            @block.vector
            def _(E):
                for t in st["vector"]:
                    t(E)

            @block.gpsimd
            def _(E):
                for t in st["gpsimd"]:
                    t(E)

            @block.tensor
            def _(E):
                for t in st["tensor"]:
                    t(E)


def make_consts():
    c = {}
    c["ident_f"] = np.eye(128, dtype=np.float32)
    c["ident_b"] = np.eye(128, dtype=np.float32).astype(ml_dtypes.bfloat16)
    half = 16
    inv = (np.float32(10000.0) ** (-np.arange(half, dtype=np.float32) / np.float32(half))).astype(np.float32)
    pos = np.concatenate([np.arange(NPR), 16384 + np.tile(np.arange(8), 16)]).astype(np.float32)
    ang = (pos[:, None] * inv[None, :]).astype(np.float32)
    cos = np.cos(ang).astype(np.float32).T
    sin = np.sin(ang).astype(np.float32).T
    rc = np.zeros((128, NT), np.float32)
    rs = np.zeros((128, NT), np.float32)
    for p in range(128):
        j = p % 16
        rc[p] = cos[j]
        rs[p] = -sin[j] if (p % 32) < 16 else sin[j]
    c["rope_c"] = rc
    c["rope_s"] = rs
    i = np.arange(128)
    le = (i[:, None] <= i[None, :]).astype(np.float32)
    lt = (i[:, None] < i[None, :]).astype(np.float32)
    blk = ((i[:, None] // 8) == (i[None, :] // 8)).astype(np.float32)
    c["m_le"] = le
    c["m_lt"] = lt
    c["m_gt"] = lt.T.copy()
    c["m_le_s"] = le * blk
    c["m_lt_s"] = lt * blk
    c["m_gt_s"] = lt.T * blk
    c["m_le_b"] = le.astype(ml_dtypes.bfloat16)
    rm = np.ones((NT,), np.float32)
    for (s0, n) in TT[:-1]:
        rm[s0] = 0.0
    rm[NPR::8] = 0.0
    c["reset"] = np.broadcast_to(rm[None, :], (128, NT)).copy()
    bo = np.zeros((128, 128), np.float32)
    bo[:64, :64] = 1.0
    bo[64:, 64:] = 1.0
    c["blockones"] = bo
    c["ones_f"] = np.ones((128, 128), np.float32)
    cm = np.zeros((128, 16, 128), np.float32)
    for b in range(16):
        cm[:, b, b * 8:(b + 1) * 8] = 1.0
    c["colmask"] = cm.reshape(128, 16 * 128)
    rmk = np.zeros((128, 16), np.float32)
    for b in range(16):
        rmk[b * 8:(b + 1) * 8, b] = 1.0
    c["rowmask"] = rmk
    nm = np.zeros((8, 16, 8), np.float32)
    for sk in range(8):
        nm[sk, :, sk:] = 1.0
    c["newmask"] = nm.reshape(8, 128).astype(ml_dtypes.bfloat16)
    return c


CONST_DT = {"ident_b": BF16, "m_le_b": BF16, "newmask": BF16}

def col_tiles():
    ct = []
    for i in range(4):
        ct.append((f"q{i}", [(0, 128 * i, 128)], 128, "copy"))
    for i in range(2):
        ct.append((f"kv{i}", [(0, 512 + 128 * i, 128)], 128, "copy"))
    ct.append(("kr", [(64, 768, 32)], 96, "copy"))
    ct.append(("krs", [(64, 784, 16), (80, 768, 16)], 96, "copy"))
    for nm, off in (("r", 800), ("k", 1824), ("v", 2848)):
        for i in range(8):
            ct.append((f"{nm}{i}", [(0, off + 128 * i, 128)], 128, "copy"))
    ct.append(("wl", [(0, 3872, 96)], 96, "copy"))
    ct.append(("al", [(0, 3968, 96)], 96, "copy"))
    ct.append(("gl0", [(0, 4064, 128)], 128, "copy"))
    ct.append(("gl1", [(0, 4192, 128)], 128, "copy"))
    for i in range(16):
        ct.append((f"ga{i}", [(0, 4320 + 128 * i, 128)], 128, "sig"))
    for i in range(16):
        ct.append((f"gb{i}", [(0, 6368 + 128 * i, 128)], 128, "sig"))
    return ct


CT = col_tiles()
CTI = {c[0]: i for i, c in enumerate(CT)}

IN_SPECS = [
    ("xp", [2048, D], F32), ("xs", [NS, D], F32), ("meta", [16, D], F32),
    ("state_wkv", [16, 16, 64, 64], F32), ("state_shift", [16, RWKV_COLS], F32),
    ("page_table", [16, 128], I32),
    ("norm_mix_g", [1, D], F32), ("w_in", [D, IN_COLS], F32), ("rwkv_mu", [RWKV_COLS, 1], F32),
    ("rwkv_w0", [1024, 1], F32), ("rwkv_w2", [96, 1024], F32), ("rwkv_a0", [1024, 1], F32),
    ("rwkv_a2", [96, 1024], F32), ("rwkv_g2", [256, 1024], F32), ("rwkv_k_k", [1024, 1], F32),
    ("rwkv_k_a", [1024, 1], F32), ("rwkv_r_k", [1024, 1], F32), ("rwkv_ln_g", [1024, 1], F32),
    ("rwkv_ln_b", [1024, 1], F32), ("mla_q_norm_g", [512, 1], F32), ("mla_w_uq", [512, 1536], F32),
    ("mla_kv_norm_g", [256, 1], F32), ("mla_w_uk", [256, 1024], F32), ("mla_w_uv", [256, 1024], F32),
    ("w_up_a", [1024, D], F32), ("w_up_b", [1024, D], F32), ("w_o", [D, D], F32),
    ("norm_ffn_g", [1, D], F32), ("router_w", [D, 36], F32), ("router_b", [1, 36], F32),
    ("expert_w_gate", [32, D, 512], F32), ("expert_w_up", [32, D, 512], F32),
    ("expert_w_down", [32, 512, D], F32), ("norm_final_g", [1, D], F32),
]
OUT_SPECS = [
    ("y_prompt", [2048, D]), ("y_sample", [NS, D]), ("ckv_p", [NPR, 256]), ("kr_p", [NPR, 32]),
    ("wkv_p", [16, 64, 64]), ("sh_p", [1, RWKV_COLS]), ("ckv_s", [NS, 256]), ("kr_s", [NS, 32]),
    ("wkv_s", [16, 16, 64, 64]), ("sh_s", [16, RWKV_COLS]),
]


class Ctx:
    pass


def build(n_pool=20480, upto="all", debug=(), start=None, feed=()):
    nc = bass.Bass("TRN2", target_bir_lowering=False)
    S = Sched(nc)
    X = Ctx()
    X.nc, X.S = nc, S
    X.debug, X.upto = debug, upto
    import os as _os
    X.nct = int(_os.environ.get('KB_NCT', '1000'))
    I = {}
    for name, shape, dt in IN_SPECS:
        I[name] = nc.dram_tensor(name, shape, dt, kind="ExternalInput").ap()
    I["cache_ckv"] = nc.dram_tensor("cache_ckv", [n_pool, 128, 256], F32, kind="ExternalInput").ap()
    I["cache_krope"] = nc.dram_tensor("cache_krope", [n_pool, 128, 32], F32, kind="ExternalInput").ap()
    consts = make_consts()
    for k, v in consts.items():
        I[k] = nc.dram_tensor(k, list(v.shape), CONST_DT.get(k, F32), kind="ExternalInput").ap()
    O = {}
    for name, shape in OUT_SPECS:
        O[name] = nc.dram_tensor(name, shape, F32, kind="ExternalOutput").ap()
    X.I, X.O = I, O

    def scratch(name, shape, dt):
        kind = "ExternalOutput" if name in debug else ("ExternalInput" if name in feed else "Internal")
        return T(nc.dram_tensor(name, shape, dt, kind=kind).ap())
    X.scratch = scratch

    X.V = lambda fn, r=(), w=(): S.op("vector", fn, r, w)
    X.A = lambda fn, r=(), w=(): S.op("scalar", fn, r, w)
    X.G = lambda fn, r=(), w=(): S.op("gpsimd", fn, r, w)
    X.P = lambda fn, r=(), w=(): S.op("tensor", fn, r, w)
    X.DS = lambda fn, r=(), w=(): S.dma("sync", fn, r, w)
    X.DG = lambda fn, r=(), w=(): S.dma("gpsimd", fn, r, w)

    X.PFM = scratch("PFM", [len(CT), 128, NT], F32)
    X.RW = scratch("RW", [8, 8, 128, NT], F32)
    X.OAT = scratch("OAT", [8, 128, NT], BF16)
    X.OBT = scratch("OBT", [8, 128, NT], BF16)
    X.X2 = scratch("X2", [NT, D], F32)
    X.XN2T = scratch("XN2T", [D, NT], BF16)
    X.COMB = scratch("COMB", [NT, 32], F32)
    X.MTD = scratch("MTD", [128, 16, NT], BF16)

    stages = [stage_ab, stage_c1, stage_c2, stage_d, stage_e, stage_f]
    started = start is None
    for st in stages:
        if not started:
            if st.__name__ != start:
                continue
            started = True
        st(X)
        S.barrier()
        if upto == st.__name__:
            break
    S.finish()
    return nc, consts


class Stage:
    N = 0

    def __init__(self, X):
        self.X = X
        self.es = contextlib.ExitStack()
        self.n = 0

    def __enter__(self):
        self.es.__enter__()
        return self

    def __exit__(self, *a):
        self.X.S.barrier()
        return self.es.__exit__(*a)

    def sb(self, shape, dt=F32, name=None):
        Stage.N += 1
        return T(self.es.enter_context(self.X.nc.sbuf_tensor(f"{name or 'sb'}_{Stage.N}", list(shape), dt)))

    def ps(self, shape, dt=F32, name=None):
        Stage.N += 1
        return T(self.es.enter_context(self.X.nc.psum_tensor(f"{name or 'ps'}_{Stage.N}", list(shape), dt)))

    def load(self, ap_dram, shape, dt=F32, eng="sync", **kw):
        t = self.sb(shape, dt)
        (self.X.DS if eng == "sync" else self.X.DG)(lambda E: E.dma_start(out=t[:], in_=ap_dram, **kw), (), [t])
        return t


class RR:
    def __init__(self, items):
        self.items = items
        self.i = 0

    def next(self):
        x = self.items[self.i % len(self.items)]
        self.i += 1
        return x


def stage_ab(X):
    I, V, A, G, P, DS, DG = X.I, X.V, X.A, X.G, X.P, X.DS, X.DG
    with Stage(X) as st:
        hT = st.sb([128, 16, NT], BF16, "hT")
        identb = st.load(I["ident_b"], [128, 128], BF16)
        with Stage(X) as sa:
            gm = sa.load(I["norm_mix_g"].to_broadcast([128, D]), [128, D])
            xin = RR([sa.sb([128, D]) for _ in range(2)])
            junk = sa.sb([128, D])
            ssq = RR([sa.sb([128, 1]) for _ in range(2)])
            hb = RR([sa.sb([128, D], BF16) for _ in range(2)])
            pst = RR([sa.ps([128, 16, 128], BF16) for _ in range(2)])
            for ti, (g0, n) in enumerate(TT):
                x = xin.next()
                if ti == 0:
                    src = I["meta"]
                elif ti <= 16:
                    src = I["xp"][(ti - 1) * 128: ti * 128, :]
                else:
                    src = I["xs"]
                DS(lambda E, x=x, src=src, n=n: E.dma_start(out=x[0:n, :], in_=src), (), [x])
                sq = ssq.next()
                G(lambda E, x=x, n=n: E.tensor_tensor(out=junk[0:n, :], in0=x[0:n, :], in1=x[0:n, :], op=ALU.mult),
                  [x], [junk])
                V(lambda E, sq=sq, n=n: E.tensor_reduce(out=sq[0:n, :], in_=junk[0:n, :], axis=AX.X, op=ALU.add),
                  [junk], [sq])
                A(lambda E, sq=sq, n=n: E.activation(out=sq[0:n, :], in_=sq[0:n, :], func=AF.Sqrt,
                                                     bias=1e-6, scale=1.0 / D), [sq], [sq])
                V(lambda E, sq=sq, n=n: E.reciprocal(out=sq[0:n, :], in_=sq[0:n, :]), [sq], [sq])
                h = hb.next()
                V(lambda E, x=x, sq=sq, h=h, n=n: E.scalar_tensor_tensor(
                    out=h[0:n, :], in0=x[0:n, :], scalar=sq[0:n, 0:1], in1=gm[0:n, :],
                    op0=ALU.mult, op1=ALU.mult), [x, sq, gm], [h])
                pt = pst.next()
                for kc in range(16):
                    P(lambda E, h=h, pt=pt, kc=kc, n=n: E.transpose(
                        out=pt[:, kc, 0:n], in_=h[0:n, kc * 128:(kc + 1) * 128], identity=identb[0:n, 0:n]),
                      [h, identb], [pt])
                A(lambda E, pt=pt, g0=g0, n=n: E.copy(out=hT[:, :, g0:g0 + n], in_=pt[:, :, 0:n]), [pt], [hT])
        X.S.barrier()
        if "HT" in X.debug:
            HTd = X.scratch("HT", [128, 16, NT], BF16)
            DS(lambda E: E.dma_start(out=HTd[:], in_=hT[:]), [hT], [HTd])
        if X.upto == "A":
            return
        with Stage(X) as sb_:
            wb = RR([sb_.sb([128, 16, 128], BF16) for _ in range(3)])
            stg = RR([sb_.sb([128, NT]) for _ in range(2)])
            pss = RR([sb_.ps([128, 512]) for _ in range(4)])
            w_in = I["w_in"].rearrange("(kc p) c -> p kc c", p=128)
            for ci, (name, pieces, M, kind) in enumerate(CT):
                if ci >= X.nct:
                    break
                w = wb.next()
                if pieces[0][0] != 0:
                    G(lambda E, w=w: E.memset(w[:, :, 0:64], 0.0), (), [w])
                for (dc, sc, n) in pieces:
                    DG(lambda E, w=w, dc=dc, sc=sc, n=n: E.dma_start(out=w[:, :, dc:dc + n], in_=w_in[:, :, sc:sc + n]),
                       (), [w])
                sg = stg.next()
                for (t0, tn) in GROUPS:
                    ps = pss.next()
                    for kc in range(16):
                        P(lambda E, ps=ps, w=w, kc=kc, t0=t0, tn=tn, M=M: E.matmul(
                            ps[0:M, 0:tn], lhsT=w[:, kc, 0:M], rhs=hT[:, kc, t0:t0 + tn],
                            start=(kc == 0), stop=(kc == 15)), [w], [ps])
                    func, scl = (AF.Tanh, 0.5) if kind == "sig" else (AF.Copy, 1.0)
                    A(lambda E, ps=ps, sg=sg, t0=t0, tn=tn, M=M, func=func, scl=scl: E.activation(
                        out=sg[0:M, t0:t0 + tn], in_=ps[0:M, 0:tn], func=func, scale=scl), [ps], [sg])
                DS(lambda E, sg=sg, ci=ci, M=M: E.dma_start(out=X.PFM[ci, 0:M, :], in_=sg[0:M, :]), [sg], [X.PFM])


NLW = -0.30326533


def stage_c1(X):
    I, O, V, A, G, P, DS, DG = X.I, X.O, X.V, X.A, X.G, X.P, X.DS, X.DG
    with Stage(X) as st:
        blockones = st.load(I["blockones"], [128, 128])
        reset = st.load(I["reset"], [128, NT])
        tw = st.sb([128, NT])
        xa = st.sb([128, NT])
        sg = [st.sb([128, NT]), st.sb([128, NT])]
        pbuf = RR([st.sb([128, NT]) for _ in range(2)])
        dbuf = RR([st.sb([128, NT]) for _ in range(2)])
        small = RR([st.sb([128, 32]) for _ in range(8)])
        pss = RR([st.ps([128, 512]) for _ in range(6)])

        def colvec(name, c0, M):
            t = small.next()
            DS(lambda E: E.dma_start(out=t[0:M, 0:1], in_=I[name][c0:c0 + M, :]), (), [t])
            return t

        def xs_tile(name, out):
            ci = CTI[name]
            _, pieces, M, _ = CT[ci]
            rc0 = pieces[0][1] - OFF_RWKV
            p = pbuf.next()
            DS(lambda E: E.dma_start(out=p[0:M, :], in_=X.PFM[ci, 0:M, :]), [X.PFM], [p])
            mu = colvec("rwkv_mu", rc0, M)
            sh = small.next()
            DS(lambda E: E.dma_start(out=sh[0:M, 0:16], in_=I["state_shift"][:, rc0:rc0 + M].rearrange("b c -> c b"),
                                     allow_slow_non_contiguous=True), (), [sh])
            DS(lambda E: E.dma_start(out=O["sh_p"][0:1, rc0:rc0 + M].rearrange("o c -> c o"), in_=p[0:M, NPR - 1:NPR],
                                     allow_slow_non_contiguous=True), [p], [])
            ps_ = p[0:M, NPR:NT].rearrange("p (b s) -> p b s", s=8)
            DS(lambda E: E.dma_start(out=O["sh_s"][:, rc0:rc0 + M].rearrange("b c -> c b"), in_=ps_[:, :, 7],
                                     allow_slow_non_contiguous=True), [p], [])
            d = dbuf.next()
            ds_ = d[0:M, NPR:NT].rearrange("p (b s) -> p b s", s=8)
            G(lambda E: E.tensor_tensor(out=d[0:M, 1:NPR], in0=p[0:M, 0:NPR - 1], in1=p[0:M, 1:NPR], op=ALU.subtract), [p], [d])
            V(lambda E: E.tensor_scalar(out=d[0:M, 0:1], in0=p[0:M, 0:1], scalar1=-1.0, scalar2=None, op0=ALU.mult), [p], [d])
            V(lambda E: E.tensor_tensor(out=ds_[:, :, 1:8], in0=ps_[:, :, 0:7], in1=ps_[:, :, 1:8], op=ALU.subtract), [p], [d])
            V(lambda E: E.tensor_tensor(out=ds_[:, :, 0], in0=sh[0:M, 0:16], in1=ps_[:, :, 0], op=ALU.subtract), [p, sh], [d])
            V(lambda E: E.scalar_tensor_tensor(out=out[0:M, :], in0=d[0:M, :], scalar=mu[0:M, 0:1], in1=p[0:M, :],
                                               op0=ALU.mult, op1=ALU.add), [d, mu, p], [out])

        xs_tile("wl", tw)
        A(lambda E: E.activation(out=tw[0:96, :], in_=tw[0:96, :], func=AF.Tanh), [tw], [tw])
        xs_tile("al", xa)
        for i in range(2):
            xs_tile(f"gl{i}", sg[i])
            A(lambda E, i=i: E.activation(out=sg[i][:], in_=sg[i][:], func=AF.Tanh, scale=0.5), [sg[i]], [sg[i]])
            V(lambda E, i=i: E.tensor_scalar(out=sg[i][:], in0=sg[i][:], scalar1=0.5, scalar2=0.5, op0=ALU.mult, op1=ALU.add),
              [sg[i]], [sg[i]])
        w2 = st.load(I["rwkv_w2"], [96, 1024])
        a2 = st.load(I["rwkv_a2"], [96, 1024])
        g2 = st.load(I["rwkv_g2"].rearrange("(kc p) c -> p kc c", p=128), [128, 2, 1024])
        big = RR([st.sb([128, NT]) for _ in range(12)])
        for j in range(8):
            c0 = j * 128
            xr, xk, xv = big.next(), big.next(), big.next()
            xs_tile(f"r{j}", xr)
            xs_tile(f"k{j}", xk)
            xs_tile(f"v{j}", xv)
            w0 = colvec("rwkv_w0", c0, 128)
            a0 = colvec("rwkv_a0", c0, 128)
            V(lambda E, w0=w0: E.tensor_scalar(out=w0[:, 0:1], in0=w0[:, 0:1], scalar1=0.5, scalar2=None, op0=ALU.mult), [w0], [w0])
            V(lambda E, a0=a0: E.tensor_scalar(out=a0[:, 0:1], in0=a0[:, 0:1], scalar1=0.5, scalar2=None, op0=ALU.mult), [a0], [a0])
            kkv = colvec("rwkv_k_k", c0, 128)
            kav = colvec("rwkv_k_a", c0, 128)
            rkv = colvec("rwkv_r_k", c0, 128)
            logw, alpha, gg = big.next(), big.next(), big.next()
            for (t0, tn) in GROUPS:
                ps = pss.next()
                P(lambda E, ps=ps, t0=t0, tn=tn: E.matmul(ps[:, 0:tn], lhsT=w2[0:96, c0:c0 + 128], rhs=tw[0:96, t0:t0 + tn],
                                                          start=True, stop=True), [w2, tw], [ps])
                A(lambda E, ps=ps, t0=t0, tn=tn: E.activation(out=logw[:, t0:t0 + tn], in_=ps[:, 0:tn], func=AF.Tanh,
                                                              bias=w0[:, 0:1], scale=0.5), [ps, w0], [logw])
                ps = pss.next()
                P(lambda E, ps=ps, t0=t0, tn=tn: E.matmul(ps[:, 0:tn], lhsT=a2[0:96, c0:c0 + 128], rhs=xa[0:96, t0:t0 + tn],
                                                          start=True, stop=True), [a2, xa], [ps])
                A(lambda E, ps=ps, t0=t0, tn=tn: E.activation(out=alpha[:, t0:t0 + tn], in_=ps[:, 0:tn], func=AF.Tanh,
                                                              bias=a0[:, 0:1], scale=0.5), [ps, a0], [alpha])
                ps = pss.next()
                for kc in range(2):
                    P(lambda E, ps=ps, t0=t0, tn=tn, kc=kc: E.matmul(ps[:, 0:tn], lhsT=g2[:, kc, c0:c0 + 128],
                                                                     rhs=sg[kc][:, t0:t0 + tn], start=(kc == 0), stop=(kc == 1)),
                      [g2, sg[kc]], [ps])
                A(lambda E, ps=ps, t0=t0, tn=tn: E.copy(out=gg[:, t0:t0 + tn], in_=ps[:, 0:tn]), [ps], [gg])
            V(lambda E: E.tensor_scalar(out=logw[:], in0=logw[:], scalar1=NLW, scalar2=NLW, op0=ALU.mult, op1=ALU.add), [logw], [logw])
            G(lambda E: E.tensor_scalar(out=alpha[:], in0=alpha[:], scalar1=0.5, scalar2=0.5, op0=ALU.mult, op1=ALU.add), [alpha], [alpha])
            kk, sq, rs = big.next(), big.next(), big.next()
            V(lambda E: E.tensor_scalar(out=kk[:], in0=xk[:], scalar1=kkv[:, 0:1], scalar2=None, op0=ALU.mult), [xk, kkv], [kk])
            G(lambda E: E.tensor_tensor(out=sq[:], in0=kk[:], in1=kk[:], op=ALU.mult), [kk], [sq])
            for (t0, tn) in GROUPS:
                ps = pss.next()
                P(lambda E, ps=ps, t0=t0, tn=tn: E.matmul(ps[:, 0:tn], lhsT=blockones[:], rhs=sq[:, t0:t0 + tn], start=True, stop=True),
                  [blockones, sq], [ps])
                V(lambda E, ps=ps, t0=t0, tn=tn: E.tensor_scalar(out=rs[:, t0:t0 + tn], in0=ps[:, 0:tn], scalar1=1e-24, scalar2=None,
                                                                 op0=ALU.max), [ps], [rs])
            A(lambda E: E.activation(out=rs[:], in_=rs[:], func=AF.Sqrt), [rs], [rs])
            V(lambda E: E.reciprocal(out=rs[:], in_=rs[:]), [rs], [rs])
            V(lambda E: E.tensor_tensor(out=kk[:], in0=kk[:], in1=rs[:], op=ALU.mult), [kk, rs], [kk])
            kmod = big.next()
            V(lambda E: E.tensor_scalar(out=kmod[:], in0=alpha[:], scalar1=-1.0, scalar2=kav[:, 0:1], op0=ALU.add, op1=ALU.mult),
              [alpha, kav], [kmod])
            V(lambda E: E.scalar_tensor_tensor(out=kmod[:], in0=kmod[:], scalar=1.0, in1=xk[:], op0=ALU.add, op1=ALU.mult),
              [kmod, xk], [kmod])
            cum = big.next()
            V(lambda E: E.tensor_tensor_scan(out=cum[:], data0=reset[:], data1=logw[:], initial=0.0, op0=ALU.mult, op1=ALU.add),
              [reset, logw], [cum])
            ew, ewm, ewi = big.next(), sq, rs
            A(lambda E: E.activation(out=ew[:], in_=cum[:], func=AF.Exp), [cum], [ew])
            A(lambda E: E.activation(out=ewi[:], in_=cum[:], func=AF.Exp, scale=-1.0), [cum], [ewi])
            G(lambda E: E.tensor_tensor(out=ewm[:], in0=cum[:], in1=logw[:], op=ALU.subtract), [cum, logw], [ewm])
            A(lambda E: E.activation(out=ewm[:], in_=ewm[:], func=AF.Exp), [ewm], [ewm])
            RWj = lambda a: X.RW[j, a, :, :]
            V(lambda E: E.scalar_tensor_tensor(out=ewm[:], in0=kk[:], scalar=-1.0, in1=ewm[:], op0=ALU.mult, op1=ALU.mult), [kk, ewm], [ewm])
            DS(lambda E: E.dma_start(out=RWj(0), in_=ewm[:]), [ewm], [X.RW])
            V(lambda E: E.scalar_tensor_tensor(out=cum[:], in0=xr[:], scalar=rkv[:, 0:1], in1=kmod[:], op0=ALU.mult, op1=ALU.mult),
              [xr, rkv, kmod], [cum])
            G(lambda E: E.tensor_tensor(out=xr[:], in0=xr[:], in1=ew[:], op=ALU.mult), [xr, ew], [xr])
            DS(lambda E: E.dma_start(out=RWj(1), in_=xr[:]), [xr], [X.RW])
            G(lambda E: E.tensor_tensor(out=kk[:], in0=kk[:], in1=alpha[:], op=ALU.mult), [kk, alpha], [kk])
            V(lambda E: E.tensor_tensor(out=kk[:], in0=kk[:], in1=ewi[:], op=ALU.mult), [kk, ewi], [kk])
            DS(lambda E: E.dma_start(out=RWj(2), in_=kk[:]), [kk], [X.RW])
            G(lambda E: E.tensor_tensor(out=kmod[:], in0=kmod[:], in1=ewi[:], op=ALU.mult), [kmod, ewi], [kmod])
            DS(lambda E: E.dma_start(out=RWj(3), in_=kmod[:]), [kmod], [X.RW])
            DS(lambda E: E.dma_start(out=RWj(4), in_=xv[:]), [xv], [X.RW])
            for (t0, tn) in GROUPS:
                ps = pss.next()
                P(lambda E, ps=ps, t0=t0, tn=tn: E.matmul(ps[:, 0:tn], lhsT=blockones[:], rhs=cum[:, t0:t0 + tn], start=True, stop=True),
                  [blockones, cum], [ps])
                V(lambda E, ps=ps, t0=t0, tn=tn: E.tensor_tensor(out=logw[:, t0:t0 + tn], in0=ps[:, 0:tn], in1=xv[:, t0:t0 + tn],
                                                                 op=ALU.mult), [ps, xv], [logw])
            DS(lambda E: E.dma_start(out=RWj(5), in_=logw[:]), [logw], [X.RW])
            DS(lambda E: E.dma_start(out=RWj(6), in_=gg[:]), [gg], [X.RW])
            DS(lambda E: E.dma_start(out=RWj(7), in_=ew[:]), [ew], [X.RW])


def stage_c2(X):
    I, O, V, A, G, P, DS, DG = X.I, X.O, X.V, X.A, X.G, X.P, X.DS, X.DG
    with Stage(X) as st:
        identf = st.load(I["ident_f"], [128, 128])
        mk4 = {}
        mk1 = {}
        for sfx in ("", "_s"):
            m4 = st.sb([128, 4, 128])
            for a, nm in enumerate(("m_lt", "m_gt", "m_lt", "m_le")):
                DS(lambda E: E.dma_start(out=m4[:, a, :], in_=I[nm + sfx]), (), [m4])
            mk4[sfx] = m4
            mk1[sfx] = st.load(I["m_le" + sfx], [128, 128])
        colmask = st.load(I["colmask"], [128, 2048])
        rowmask = st.load(I["rowmask"], [128, 16])
        lng = st.load(I["rwkv_ln_g"].rearrange("(j p) o -> p (j o)", p=128), [128, 8])
        lnb = st.load(I["rwkv_ln_b"].rearrange("(j p) o -> p (j o)", p=128), [128, 8])
        STp = [st.sb([128, 64]) for _ in range(8)]
        for j in range(8):
            G(lambda E: E.memset(STp[j][:], 0.0), (), [STp[j]])
        STs = [st.sb([128, 16, 64]) for _ in range(8)]
        pss = RR([st.ps([128, 512]) for _ in range(8)])
        with Stage(X) as s0:
            sin = RR([s0.sb([64, 16, 128]) for _ in range(2)])
            for j in range(8):
                si = sin.next()
                for h2 in range(2):
                    DS(lambda E: E.dma_start(out=si[:, :, h2 * 64:h2 * 64 + 64],
                                             in_=I["state_wkv"][:, 2 * j + h2, :, :].rearrange("b v k -> v b k")), (), [si])
                for half in range(2):
                    ps = pss.next()
                    for bb in range(8):
                        b = half * 8 + bb
                        P(lambda E: E.transpose(out=ps[:, bb * 64:(bb + 1) * 64], in_=si[0:64, b, :], identity=identf[0:64, 0:64]),
                          [si, identf], [ps])
                    A(lambda E: E.copy(out=STs[j][:, half * 8:half * 8 + 8, :], in_=ps[:, :].rearrange("p (b v) -> p b v", v=64)),
                      [ps], [STs[j]])
        inpool = RR([st.sb([128, 8, 5, 128]) for _ in range(1)])
        tmpool = RR([st.sb([128, 8, 3, 128]) for _ in range(1)])
        bgpool = RR([st.sb([128, 8, 2, 128]) for _ in range(1)])
        wcpool = RR([st.sb([128, 8, 16]) for _ in range(2)])
        M4 = [st.sb([128, 4, 128]) for _ in range(8)]
        MKR = [st.sb([128, 128]) for _ in range(8)]
        LV = [[st.sb([128, 2, 128]) for _ in range(2)] for _ in range(8)]
        PM = [[st.sb([128, 128]) for _ in range(2)] for _ in range(8)]
        XT = st.sb([128, 16, 64])
        UT = st.sb([128, 16, 64])
        Y = st.sb([128, 16, 64])
        cen = st.sb([128, 16, 64])
        sqv = st.sb([128, 16, 64])
        stat = RR([st.sb([128, 16]) for _ in range(4)])
        bdp = RR([st.sb([128, 16, 128]) for _ in range(2)])
        ubd = RR([st.sb([128, 16, 64]) for _ in range(2)])
        tmpf = RR([st.sb([128, 128]) for _ in range(3)])
        oap = RR([st.sb([128, 8, 128], BF16) for _ in range(2)])
        stmp = RR([st.sb([128, 16, 64]) for _ in range(2)])

        for ci, (g0, n) in enumerate(TT):
            is_s = (ci == len(TT) - 1)
            sfx = "_s" if is_s else ""
            L = 3 if is_s else (4 if n == 16 else 7)
            IN = inpool.next()
            for a in range(5):
                DS(lambda E: E.dma_start(out=IN[:, :, a, 0:n], in_=X.RW[:, a, :, g0:g0 + n].rearrange("j p t -> p j t")), [X.RW], [IN])
            BG = bgpool.next()
            for a in range(2):
                DS(lambda E: E.dma_start(out=BG[:, :, a, 0:n], in_=X.RW[:, 5 + a, :, g0:g0 + n].rearrange("j p t -> p j t")), [X.RW], [BG])
            WC = wcpool.next()
            if not is_s:
                DS(lambda E: E.dma_start(out=WC[:, :, 0], in_=X.RW[:, 7, :, g0 + n - 1].rearrange("j p -> p j"),
                                         allow_slow_non_contiguous=True), [X.RW], [WC])
            else:
                for j in range(8):
                    DS(lambda E: E.dma_start(out=WC[:, j, :], in_=X.RW[j, 7, :, NPR:NT].rearrange("p (b s) -> p b s", s=8)[:, :, 7],
                                             allow_slow_non_contiguous=True), [X.RW], [WC])
            TM = tmpool.next()
            for j in range(8):
                ps = pss.next()
                for a3, a in enumerate((2, 3, 4)):
                    P(lambda E: E.transpose(out=ps[0:n, a3 * 128:(a3 + 1) * 128], in_=IN[:, j, a, 0:n], identity=identf[:, :]),
                      [IN, identf], [ps])
                A(lambda E: E.copy(out=TM[0:n, j, :, :], in_=ps[0:n, 0:384].rearrange("p (a t) -> p a t", a=3)), [ps], [TM])
            npb = 1 if is_s else 4
            for hb in range(8 // npb):
                heads = [(hb * npb + jj, h2) for jj in range(npb) for h2 in range(2)]
                NH = len(heads)
                for hi, (j, h2) in enumerate(heads):
                    rows = slice(h2 * 64, h2 * 64 + 64)
                    at, rt, bt, kt = (IN[rows, j, a, 0:n] for a in range(4))
                    ps = pss.next()
                    for a, (l, r) in enumerate(((bt, at), (at, bt), (kt, at), (bt, rt))):
                        P(lambda E: E.matmul(ps[0:n, a * 128:a * 128 + n], lhsT=l, rhs=r, start=True, stop=True), [IN], [ps])
                    V(lambda E: E.tensor_tensor(out=M4[hi][0:n, :, 0:n], in0=ps[0:n, :].rearrange("p (a t) -> p a t", a=4)[:, :, 0:n],
                                                in1=mk4[sfx][0:n, :, 0:n], op=ALU.mult), [ps, mk4[sfx]], [M4[hi]])
                    ps = pss.next()
                    P(lambda E: E.matmul(ps[0:n, 0:n], lhsT=kt, rhs=rt, start=True, stop=True), [IN], [ps])
                    V(lambda E: E.tensor_tensor(out=MKR[hi][0:n, 0:n], in0=ps[0:n, 0:n], in1=mk1[sfx][0:n, 0:n], op=ALU.mult),
                      [ps, mk1[sfx]], [MKR[hi]])
                    G(lambda E: E.tensor_tensor(out=PM[hi][0][0:n, 0:n], in0=M4[hi][0:n, 0, 0:n], in1=identf[0:n, 0:n], op=ALU.add),
                      [M4[hi], identf], [PM[hi][0]])
                for i in range(1, L):
                    for hi in range(NH):
                        if i == 1:
                            Ap, ATp, srcT = M4[hi][0:n, 0, 0:n], M4[hi][0:n, 1, 0:n], M4[hi]
                        else:
                            srcT = LV[hi][(i - 1) % 2]
                            Ap, ATp = srcT[0:n, 0, 0:n], srcT[0:n, 1, 0:n]
                        ps = pss.next()
                        P(lambda E: E.matmul(ps[0:n, 0:n], lhsT=ATp, rhs=Ap, start=True, stop=True), [srcT], [ps])
                        P(lambda E: E.matmul(ps[0:n, 128:128 + n], lhsT=Ap, rhs=ATp, start=True, stop=True), [srcT], [ps])
                        dst = LV[hi][i % 2]
                        A(lambda E: E.copy(out=dst[0:n, :, 0:n], in_=ps[0:n, 0:256].rearrange("p (a t) -> p a t", a=2)[:, :, 0:n]),
                          [ps], [dst])
                    for hi in range(NH):
                        cur = LV[hi][i % 2]
                        Pp, Pn = PM[hi][(i - 1) % 2], PM[hi][i % 2]
                        ps = pss.next()
                        P(lambda E: E.matmul(ps[0:n, 0:n], lhsT=cur[0:n, 1, 0:n], rhs=Pp[0:n, 0:n], start=True, stop=True), [cur, Pp], [ps])
                        V(lambda E: E.tensor_tensor(out=Pn[0:n, 0:n], in0=ps[0:n, 0:n], in1=Pp[0:n, 0:n], op=ALU.add), [ps, Pp], [Pn])
                Pf = [PM[hi][(L - 1) % 2] for hi in range(NH)]
                bd = {}
                if is_s:
                    for jj in range(npb):
                        j = hb * npb + jj
                        for a in range(2):
                            t = bdp.next()
                            V(lambda E: E.tensor_tensor(out=t[:], in0=IN[:, j, a, :].unsqueeze(1).to_broadcast([128, 16, 128]),
                                                        in1=colmask[:, :].rearrange("p (b t) -> p b t", b=16), op=ALU.mult),
                              [IN, colmask], [t])
                            bd[(j, a)] = t
                for hi, (j, h2) in enumerate(heads):
                    h = 2 * j + h2
                    rows = slice(h2 * 64, h2 * 64 + 64)
                    vtm = TM[0:n, j, 2, h2 * 64:h2 * 64 + 64]
                    ps = pss.next()
                    if not is_s:
                        P(lambda E: E.matmul(ps[0:n, 0:64], lhsT=IN[rows, j, 0, 0:n], rhs=STp[j][rows, :], start=True, stop=False),
                          [IN, STp[j]], [ps])
                    else:
                        for b in range(16):
                            P(lambda E: E.matmul(ps[0:n, 0:64], lhsT=bd[(j, 0)][rows, b, :], rhs=STs[j][rows, b, :],
                                                 start=(b == 0), stop=False), [bd[(j, 0)], STs[j]], [ps])
                    P(lambda E: E.matmul(ps[0:n, 0:64], lhsT=M4[hi][0:n, 2, 0:n], rhs=vtm, start=False, stop=True), [M4[hi], TM], [ps])
                    A(lambda E: E.copy(out=XT[0:n, h, :], in_=ps[0:n, 0:64]), [ps], [XT])
                for hi, (j, h2) in enumerate(heads):
                    h = 2 * j + h2
                    ps = pss.next()
                    P(lambda E: E.matmul(ps[0:n, 0:64], lhsT=Pf[hi][0:n, 0:n], rhs=XT[0:n, h, :], start=True, stop=True), [Pf[hi], XT], [ps])
                    A(lambda E: E.copy(out=UT[0:n, h, :], in_=ps[0:n, 0:64]), [ps], [UT])
                for hi, (j, h2) in enumerate(heads):
                    h = 2 * j + h2
                    rows = slice(h2 * 64, h2 * 64 + 64)
                    vtm = TM[0:n, j, 2, h2 * 64:h2 * 64 + 64]
                    ps = pss.next()
                    if not is_s:
                        P(lambda E: E.matmul(ps[0:n, 0:64], lhsT=IN[rows, j, 1, 0:n], rhs=STp[j][rows, :], start=True, stop=False),
                          [IN, STp[j]], [ps])
                    else:
                        for b in range(16):
                            P(lambda E: E.matmul(ps[0:n, 0:64], lhsT=bd[(j, 1)][rows, b, :], rhs=STs[j][rows, b, :],
                                                 start=(b == 0), stop=False), [bd[(j, 1)], STs[j]], [ps])
                    P(lambda E: E.matmul(ps[0:n, 0:64], lhsT=M4[hi][0:n, 3, 0:n], rhs=UT[0:n, h, :], start=False, stop=False), [M4[hi], UT], [ps])
                    P(lambda E: E.matmul(ps[0:n, 0:64], lhsT=MKR[hi][0:n, 0:n], rhs=vtm, start=False, stop=True), [MKR[hi], TM], [ps])
                    A(lambda E: E.copy(out=Y[0:n, h, :], in_=ps[0:n, 0:64]), [ps], [Y])
                for hi, (j, h2) in enumerate(heads):
                    h = 2 * j + h2
                    rows = slice(h2 * 64, h2 * 64 + 64)
                    vtm = TM[0:n, j, 2, h2 * 64:h2 * 64 + 64]
                    if not is_s:
                        ps = pss.next()
                        P(lambda E: E.matmul(ps[:, 0:64], lhsT=TM[0:n, j, 0, :], rhs=UT[0:n, h, :], start=True, stop=False), [TM, UT], [ps])
                        P(lambda E: E.matmul(ps[:, 0:64], lhsT=TM[0:n, j, 1, :], rhs=vtm, start=False, stop=True), [TM], [ps])
                        V(lambda E: E.tensor_tensor(out=STp[j][rows, :], in0=ps[rows, 0:64], in1=STp[j][rows, :], op=ALU.add), [ps, STp[j]], [STp[j]])
                        V(lambda E: E.tensor_scalar(out=STp[j][rows, :], in0=STp[j][rows, :], scalar1=WC[rows, j, 0:1], scalar2=None,
                                                    op0=ALU.mult), [STp[j], WC], [STp[j]])
                    else:
                        ub, vb = ubd.next(), ubd.next()
                        rmb = rowmask[:, :].unsqueeze(2).to_broadcast([128, 16, 64])
                        V(lambda E: E.tensor_tensor(out=ub[:], in0=UT[:, h, :].unsqueeze(1).to_broadcast([128, 16, 64]), in1=rmb, op=ALU.mult),
                          [UT, rowmask], [ub])
                        V(lambda E: E.tensor_tensor(out=vb[:], in0=vtm.unsqueeze(1).to_broadcast([128, 16, 64]), in1=rmb, op=ALU.mult),
                          [TM, rowmask], [vb])
                        for half in range(2):
                            ps = pss.next()
                            bs = slice(half * 8, half * 8 + 8)
                            P(lambda E: E.matmul(ps[:, :], lhsT=TM[:, j, 0, :], rhs=ub[:, bs, :].rearrange("p b v -> p (b v)"),
                                                 start=True, stop=False), [TM, ub], [ps])
                            P(lambda E: E.matmul(ps[:, :], lhsT=TM[:, j, 1, :], rhs=vb[:, bs, :].rearrange("p b v -> p (b v)"),
                                                 start=False, stop=True), [TM, vb], [ps])
                            tt = stmp.next()
                            V(lambda E: E.tensor_tensor(out=tt[rows, 0:8, :], in0=ps[rows, :].rearrange("p (b v) -> p b v", v=64),
                                                        in1=STs[j][rows, bs, :], op=ALU.add), [ps, STs[j]], [tt])
                            V(lambda E: E.tensor_tensor(out=STs[j][rows, bs, :], in0=tt[rows, 0:8, :],
                                                        in1=WC[rows, j, bs].unsqueeze(2).to_broadcast([64, 8, 64]), op=ALU.mult),
                              [tt, WC], [STs[j]])
            mu, var = stat.next(), stat.next()
            V(lambda E: E.tensor_reduce(out=mu[0:n, :], in_=Y[0:n, :, :], axis=AX.X, op=ALU.add), [Y], [mu])
            V(lambda E: E.tensor_scalar(out=mu[0:n, :], in0=mu[0:n, :], scalar1=1.0 / 64, scalar2=None, op0=ALU.mult), [mu], [mu])
            G(lambda E: E.tensor_tensor(out=cen[0:n], in0=Y[0:n], in1=mu[0:n, :].unsqueeze(2).to_broadcast([n, 16, 64]), op=ALU.subtract),
              [Y, mu], [cen])
            G(lambda E: E.tensor_tensor(out=sqv[0:n], in0=cen[0:n], in1=cen[0:n], op=ALU.mult), [cen], [sqv])
            V(lambda E: E.tensor_reduce(out=var[0:n, :], in_=sqv[0:n, :, :], axis=AX.X, op=ALU.add), [sqv], [var])
            A(lambda E: E.activation(out=var[0:n, :], in_=var[0:n, :], func=AF.Sqrt, bias=64e-5, scale=1.0 / 64), [var], [var])
            V(lambda E: E.reciprocal(out=var[0:n, :], in_=var[0:n, :]), [var], [var])
            V(lambda E: E.tensor_tensor(out=cen[0:n], in0=cen[0:n], in1=var[0:n, :].unsqueeze(2).to_broadcast([n, 16, 64]), op=ALU.mult),
              [cen, var], [cen])
            OA = oap.next()
            for j in range(8):
                ps = pss.next()
                P(lambda E: E.transpose(out=ps[:, 0:n], in_=cen[0:n, 2 * j:2 * j + 2, :].rearrange("p h v -> p (h v)"),
                                        identity=identf[0:n, 0:n]), [cen, identf], [ps])
                tf = tmpf.next()
                V(lambda E: E.tensor_scalar(out=tf[:, 0:n], in0=ps[:, 0:n], scalar1=lng[:, j:j + 1], scalar2=lnb[:, j:j + 1],
                                            op0=ALU.mult, op1=ALU.add), [ps, lng, lnb], [tf])
                G(lambda E: E.tensor_tensor(out=tf[:, 0:n], in0=tf[:, 0:n], in1=BG[:, j, 0, 0:n], op=ALU.add), [tf, BG], [tf])
                G(lambda E: E.tensor_tensor(out=OA[:, j, 0:n], in0=tf[:, 0:n], in1=BG[:, j, 1, 0:n], op=ALU.mult), [tf, BG], [OA])
            DS(lambda E: E.dma_start(out=X.OAT[:, :, g0:g0 + n].rearrange("j p t -> p j t"), in_=OA[:, :, 0:n]), [OA], [X.OAT])
        with Stage(X) as s1:
            so = s1.sb([64, 8, 128])
            for j in range(8):
                ps = pss.next()
                P(lambda E: E.transpose(out=ps[0:64, 0:128], in_=STp[j][:, :], identity=identf[:, :]), [STp[j], identf], [ps])
                A(lambda E: E.copy(out=so[:, j, :], in_=ps[0:64, 0:128]), [ps], [so])
            for h2 in range(2):
                DS(lambda E: E.dma_start(out=O["wkv_p"].rearrange("(j h) v k -> h v j k", h=2)[h2],
                                         in_=so[:, :, h2 * 64:h2 * 64 + 64]), [so], [])
            sos = RR([s1.sb([64, 16, 128]) for _ in range(1)])
            for j in range(8):
                sj = sos.next()
                for q in range(4):
                    ps = pss.next()
                    for bb in range(4):
                        b = q * 4 + bb
                        P(lambda E: E.transpose(out=ps[0:64, bb * 128:(bb + 1) * 128], in_=STs[j][:, b, :], identity=identf[:, :]),
                          [STs[j], identf], [ps])
                    A(lambda E: E.copy(out=sj[:, q * 4:q * 4 + 4, :], in_=ps[0:64, :].rearrange("p (b k) -> p b k", b=4)), [ps], [sj])
                for h2 in range(2):
                    DS(lambda E: E.dma_start(out=O["wkv_s"][:, 2 * j + h2, :, :].rearrange("b v k -> v b k"),
                                             in_=sj[:, :, h2 * 64:h2 * 64 + 64]), [sj], [])


def stage_d(X):
    I, O, V, A, G, P, DS, DG = X.I, X.O, X.V, X.A, X.G, X.P, X.DS, X.DG
    with Stage(X) as st:
        identb = st.load(I["ident_b"], [128, 128], BF16)
        identf = st.load(I["ident_f"], [128, 128])
        onesf = st.load(I["ones_f"], [128, 128])
        mleb = st.load(I["m_le_b"], [128, 128], BF16)
        newmask = st.load(I["newmask"], [8, 128], BF16)
        ropec = st.load(I["rope_c"], [128, NT])
        ropes = st.load(I["rope_s"], [128, NT])
        CQ = st.sb([128, 4, NT], BF16)
        CKVb = st.sb([128, 2, NT], BF16)
        KRb = st.sb([128, NT], BF16)
        KCN = st.sb([8, 16, 257], BF16)
        with Stage(X) as s1:
            pss = RR([s1.ps([128, 512]) for _ in range(4)])
            pbuf = [s1.sb([128, NT]) for _ in range(4)]
            sq = s1.sb([128, NT])
            rq = s1.sb([128, NT])
            CKV = s1.sb([128, 2, NT])
            KR = s1.sb([128, NT])
            gv = s1.sb([128, 8])

            def norm(names, gname, width, outs):
                nt_ = len(names)
                for i, nm in enumerate(names):
                    DS(lambda E: E.dma_start(out=pbuf[i][:], in_=X.PFM[CTI[nm], :, :]), [X.PFM], [pbuf[i]])
                DS(lambda E: E.dma_start(out=gv[:, 0:nt_], in_=I[gname].rearrange("(j p) o -> p (j o)", p=128)), (), [gv])
                for (t0, tn) in GROUPS:
                    ps = pss.next()
                    for i in range(nt_):
                        G(lambda E: E.tensor_tensor(out=sq[:, t0:t0 + tn], in0=pbuf[i][:, t0:t0 + tn], in1=pbuf[i][:, t0:t0 + tn], op=ALU.mult),
                          [pbuf[i]], [sq])
                        P(lambda E: E.matmul(ps[:, 0:tn], lhsT=onesf[:], rhs=sq[:, t0:t0 + tn], start=(i == 0), stop=(i == nt_ - 1)),
                          [onesf, sq], [ps])
                    A(lambda E: E.activation(out=rq[:, t0:t0 + tn], in_=ps[:, 0:tn], func=AF.Sqrt, bias=1e-6, scale=1.0 / width), [ps], [rq])
                V(lambda E: E.reciprocal(out=rq[:], in_=rq[:]), [rq], [rq])
                for i in range(nt_):
                    for o in outs:
                        V(lambda E: E.scalar_tensor_tensor(out=o[:, i, :], in0=pbuf[i][:], scalar=gv[:, i:i + 1], in1=rq[:],
                                                           op0=ALU.mult, op1=ALU.mult), [pbuf[i], gv, rq], [o])

            norm([f"q{i}" for i in range(4)], "mla_q_norm_g", 512, [CQ])
            norm(["kv0", "kv1"], "mla_kv_norm_g", 256, [CKV, CKVb])
            DS(lambda E: E.dma_start(out=pbuf[0][64:96, :], in_=X.PFM[CTI["kr"], 64:96, :]), [X.PFM], [pbuf[0]])
            DS(lambda E: E.dma_start(out=pbuf[1][64:96, :], in_=X.PFM[CTI["krs"], 64:96, :]), [X.PFM], [pbuf[1]])
            r_ = slice(64, 96)
            V(lambda E: E.tensor_tensor(out=KR[r_, :], in0=pbuf[0][r_, :], in1=ropec[r_, :], op=ALU.mult), [pbuf[0], ropec], [KR])
            G(lambda E: E.tensor_tensor(out=sq[r_, :], in0=pbuf[1][r_, :], in1=ropes[r_, :], op=ALU.mult), [pbuf[1], ropes], [sq])
            V(lambda E: E.tensor_tensor(out=KR[r_, :], in0=KR[r_, :], in1=sq[r_, :], op=ALU.add), [KR, sq], [KR])
            A(lambda E: E.copy(out=KRb[r_, :], in_=KR[r_, :]), [KR], [KRb])
            otm = RR([s1.sb([128, 288]) for _ in range(2)])
            for ti, (g0, n) in enumerate(TT):
                ps = pss.next()
                for kc in range(2):
                    P(lambda E: E.transpose(out=ps[0:n, kc * 128:(kc + 1) * 128], in_=CKV[:, kc, g0:g0 + n], identity=identf[:, :]),
                      [CKV, identf], [ps])
                P(lambda E: E.transpose(out=ps[0:n, 256:288], in_=KR[r_, g0:g0 + n], identity=identf[r_, r_]), [KR, identf], [ps])
                ot = otm.next()
                A(lambda E: E.copy(out=ot[0:n, :], in_=ps[0:n, 0:288]), [ps], [ot])
                if ti < 17:
                    DS(lambda E: E.dma_start(out=O["ckv_p"][g0:g0 + n, :], in_=ot[0:n, 0:256]), [ot], [])
                    DS(lambda E: E.dma_start(out=O["kr_p"][g0:g0 + n, :], in_=ot[0:n, 256:288]), [ot], [])
                else:
                    wdep = Buf()
                    DS(lambda E: E.dma_start(out=O["ckv_s"][:, :], in_=ot[0:n, 0:256]), [ot], [wdep])
                    DS(lambda E: E.dma_start(out=O["kr_s"][:, :], in_=ot[0:n, 256:288]), [ot], [])
                    kcf = s1.sb([8, 16, 256])
                    DS(lambda E: E.dma_start(out=kcf[:], in_=O["ckv_s"].rearrange("(b s) c -> s b c", s=8)), [wdep], [kcf])
                    V(lambda E: E.tensor_copy(out=KCN[:, :, 0:256], in_=kcf[:]), [kcf], [KCN])
                    V(lambda E: E.memset(KCN[:, :, 256:257], 1.0), (), [KCN])
        QLAT = st.sb([128, 2, 16, 16, 8], BF16)
        QR = st.sb([128, 16, 16, 8], BF16)
        WUVb = st.sb([128, 2, 1024], BF16)
        DG(lambda E: E.dma_start(out=WUVb[:], in_=I["mla_w_uv"].rearrange("(kc p) c -> p kc c", p=128)), (), [WUVb])
        with Stage(X) as s2:
            WQ = s2.sb([128, 4, 1536], BF16)
            DG(lambda E: E.dma_start(out=WQ[:], in_=I["mla_w_uq"].rearrange("(kc p) c -> p kc c", p=128)), (), [WQ])
            WQS = s2.sb([128, 4, 16, 32], BF16)
            wq4 = I["mla_w_uq"].rearrange("(kc p) (h c) -> p kc h c", p=128, c=96)
            for kc in range(4):
                DG(lambda E: E.dma_start(out=WQS[:, kc, :, 0:16], in_=wq4[:, kc, :, 80:96]), (), [WQS])
                DG(lambda E: E.dma_start(out=WQS[:, kc, :, 16:32], in_=wq4[:, kc, :, 64:80]), (), [WQS])
            WUKb = s2.sb([128, 2, 1024], BF16)
            DG(lambda E: E.dma_start(out=WUKb[:], in_=I["mla_w_uk"].rearrange("(kc p) c -> p kc c", p=128)), (), [WUKb])
            WUKT = s2.sb([64, 16, 2, 128], BF16)
            psb = RR([s2.ps([128, 1024], BF16) for _ in range(1)])
            psq = RR([s2.ps([128, 512]) for _ in range(2)])
            pss_ = RR([s2.ps([128, 512]) for _ in range(2)])
            pso = RR([s2.ps([128, 512]) for _ in range(2)])
            for h in range(16):
                ps = psb.next()
                for kc in range(2):
                    P(lambda E: E.transpose(out=ps[0:64, kc * 128:(kc + 1) * 128], in_=WUKb[:, kc, h * 64:(h + 1) * 64], identity=identb[:, :]),
                      [WUKb, identb], [ps])
                A(lambda E: E.copy(out=WUKT[:, h, :, :], in_=ps[0:64, 0:256].rearrange("p (a c) -> p a c", a=2)), [ps], [WUKT])
            Qp = RR([s2.sb([96, NT], BF16) for _ in range(2)])
            Kp = RR([s2.sb([96, NPR], BF16) for _ in range(2)])
            Vp = RR([s2.sb([128, 17, 65], BF16) for _ in range(2)])
            OBp = RR([s2.sb([64, NPR], BF16) for _ in range(2)])
            tmp_r = RR([s2.sb([96, 512]) for _ in range(2)])
            tmp_r2 = RR([s2.sb([96, 512]) for _ in range(2)])
            PTp = RR([s2.sb([128, 4, 128], BF16) for _ in range(3)])
            otp = RR([s2.sb([128, 64], BF16) for _ in range(2)])
            rlp = RR([s2.sb([128, 1]) for _ in range(2)])
            for h in range(16):
                Qh, Kh, Vh, OBh = Qp.next(), Kp.next(), Vp.next(), OBp.next()
                for (t0, tn) in GROUPS:
                    psA, psB = psq.next(), psq.next()
                    for kc in range(4):
                        P(lambda E: E.matmul(psA[0:96, 0:tn], lhsT=WQ[:, kc, h * 96:(h + 1) * 96], rhs=CQ[:, kc, t0:t0 + tn],
                                             start=(kc == 0), stop=(kc == 3)), [WQ, CQ], [psA])
                    for kc in range(4):
                        P(lambda E: E.matmul(psB[0:32, 0:tn], lhsT=WQS[:, kc, h, :], rhs=CQ[:, kc, t0:t0 + tn],
                                             start=(kc == 0), stop=(kc == 3)), [WQS, CQ], [psB])
                    A(lambda E: E.copy(out=Qh[0:64, t0:t0 + tn], in_=psA[0:64, 0:tn]), [psA], [Qh])
                    t1, t2 = tmp_r.next(), tmp_r2.next()
                    V(lambda E: E.tensor_tensor(out=t1[64:96, 0:tn], in0=psB[0:32, 0:tn], in1=ropes[0:32, t0:t0 + tn], op=ALU.mult),
                      [psB, ropes], [t1])
                    V(lambda E: E.tensor_tensor(out=t2[64:96, 0:tn], in0=psA[64:96, 0:tn], in1=ropec[64:96, t0:t0 + tn], op=ALU.mult),
                      [psA, ropec], [t2])
                    G(lambda E: E.tensor_tensor(out=Qh[64:96, t0:t0 + tn], in0=t1[64:96, 0:tn], in1=t2[64:96, 0:tn], op=ALU.add),
                      [t1, t2], [Qh])
                    if t0 < NPR:
                        kn = min(tn, NPR - t0)
                        psK = psq.next()
                        for kc in range(2):
                            P(lambda E: E.matmul(psK[0:64, 0:kn], lhsT=WUKb[:, kc, h * 64:(h + 1) * 64], rhs=CKVb[:, kc, t0:t0 + kn],
                                                 start=(kc == 0), stop=(kc == 1)), [WUKb, CKVb], [psK])
                        A(lambda E: E.copy(out=Kh[0:64, t0:t0 + kn], in_=psK[0:64, 0:kn]), [psK], [Kh])
                G(lambda E: E.tensor_copy(out=Kh[64:96, :], in_=KRb[64:96, 0:NPR]), [KRb], [Kh])
                G(lambda E: E.memset(Vh[:, :, 64:65], 1.0), (), [Vh])
                for ti in range(17):
                    g0, n = TT[ti]
                    psV = psq.next()
                    for kc in range(2):
                        P(lambda E: E.matmul(psV[0:n, 0:64], lhsT=CKVb[:, kc, g0:g0 + n], rhs=WUVb[:, kc, h * 64:(h + 1) * 64],
                                             start=(kc == 0), stop=(kc == 1)), [CKVb, WUVb], [psV])
                    A(lambda E: E.copy(out=Vh[0:n, ti, 0:64], in_=psV[0:n, 0:64]), [psV], [Vh])
                for kc in range(2):
                    psL = psq.next()
                    P(lambda E: E.matmul(psL[:, 0:128], lhsT=WUKT[0:64, h, kc, :], rhs=Qh[0:64, NPR:NT], start=True, stop=True), [WUKT, Qh], [psL])
                    A(lambda E: E.copy(out=QLAT[:, kc, :, h, :], in_=psL[:, 0:128].rearrange("p (b s) -> p b s", s=8)), [psL], [QLAT])
                G(lambda E: E.tensor_copy(out=QR[64:96, :, h, :], in_=Qh[64:96, NPR:NT].rearrange("p (b s) -> p b s", s=8)), [Qh], [QR])
                for qi in range(17):
                    q0, nq = TT[qi]
                    po = pso.next()
                    klist = list(range(qi + 1))
                    groups = [[0]] + [klist[1:][i:i + 4] for i in range(0, len(klist) - 1, 4)]
                    nk_total = len(klist)
                    done = 0
                    for grp in groups:
                        psS = pss_.next()
                        PT = PTp.next()
                        nk = TT[grp[0]][1]
                        for a, kt in enumerate(grp):
                            k0, _ = TT[kt]
                            P(lambda E: E.matmul(psS[0:nk, a * 128:a * 128 + nq], lhsT=Kh[:, k0:k0 + nk], rhs=Qh[:, q0:q0 + nq],
                                                 start=True, stop=True), [Kh, Qh], [psS])
                        na = len(grp)
                        A(lambda E: E.activation(out=PT[0:nk, 0:na, 0:nq], in_=psS[0:nk, 0:na * 128].rearrange("p (a q) -> p a q", a=na)[:, :, 0:nq],
                                                 func=AF.Exp, scale=SCALE), [psS], [PT])
                        if grp[-1] == qi:
                            a = na - 1
                            G(lambda E: E.tensor_tensor(out=PT[0:nk, a, 0:nq], in0=PT[0:nk, a, 0:nq], in1=mleb[0:nk, 0:nq], op=ALU.mult),
                              [PT, mleb], [PT])
                        for a, kt in enumerate(grp):
                            P(lambda E: E.matmul(po[0:nq, 0:65], lhsT=PT[0:nk, a, 0:nq], rhs=Vh[0:nk, kt, :], start=(done == 0),
                                                 stop=(done == nk_total - 1)), [PT, Vh], [po])
                            done += 1
                    rl = rlp.next()
                    V(lambda E: E.reciprocal(out=rl[0:nq, :], in_=po[0:nq, 64:65]), [po], [rl])
                    ot = otp.next()
                    V(lambda E: E.tensor_scalar(out=ot[0:nq, :], in0=po[0:nq, 0:64], scalar1=rl[0:nq, 0:1], scalar2=None, op0=ALU.mult),
                      [po, rl], [ot])
                    pt_ = psb.next()
                    P(lambda E: E.transpose(out=pt_[0:64, 0:nq], in_=ot[0:nq, :], identity=identb[0:nq, 0:nq]), [ot, identb], [pt_])
                    A(lambda E: E.copy(out=OBh[:, q0:q0 + nq], in_=pt_[0:64, 0:nq]), [pt_], [OBh])
                DS(lambda E: E.dma_start(out=X.OBT[h // 2, (h % 2) * 64:(h % 2) * 64 + 64, 0:NPR], in_=OBh[:, :]), [OBh], [X.OBT])
        with Stage(X) as s3:
            ptb = s3.sb([128, 16], I32)
            DS(lambda E: E.dma_start(out=ptb[:], in_=I["page_table"].rearrange("b j -> j b")), (), [ptb])
            idx = s3.sb([128, 16, 16], I32)
            for g in range(16):
                V(lambda E: E.tensor_scalar(out=idx[:, :, g], in0=ptb[:], scalar1=16, scalar2=g, op0=ALU.mult, op1=ALU.add), [ptb], [idx])
            ckv_v = I["cache_ckv"].rearrange("n (g r) c -> (n g) (r c)", g=16)
            kr_v = I["cache_krope"].rearrange("n (g r) c -> (n g) (r c)", g=16)
            Gk = RR([s3.sb([128, 8, 256]) for _ in range(3)])
            Gr = RR([s3.sb([128, 8, 32]) for _ in range(3)])
            KCb = RR([s3.sb([128, 8, 257], BF16) for _ in range(2)])
            KRc = RR([s3.sb([128, 8, 96], BF16) for _ in range(2)])
            KCh = {}
            for t in KCb.items:
                G(lambda E: E.memset(t[:, :, 256:257], 1.0), (), [t])
                KCh[id(t)] = (T(t.t), T(t.t))
            for t in KRc.items:
                G(lambda E: E.memset(t[:, :, 0:64], 0.0), (), [t])
            KT = RR([s3.sb([128, 2, 4, 128], BF16) for _ in range(2)])
            KTR = RR([s3.sb([96, 4, 128], BF16) for _ in range(2)])
            PTs = RR([s3.sb([128, 4, 128], BF16) for _ in range(2)])
            PN = RR([s3.sb([8, 128], BF16) for _ in range(2)])
            OL = RR([s3.sb([128, 256], BF16) for _ in range(2)])
            rlp = RR([s3.sb([128, 1]) for _ in range(2)])
            OLT = s3.sb([128, 2, 16, 16, 8], BF16)
            psA = RR([s3.ps([128, 512]) for _ in range(2)])
            psS = RR([s3.ps([128, 512]) for _ in range(2)])
            psT = RR([s3.ps([128, 2, 4, 128], BF16) for _ in range(1)])
            psR = RR([s3.ps([128, 4, 128], BF16) for _ in range(2)])
            for b in range(16):
                pa = psA.next()
                first = True
                for g in range(16):
                    gk, gr = Gk.next(), Gr.next()
                    DG(lambda E: E.indirect_dma_start(out=gk[:].rearrange("p r c -> p (r c)"), out_offset=None, in_=ckv_v,
                                                      in_offset=bass.IndirectOffsetOnAxis(ap=idx[:, b, g:g + 1], axis=0)), [idx], [gk])
                    DG(lambda E: E.indirect_dma_start(out=gr[:].rearrange("p r c -> p (r c)"), out_offset=None, in_=kr_v,
                                                      in_offset=bass.IndirectOffsetOnAxis(ap=idx[:, b, g:g + 1], axis=0)), [idx], [gr])
                    kcb, krc = KCb.next(), KRc.next()
                    kh = KCh[id(kcb)]
                    G(lambda E: E.tensor_copy(out=kcb[:, 0:4, 0:256], in_=gk[:, 0:4, :]), [gk, kcb], [kh[0]])
                    A(lambda E: E.copy(out=kcb[:, 4:8, 0:256], in_=gk[:, 4:8, :]), [gk, kcb], [kh[1]])
                    V(lambda E: E.tensor_copy(out=krc[:, :, 64:96], in_=gr[:]), [gr], [krc])
                    for r4 in range(2):
                        pT, pR = psT.next(), psR.next()
                        for rr in range(4):
                            r = r4 * 4 + rr
                            for kc in range(2):
                                P(lambda E: E.transpose(out=pT[:, kc, rr, :], in_=kcb[:, r, kc * 128:(kc + 1) * 128], identity=identb[:, :]),
                                  [kh[r4], identb], [pT])
                            P(lambda E: E.transpose(out=pR[0:96, rr, :], in_=krc[:, r, :], identity=identb[:, :]), [krc, identb], [pR])
                        kt, ktr = KT.next(), KTR.next()
                        A(lambda E: E.copy(out=kt[:, 0], in_=pT[:, 0]), [pT], [kt])
                        V(lambda E: E.tensor_copy(out=kt[:, 1], in_=pT[:, 1]), [pT], [kt])
                        V(lambda E: E.tensor_copy(out=ktr[64:96], in_=pR[64:96]), [pR], [ktr])
                        pS = psS.next()
                        for rr in range(4):
                            for kc in range(2):
                                P(lambda E: E.matmul(pS[:, rr * 128:(rr + 1) * 128], lhsT=kt[:, kc, rr, :],
                                                     rhs=QLAT[:, kc, b].rearrange("p h s -> p (h s)"), start=(kc == 0), stop=False),
                                  [kt, QLAT], [pS])
                            P(lambda E: E.matmul(pS[:, rr * 128:(rr + 1) * 128], lhsT=ktr[64:96, rr, :],
                                                 rhs=QR[64:96, b].rearrange("p h s -> p (h s)"), start=False, stop=True), [ktr, QR], [pS])
                        pts = PTs.next()
                        A(lambda E: E.activation(out=pts[:].rearrange("p a q -> p (a q)"), in_=pS[:, :], func=AF.Exp, scale=SCALE), [pS], [pts])
                        for rr in range(4):
                            r = r4 * 4 + rr
                            P(lambda E: E.matmul(pa[:, 0:257], lhsT=pts[:, rr, :], rhs=kcb[:, r, :], start=first, stop=False), [pts, kh[r4]], [pa])
                            first = False
                pS = psS.next()
                c0 = NPR + 8 * b
                for kc in range(2):
                    P(lambda E: E.matmul(pS[0:8, 0:128], lhsT=CKVb[:, kc, c0:c0 + 8], rhs=QLAT[:, kc, b].rearrange("p h s -> p (h s)"),
                                         start=(kc == 0), stop=False), [CKVb, QLAT], [pS])
                P(lambda E: E.matmul(pS[0:8, 0:128], lhsT=KRb[64:96, c0:c0 + 8], rhs=QR[64:96, b].rearrange("p h s -> p (h s)"),
                                     start=False, stop=True), [KRb, QR], [pS])
                pn = PN.next()
                A(lambda E: E.activation(out=pn[:], in_=pS[0:8, 0:128], func=AF.Exp, scale=SCALE), [pS], [pn])
                V(lambda E: E.tensor_tensor(out=pn[:], in0=pn[:], in1=newmask[:], op=ALU.mult), [pn, newmask], [pn])
                P(lambda E: E.matmul(pa[:, 0:257], lhsT=pn[:], rhs=KCN[:, b, :], start=False, stop=True), [pn, KCN], [pa])
                rl = rlp.next()
                V(lambda E: E.reciprocal(out=rl[:], in_=pa[:, 256:257]), [pa], [rl])
                ol = OL.next()
                V(lambda E: E.tensor_scalar(out=ol[:], in0=pa[:, 0:256], scalar1=rl[:, 0:1], scalar2=None, op0=ALU.mult), [pa, rl], [ol])
                pR = psR.next()
                for kc in range(2):
                    P(lambda E: E.transpose(out=pR[:, kc, :], in_=ol[:, kc * 128:(kc + 1) * 128], identity=identb[:, :]), [ol, identb], [pR])
                A(lambda E: E.copy(out=OLT[:, :, b].rearrange("p k h s -> p k (h s)"), in_=pR[:, 0:2, :]), [pR], [OLT])
            obs = RR([s3.sb([64, 128], BF16) for _ in range(2)])
            for h in range(16):
                pS = psS.next()
                for kc in range(2):
                    P(lambda E: E.matmul(pS[0:64, 0:128], lhsT=WUVb[:, kc, h * 64:(h + 1) * 64], rhs=OLT[:, kc, :, h, :],
                                         start=(kc == 0), stop=(kc == 1)), [WUVb, OLT], [pS])
                ob = obs.next()
                A(lambda E: E.copy(out=ob[:], in_=pS[0:64, 0:128]), [pS], [ob])
                DS(lambda E: E.dma_start(out=X.OBT[h // 2, (h % 2) * 64:(h % 2) * 64 + 64, NPR:NT], in_=ob[:]), [ob], [X.OBT])


EGROUPS = [(16, [1, 2, 3, 4]), (528, [5, 6, 7, 8]), (1040, [9, 10, 11, 12]), (1552, [13, 14, 15, 16]), (2064, [17])]


def x_rows(X, ti):
    return X.I["xp"][(ti - 1) * 128: ti * 128, :] if ti <= 16 else X.I["xs"]


def stage_e(X):
    I, O, V, A, G, P, DS, DG = X.I, X.O, X.V, X.A, X.G, X.P, X.DS, X.DG
    with Stage(X) as st:
        WUA = st.sb([128, 8, D], BF16)
        WUB = st.sb([128, 8, D], BF16)
        for kc in range(8):
            DG(lambda E: E.dma_start(out=WUA[:, kc, :], in_=I["w_up_a"][kc * 128:(kc + 1) * 128, :]), (), [WUA])
            DG(lambda E: E.dma_start(out=WUB[:, kc, :], in_=I["w_up_b"][kc * 128:(kc + 1) * 128, :]), (), [WUB])
        OAg = RR([st.sb([128, 8, 512], BF16) for _ in range(2)])
        OBg = RR([st.sb([128, 8, 512], BF16) for _ in range(2)])
        MTg = RR([st.sb([128, 16, 512], BF16) for _ in range(2)])
        gat = RR([st.sb([128, 2, 512]) for _ in range(3)])
        mtmp = RR([st.sb([128, 2, 512]) for _ in range(2)])
        psm = RR([st.ps([128, 512]) for _ in range(4)])
        for (t0, tiles) in EGROUPS:
            tn = 128 * len(tiles)
            oa, ob, mt = OAg.next(), OBg.next(), MTg.next()
            DS(lambda E: E.dma_start(out=oa[:, :, 0:tn], in_=X.OAT[:, :, t0:t0 + tn].rearrange("j p t -> p j t")), [X.OAT], [oa])
            DS(lambda E: E.dma_start(out=ob[:, :, 0:tn], in_=X.OBT[:, :, t0:t0 + tn].rearrange("j p t -> p j t")), [X.OBT], [ob])
            for dt in range(16):
                gt = gat.next()
                DS(lambda E: E.dma_start(out=gt[:, 0, 0:tn], in_=X.PFM[CTI[f"ga{dt}"], :, t0:t0 + tn]), [X.PFM], [gt])
                DS(lambda E: E.dma_start(out=gt[:, 1, 0:tn], in_=X.PFM[CTI[f"gb{dt}"], :, t0:t0 + tn]), [X.PFM], [gt])
                pa, pb = psm.next(), psm.next()
                for kc in range(8):
                    P(lambda E: E.matmul(pa[:, 0:tn], lhsT=WUA[:, kc, dt * 128:(dt + 1) * 128], rhs=oa[:, kc, 0:tn], start=(kc == 0), stop=(kc == 7)),
                      [WUA, oa], [pa])
                for kc in range(8):
                    P(lambda E: E.matmul(pb[:, 0:tn], lhsT=WUB[:, kc, dt * 128:(dt + 1) * 128], rhs=ob[:, kc, 0:tn], start=(kc == 0), stop=(kc == 7)),
                      [WUB, ob], [pb])
                m_ = mtmp.next()
                V(lambda E: E.scalar_tensor_tensor(out=m_[:, 0, 0:tn], in0=gt[:, 0, 0:tn], scalar=1.0, in1=pa[:, 0:tn], op0=ALU.add, op1=ALU.mult),
                  [gt, pa], [m_])
                V(lambda E: E.scalar_tensor_tensor(out=m_[:, 1, 0:tn], in0=gt[:, 1, 0:tn], scalar=1.0, in1=pb[:, 0:tn], op0=ALU.add, op1=ALU.mult),
                  [gt, pb], [m_])
                G(lambda E: E.tensor_tensor(out=mt[:, dt, 0:tn], in0=m_[:, 0, 0:tn], in1=m_[:, 1, 0:tn], op=ALU.add), [m_], [mt])
            DS(lambda E: E.dma_start(out=X.MTD[:, :, t0:t0 + tn], in_=mt[:, :, 0:tn]), [mt], [X.MTD])
    with Stage(X) as st:
        identf = st.load(I["ident_f"], [128, 128])
        WO = st.sb([128, 16, D], BF16)
        for kc in range(16):
            DG(lambda E: E.dma_start(out=WO[:, kc, :], in_=I["w_o"][kc * 128:(kc + 1) * 128, :]), (), [WO])
        RWt = st.load(I["router_w"].rearrange("(kc p) c -> p kc c", p=128), [128, 16, 36])
        RB = st.load(I["router_b"].to_broadcast([128, 36]), [128, 36])
        g2 = st.load(I["norm_ffn_g"].to_broadcast([128, D]), [128, D])
        MTt = RR([st.sb([128, 16, 128], BF16) for _ in range(2)])
        xin = RR([st.sb([128, D]) for _ in range(2)])
        x2p = RR([st.sb([128, D]) for _ in range(2)])
        xn = RR([st.sb([128, D]) for _ in range(2)])
        junk = st.sb([128, D])
        XT32 = RR([st.sb([128, 16, 128]) for _ in range(2)])
        XTb = RR([st.sb([128, 16, 128], BF16) for _ in range(2)])
        sm = RR([st.sb([128, 64]) for _ in range(24)])
        cmb = RR([st.sb([128, 32]) for _ in range(2)])
        psm = RR([st.ps([128, 512]) for _ in range(4)])
        pst = RR([st.ps([128, 512]) for _ in range(4)])
        for (t0, tiles) in EGROUPS:
            for li, ti in enumerate(tiles):
                g0, n = TT[ti]
                mt = MTt.next()
                DS(lambda E: E.dma_start(out=mt[:], in_=X.MTD[:, :, g0:g0 + n]), [X.MTD], [mt])
                li = 0
                x = xin.next()
                DS(lambda E: E.dma_start(out=x[:], in_=x_rows(X, ti)), (), [x])
                x2 = x2p.next()
                for cc in range(4):
                    ps = psm.next()
                    for kc in range(16):
                        P(lambda E: E.matmul(ps[:, :], lhsT=mt[:, kc, li * 128:(li + 1) * 128], rhs=WO[:, kc, cc * 512:(cc + 1) * 512],
                                             start=(kc == 0), stop=(kc == 15)), [mt, WO], [ps])
                    V(lambda E: E.scalar_tensor_tensor(out=x2[:, cc * 512:(cc + 1) * 512], in0=ps[:, :], scalar=0.5,
                                                       in1=x[:, cc * 512:(cc + 1) * 512], op0=ALU.mult, op1=ALU.add), [ps, x], [x2])
                DS(lambda E: E.dma_start(out=X.X2[g0:g0 + n, :], in_=x2[:]), [x2], [X.X2])
                sq = sm.next()
                G(lambda E: E.tensor_tensor(out=junk[:], in0=x2[:], in1=x2[:], op=ALU.mult), [x2], [junk])
                V(lambda E: E.tensor_reduce(out=sq[:, 0:1], in_=junk[:], axis=AX.X, op=ALU.add), [junk], [sq])
                A(lambda E: E.activation(out=sq[:, 0:1], in_=sq[:, 0:1], func=AF.Sqrt, bias=1e-6, scale=1.0 / D), [sq], [sq])
                V(lambda E: E.reciprocal(out=sq[:, 0:1], in_=sq[:, 0:1]), [sq], [sq])
                xn2 = xn.next()
                V(lambda E: E.scalar_tensor_tensor(out=xn2[:], in0=x2[:], scalar=sq[:, 0:1], in1=g2[:], op0=ALU.mult, op1=ALU.mult),
                  [x2, sq, g2], [xn2])
                xt32, xtb = XT32.next(), XTb.next()
                for q4 in range(4):
                    ps = pst.next()
                    for a in range(4):
                        kc = q4 * 4 + a
                        P(lambda E: E.transpose(out=ps[:, a * 128:(a + 1) * 128], in_=xn2[:, kc * 128:(kc + 1) * 128], identity=identf[:, :]),
                          [xn2, identf], [ps])
                    A(lambda E: E.copy(out=xt32[:, q4 * 4:q4 * 4 + 4, :], in_=ps[:, :].rearrange("p (a t) -> p a t", a=4)), [ps], [xt32])
                G(lambda E: E.tensor_copy(out=xtb[:], in_=xt32[:]), [xt32], [xtb])
                DS(lambda E: E.dma_start(out=X.XN2T.t.rearrange("(kc p) t -> p kc t", p=128)[:, :, g0:g0 + n], in_=xtb[:]), [xtb], [X.XN2T])
                ps = psm.next()
                for kc in range(16):
                    P(lambda E: E.matmul(ps[:, 0:36], lhsT=xt32[:, kc, :], rhs=RWt[:, kc, :], start=(kc == 0), stop=(kc == 15)), [xt32, RWt], [ps])
                lg = sm.next()
                A(lambda E: E.copy(out=lg[:, 0:36], in_=ps[:, 0:36]), [ps], [lg])
                gl = lg[:, 0:4]
                el = lg[:, 4:36].rearrange("p (g e) -> p g e", g=4)
                mx, e4, s4, pr = sm.next(), sm.next(), sm.next(), sm.next()
                V(lambda E: E.tensor_reduce(out=mx[:, 0:1], in_=gl, axis=AX.X, op=ALU.max), [lg], [mx])
                V(lambda E: E.tensor_scalar(out=e4[:, 0:4], in0=gl, scalar1=mx[:, 0:1], scalar2=None, op0=ALU.subtract), [lg, mx], [e4])
                A(lambda E: E.activation(out=e4[:, 0:4], in_=e4[:, 0:4], func=AF.Exp), [e4], [e4])
                V(lambda E: E.tensor_reduce(out=s4[:, 0:1], in_=e4[:, 0:4], axis=AX.X, op=ALU.add), [e4], [s4])
                V(lambda E: E.reciprocal(out=s4[:, 0:1], in_=s4[:, 0:1]), [s4], [s4])
                V(lambda E: E.tensor_scalar(out=pr[:, 0:4], in0=e4[:, 0:4], scalar1=s4[:, 0:1], scalar2=None, op0=ALU.mult), [e4, s4], [pr])
                glb, mxb, oh = sm.next(), sm.next(), sm.next()
                V(lambda E: E.tensor_tensor(out=glb[:, 0:4], in0=gl, in1=RB[:, 0:4], op=ALU.add), [lg, RB], [glb])
                V(lambda E: E.tensor_reduce(out=mxb[:, 0:1], in_=glb[:, 0:4], axis=AX.X, op=ALU.max), [glb], [mxb])
                V(lambda E: E.tensor_scalar(out=oh[:, 0:4], in0=glb[:, 0:4], scalar1=mxb[:, 0:1], scalar2=None, op0=ALU.is_ge), [glb, mxb], [oh])
                pg, t4 = sm.next(), sm.next()
                V(lambda E: E.tensor_tensor(out=t4[:, 0:4], in0=pr[:, 0:4], in1=oh[:, 0:4], op=ALU.mult), [pr, oh], [t4])
                V(lambda E: E.tensor_reduce(out=pg[:, 0:1], in_=t4[:, 0:4], axis=AX.X, op=ALU.add), [t4], [pg])
                ohb = oh[:, 0:4].unsqueeze(2).to_broadcast([128, 4, 8])
                t32, ein, eb = sm.next(), sm.next(), sm.next()
                V(lambda E: E.tensor_tensor(out=t32[:, 0:32].rearrange("p (g e) -> p g e", g=4), in0=el, in1=ohb, op=ALU.mult), [lg, oh], [t32])
                V(lambda E: E.tensor_reduce(out=ein[:, 0:8], in_=t32[:, 0:32].rearrange("p (g e) -> p e g", g=4), axis=AX.X, op=ALU.add), [t32], [ein])
                t32b = sm.next()
                V(lambda E: E.tensor_tensor(out=t32b[:, 0:32].rearrange("p (g e) -> p g e", g=4),
                                            in0=RB[:, 4:36].rearrange("p (g e) -> p g e", g=4), in1=ohb, op=ALU.mult), [RB, oh], [t32b])
                V(lambda E: E.tensor_reduce(out=eb[:, 0:8], in_=t32b[:, 0:32].rearrange("p (g e) -> p e g", g=4), axis=AX.X, op=ALU.add), [t32b], [eb])
                V(lambda E: E.tensor_tensor(out=eb[:, 0:8], in0=eb[:, 0:8], in1=ein[:, 0:8], op=ALU.add), [eb, ein], [eb])
                m1, oh1, eb2, m2, oh2 = sm.next(), sm.next(), sm.next(), sm.next(), sm.next()
                V(lambda E: E.tensor_reduce(out=m1[:, 0:1], in_=eb[:, 0:8], axis=AX.X, op=ALU.max), [eb], [m1])
                V(lambda E: E.tensor_scalar(out=oh1[:, 0:8], in0=eb[:, 0:8], scalar1=m1[:, 0:1], scalar2=None, op0=ALU.is_ge), [eb, m1], [oh1])
                V(lambda E: E.scalar_tensor_tensor(out=eb2[:, 0:8], in0=oh1[:, 0:8], scalar=-1e30, in1=eb[:, 0:8], op0=ALU.mult, op1=ALU.add),
                  [oh1, eb], [eb2])
                V(lambda E: E.tensor_reduce(out=m2[:, 0:1], in_=eb2[:, 0:8], axis=AX.X, op=ALU.max), [eb2], [m2])
                V(lambda E: E.tensor_scalar(out=oh2[:, 0:8], in0=eb2[:, 0:8], scalar1=m2[:, 0:1], scalar2=None, op0=ALU.is_ge), [eb2, m2], [oh2])
                v1, v2, tt8 = sm.next(), sm.next(), sm.next()
                V(lambda E: E.tensor_tensor(out=tt8[:, 0:8], in0=ein[:, 0:8], in1=oh1[:, 0:8], op=ALU.mult), [ein, oh1], [tt8])
                V(lambda E: E.tensor_reduce(out=v1[:, 0:1], in_=tt8[:, 0:8], axis=AX.X, op=ALU.add), [tt8], [v1])
                V(lambda E: E.tensor_tensor(out=tt8[:, 8:16], in0=ein[:, 0:8], in1=oh2[:, 0:8], op=ALU.mult), [ein, oh2], [tt8])
                V(lambda E: E.tensor_reduce(out=v2[:, 0:1], in_=tt8[:, 8:16], axis=AX.X, op=ALU.add), [tt8], [v2])
                V(lambda E: E.tensor_tensor(out=v2[:, 0:1], in0=v2[:, 0:1], in1=v1[:, 0:1], op=ALU.subtract), [v2, v1], [v2])
                A(lambda E: E.activation(out=v2[:, 0:1], in_=v2[:, 0:1], func=AF.Exp), [v2], [v2])
                V(lambda E: E.tensor_scalar(out=v1[:, 0:1], in0=v2[:, 0:1], scalar1=1.0, scalar2=None, op0=ALU.add), [v2], [v1])
                V(lambda E: E.reciprocal(out=v1[:, 0:1], in_=v1[:, 0:1]), [v1], [v1])
                V(lambda E: E.tensor_tensor(out=v1[:, 0:1], in0=v1[:, 0:1], in1=pg[:, 0:1], op=ALU.mult), [v1, pg], [v1])
                V(lambda E: E.tensor_tensor(out=v2[:, 0:1], in0=v2[:, 0:1], in1=v1[:, 0:1], op=ALU.mult), [v2, v1], [v2])
                c8 = sm.next()
                V(lambda E: E.tensor_scalar(out=c8[:, 0:8], in0=oh1[:, 0:8], scalar1=v1[:, 0:1], scalar2=None, op0=ALU.mult), [oh1, v1], [c8])
                V(lambda E: E.scalar_tensor_tensor(out=c8[:, 0:8], in0=oh2[:, 0:8], scalar=v2[:, 0:1], in1=c8[:, 0:8], op0=ALU.mult, op1=ALU.add),
                  [oh2, v2, c8], [c8])
                cb = cmb.next()
                V(lambda E: E.tensor_tensor(out=cb[:, :].rearrange("p (g e) -> p g e", g=4), in0=c8[:, 0:8].unsqueeze(1).to_broadcast([128, 4, 8]),
                                            in1=ohb, op=ALU.mult), [c8, oh], [cb])
                DS(lambda E: E.dma_start(out=X.COMB[g0:g0 + n, :], in_=cb[:]), [cb], [X.COMB])


FGROUPS = [[1, 2, 3, 4, 5, 6], [7, 8, 9, 10, 11, 12], [13, 14, 15, 16, 17]]


def stage_f(X):
    I, O, V, A, G, P, DS, DG = X.I, X.O, X.V, X.A, X.G, X.P, X.DS, X.DG
    with Stage(X) as st:
        gf = st.load(I["norm_final_g"].to_broadcast([128, D]), [128, D])
        XTg = st.sb([128, 16, 768], BF16)
        YACC = [st.sb([128, D]) for _ in range(6)]
        CMB = st.sb([128, 6, 32])
        Wg = RR([st.sb([128, 16, 512], BF16) for _ in range(2)])
        Wu = RR([st.sb([128, 16, 512], BF16) for _ in range(2)])
        Wd = RR([st.sb([128, 4, D], BF16) for _ in range(2)])
        act = RR([st.sb([128, 4, 768], BF16) for _ in range(2)])
        sgp = RR([st.sb([128, 384]) for _ in range(3)])
        small = RR([st.sb([128, 1]) for _ in range(2)])
        x2f = RR([st.sb([128, D]) for _ in range(1)])
        psg = RR([st.ps([128, 512]) for _ in range(4)])
        psd = RR([st.ps([128, 512]) for _ in range(4)])
        for tiles in FGROUPS:
            t0 = TT[tiles[0]][0]
            nt_ = len(tiles)
            tn = 128 * nt_
            hn = tn // 2
            DS(lambda E: E.dma_start(out=XTg[:, :, 0:tn], in_=X.XN2T.t.rearrange("(p k) t -> p k t", k=16)[:, :, t0:t0 + tn]), [X.XN2T], [XTg])
            for li in range(nt_):
                DS(lambda E: E.dma_start(out=CMB[:, li, :], in_=X.COMB[t0 + li * 128:t0 + (li + 1) * 128, :]), [X.COMB], [CMB])
            for e in range(32):
                wg, wu, wd = Wg.next(), Wu.next(), Wd.next()
                DG(lambda E: E.dma_start(out=wg[:], in_=I["expert_w_gate"][e].rearrange("(p k) f -> p k f", k=16)), (), [wg])
                DG(lambda E: E.dma_start(out=wu[:], in_=I["expert_w_up"][e].rearrange("(p k) f -> p k f", k=16)), (), [wu])
                DG(lambda E: E.dma_start(out=wd[:], in_=I["expert_w_down"][e].rearrange("(p k) d -> p k d", k=4)), (), [wd])
                ac = act.next()
                for half in range(2):
                    hs = slice(half * hn, (half + 1) * hn)
                    for ft in range(4):
                        pg_, pu_ = psg.next(), psg.next()
                        for kc in range(16):
                            P(lambda E: E.matmul(pg_[:, 0:hn], lhsT=wg[:, kc, ft:512:4], rhs=XTg[:, kc, hs], start=(kc == 0), stop=(kc == 15)),
                              [wg, XTg], [pg_])
                        for kc in range(16):
                            P(lambda E: E.matmul(pu_[:, 0:hn], lhsT=wu[:, kc, ft:512:4], rhs=XTg[:, kc, hs], start=(kc == 0), stop=(kc == 15)),
                              [wu, XTg], [pu_])
                        sg = sgp.next()
                        A(lambda E: E.activation(out=sg[:, 0:hn], in_=pg_[:, 0:hn], func=AF.Silu), [pg_], [sg])
                        V(lambda E: E.tensor_tensor(out=ac[:, ft, hs], in0=sg[:, 0:hn], in1=pu_[:, 0:hn], op=ALU.mult), [sg, pu_], [ac])
                for li in range(nt_):
                    for cc in range(4):
                        pd = psd.next()
                        for ft in range(4):
                            P(lambda E: E.matmul(pd[:, :], lhsT=ac[:, ft, li * 128:(li + 1) * 128], rhs=wd[:, ft, cc * 512:(cc + 1) * 512],
                                                 start=(ft == 0), stop=(ft == 3)), [ac, wd], [pd])
                        ya = YACC[li]
                        if e == 0:
                            V(lambda E: E.tensor_scalar(out=ya[:, cc * 512:(cc + 1) * 512], in0=pd[:, :], scalar1=CMB[:, li, e:e + 1], scalar2=None,
                                                        op0=ALU.mult), [pd, CMB], [ya])
                        else:
                            V(lambda E: E.scalar_tensor_tensor(out=ya[:, cc * 512:(cc + 1) * 512], in0=pd[:, :], scalar=CMB[:, li, e:e + 1],
                                                               in1=ya[:, cc * 512:(cc + 1) * 512], op0=ALU.mult, op1=ALU.add), [pd, CMB, ya], [ya])
            for li, ti in enumerate(tiles):
                g0, n = TT[ti]
                ya = YACC[li]
                x2t = x2f.next()
                DS(lambda E: E.dma_start(out=x2t[:], in_=X.X2[g0:g0 + n, :]), [X.X2], [x2t])
                G(lambda E: E.tensor_tensor(out=ya[:], in0=ya[:], in1=x2t[:], op=ALU.add), [ya, x2t], [ya])
                G(lambda E: E.tensor_tensor(out=x2t[:], in0=ya[:], in1=ya[:], op=ALU.mult), [ya], [x2t])
                sq = small.next()
                V(lambda E: E.tensor_reduce(out=sq[:, 0:1], in_=x2t[:], axis=AX.X, op=ALU.add), [x2t], [sq])
                A(lambda E: E.activation(out=sq[:, 0:1], in_=sq[:, 0:1], func=AF.Sqrt, bias=1e-6, scale=1.0 / D), [sq], [sq])
                V(lambda E: E.reciprocal(out=sq[:, 0:1], in_=sq[:, 0:1]), [sq], [sq])
                V(lambda E: E.scalar_tensor_tensor(out=x2t[:], in0=ya[:], scalar=sq[:, 0:1], in1=gf[:], op0=ALU.mult, op1=ALU.mult),
                  [ya, sq, gf], [x2t])
                dst = O["y_prompt"][(ti - 1) * 128: ti * 128, :] if ti <= 16 else O["y_sample"]
                DS(lambda E: E.dma_start(out=dst, in_=x2t[:]), [x2t], [])


def shard_inputs(inputs, n_cores=8, n_pool=None):
    f = lambda a: np.ascontiguousarray(np.asarray(a))
    g = {k: np.asarray(v) for k, v in inputs.items()}
    col = lambda a: f(a.reshape(-1, 1))
    shared = {
        "meta": f(g["meta_tokens"]),
        "norm_mix_g": f(g["norm_mix_g"].reshape(1, D)), "w_in": f(g["w_in"][0]),
        "rwkv_mu": col(g["rwkv_mu"][0]), "rwkv_w0": col(g["rwkv_w0"][0]), "rwkv_w2": f(g["rwkv_w2"][0]),
        "rwkv_a0": col(g["rwkv_a0"][0]), "rwkv_a2": f(g["rwkv_a2"][0]), "rwkv_g2": f(g["rwkv_g2"][0]),
        "rwkv_k_k": col(g["rwkv_k_k"][0]), "rwkv_k_a": col(g["rwkv_k_a"][0]), "rwkv_r_k": col(g["rwkv_r_k"][0]),
        "rwkv_ln_g": col(g["rwkv_ln_g"][0]), "rwkv_ln_b": col(g["rwkv_ln_b"][0]),
        "mla_q_norm_g": col(g["mla_q_norm_g"][0]), "mla_w_uq": f(g["mla_w_uq"][0]),
        "mla_kv_norm_g": col(g["mla_kv_norm_g"][0]), "mla_w_uk": f(g["mla_w_uk"][0].reshape(256, 1024)),
        "mla_w_uv": f(g["mla_w_uv"][0].reshape(256, 1024)),
        "w_up_a": f(g["w_up_a"][0]), "w_up_b": f(g["w_up_b"][0]), "w_o": f(g["w_o"][0]),
        "norm_ffn_g": f(g["norm_ffn_g"].reshape(1, D)),
        "router_w": f(np.concatenate([g["router_group_w"][0], g["router_expert_w"][0]], axis=1)),
        "router_b": f(np.concatenate([g["router_group_b"][0], g["router_expert_b"][0]]).reshape(1, 36)),
        "expert_w_gate": f(g["expert_w_gate"][0]), "expert_w_up": f(g["expert_w_up"][0]),
        "expert_w_down": f(g["expert_w_down"][0]), "norm_final_g": f(g["norm_final_g"].reshape(1, D)),
        "cache_ckv": f(g["cache_ckv"][0]), "cache_krope": f(g["cache_krope"][0]),
    }
    shared.update(make_consts())
    maps = []
    for c in range(n_cores):
        m = dict(shared)
        m["xp"] = f(g["x_prompt"][c])
        m["xs"] = f(g["x_sample"][16 * c:16 * c + 16].reshape(NS, D))
        m["state_wkv"] = f(g["state_wkv"][0, 16 * c:16 * c + 16])
        m["state_shift"] = f(g["state_shift"][0, 16 * c:16 * c + 16])
        m["page_table"] = f(g["page_table"][16 * c:16 * c + 16]).astype(np.int32)
        maps.append(m)
    return maps


def kernel(**inputs):
    n_pool = int(np.asarray(inputs["cache_ckv"]).shape[1])
    nc, _ = build(n_pool=n_pool)
    maps = shard_inputs(inputs)
    res = run_bass_kernel_spmd(nc, maps, core_ids=list(range(8)))
    R = res.results
    cat = lambda k: np.stack([r[k] for r in R], axis=0)
    y_prompt = cat("y_prompt")
    y_sample = np.concatenate([r["y_sample"].reshape(16, 8, D) for r in R], axis=0)
    ckv_p = cat("ckv_p")[None]
    kr_p = cat("kr_p")[None]
    wkv_p = cat("wkv_p")[None]
    sh_p = np.concatenate([r["sh_p"] for r in R], axis=0)[None]
    ckv_s = np.concatenate([r["ckv_s"].reshape(16, 8, 256) for r in R], axis=0)[None]
    kr_s = np.concatenate([r["kr_s"].reshape(16, 8, 32) for r in R], axis=0)[None]
    wkv_s = np.concatenate([r["wkv_s"] for r in R], axis=0)[None]
    sh_s = np.concatenate([r["sh_s"] for r in R], axis=0)[None]
    return (y_prompt, y_sample, ckv_p, kr_p, wkv_p, sh_p, ckv_s, kr_s, wkv_s, sh_s)
```

```python
import contextlib
import numpy as np
import ml_dtypes
import concourse.bass as bass
import concourse.mybir as mybir
from concourse.bass_utils import run_bass_kernel_spmd

F32 = mybir.dt.float32
BF16 = mybir.dt.bfloat16
F32R = mybir.dt.float32r
I32 = mybir.dt.int32
AF = mybir.ActivationFunctionType
ALU = mybir.AluOpType
AX = mybir.AxisListType

NT = 2192
NPR = 2064
NS = 128
D = 2048
TT = [(0, 16)] + [(16 + 128 * i, 128) for i in range(16)] + [(2064, 128)]
GROUPS = [(0, 512), (512, 512), (1024, 512), (1536, 512), (2048, 144)]
RWKV_COLS = 3520
OFF_RWKV = 800
IN_COLS = 8416
SCALE = 96 ** -0.5


class Buf:
    __slots__ = ("writers", "readers")

    def __init__(self):
        self.writers = {}
        self.readers = {}


class T:
    __slots__ = ("t", "b")

    def __init__(self, t, b=None):
        self.t = t
        self.b = b if b is not None else Buf()

    def __getitem__(self, k):
        return self.t[k]


def _bufs(xs):
    out = []
    for x in xs:
        if x is None:
            continue
        out.append(x.b if isinstance(x, T) else x)
    return out


class _Rec:
    def __getattr__(self, name):
        return lambda *a, **k: (name, a, k)


_REC = _Rec()


def _replay(E, call):
    kw = call[2]
    if call[0] == "dma_start" and "allow_slow_non_contiguous" not in kw:
        kw = dict(kw, allow_slow_non_contiguous=True)
    return getattr(E, call[0])(*call[1], **kw)


class _Op:
    __slots__ = ("eng", "call", "needed", "key", "val", "is_dma")

    def __init__(self, eng, call, is_dma=False):
        self.eng, self.call, self.is_dma = eng, call, is_dma
        self.needed = False
        self.key = None
        self.val = None


class _Wait:
    __slots__ = ("op",)

    def __init__(self, op):
        self.op = op


class Sched:
    ENGS = ("sync", "scalar", "vector", "gpsimd", "tensor")
    EPOCH = 30000

    def __init__(self, nc):
        self.nc = nc
        self.streams = {e: [] for e in self.ENGS}
        self.sems = {}
        self.nsem = 0
        self.ninst = 0
        self.fp32r = False
        self.last = {e: None for e in self.ENGS}
        self.ndma = {e: 0 for e in self.ENGS}
        self.n_dma_sems = 8
        self.dma_slot_last = {}

    def _alloc(self, key):
        h = self.nc.alloc_semaphore(name=f"s{self.nsem}")
        self.nsem += 1
        self.sems[key] = h
        return key

    def _wait(self, eng, op):
        if op.eng == eng and eng == "tensor" and not op.is_dma:
            return
        op.needed = True
        self.streams[eng].append(_Wait(op))

    def _deps(self, eng, reads, writes):
        for b in reads:
            for d in b.writers.values():
                self._wait(eng, d)
        for b in writes:
            for d in b.writers.values():
                self._wait(eng, d)
            for d in b.readers.values():
                self._wait(eng, d)

    def _mark(self, op, tag, reads, writes):
        for b in reads:
            b.readers[tag] = op
        for b in writes:
            b.writers = {tag: op}
            b.readers = {}

    def op(self, eng, fn, reads=(), writes=()):
        reads = _bufs(reads)
        writes = _bufs(writes)
        self._deps(eng, reads, writes)
        call = fn(_REC)
        if self.fp32r and call[0] == "matmul":
            kw = dict(call[2])
            for k in ("lhsT", "rhs"):
                if kw[k].dtype == F32:
                    kw[k] = kw[k].bitcast(F32R)
            call = (call[0], call[1], kw)
        o = _Op(eng, call)
        self.streams[eng].append(o)
        self.last[eng] = o
        self._mark(o, eng, reads, writes)
        self.ninst += 1
        return o

    def dma(self, eng, fn, reads=(), writes=()):
        reads = _bufs(reads)
        writes = _bufs(writes)
        self._deps(eng, reads, writes)
        slot = self.ndma[eng] % self.n_dma_sems
        self.ndma[eng] += 1
        prev = self.dma_slot_last.get((eng, slot))
        if prev is not None:
            self._wait(eng, prev)
        o = _Op(eng, fn(_REC), is_dma=True)
        o.key = f"{eng}_dma{slot}"
        o.needed = True
        self.dma_slot_last[(eng, slot)] = o
        self.streams[eng].append(o)
        self._mark(o, o.key, reads, writes)
        self.ninst += 1
        return o

    def barrier(self):
        deps = [o for o in self.last.values() if o is not None] + list(self.dma_slot_last.values())
        for e in self.ENGS:
            for d in deps:
                if not (d.eng == e and not d.is_dma):
                    d.needed = True
                    self.streams[e].append(_Wait(d))

    def finish(self):
        self.barrier()
        cnt = {}
        for e in self.ENGS:
            key = None
            for it in self.streams[e]:
                if isinstance(it, _Wait):
                    continue
                if it.is_dma:
                    if it.key not in self.sems:
                        self._alloc(it.key)
                    cnt[it.key] = cnt.get(it.key, 0) + 16
                    it.val = cnt[it.key]
                elif it.needed:
                    if key is None or cnt[key] >= self.EPOCH:
                        key = self._alloc(f"{e}_ep{self.nsem}")
                        cnt[key] = 0
                    cnt[key] += 1
                    it.key, it.val = key, cnt[key]
        nc = self.nc
        sems = self.sems

        def emit(E, stream):
            waited = {}
            for it in stream:
                if isinstance(it, _Wait):
                    o = it.op
                    if waited.get(o.key, 0) >= o.val:
                        continue
                    waited[o.key] = o.val
                    E.wait_ge(sems[o.key], o.val)
                elif it.is_dma:
                    _replay(E, it.call).then_inc(sems[it.key], 16)
                elif it.needed:
                    _replay(E, it.call).then_inc(sems[it.key], 1)
                else:
                    _replay(E, it.call)

        st = self.streams
        with nc.Block() as block:
            @block.sync
            def _(E):
                emit(E, st["sync"])

            @block.scalar
            def _(E):
                emit(E, st["scalar"])

            @block.vector
            def _(E):
                emit(E, st["vector"])

            @block.gpsimd
            def _(E):
                emit(E, st["gpsimd"])

            @block.tensor
            def _(E):
                emit(E, st["tensor"])


def make_consts():
    c = {}
    c["ident_f"] = np.eye(128, dtype=np.float32)
    c["ident_b"] = np.eye(128, dtype=np.float32).astype(ml_dtypes.bfloat16)
    half = 16
    inv = (np.float32(10000.0) ** (-np.arange(half, dtype=np.float32) / np.float32(half))).astype(np.float32)
    pos = np.concatenate([np.arange(NPR), 16384 + np.tile(np.arange(8), 16)]).astype(np.float32)
    ang = (pos[:, None] * inv[None, :]).astype(np.float32)
    cos = np.cos(ang).astype(np.float32).T
    sin = np.sin(ang).astype(np.float32).T
    rc = np.zeros((128, NT), np.float32)
    rs = np.zeros((128, NT), np.float32)
    for p in range(128):
        j = p % 16
        rc[p] = cos[j]
        rs[p] = -sin[j] if (p % 32) < 16 else sin[j]
    c["rope_c"] = rc
    c["rope_s"] = rs
    i = np.arange(128)
    le = (i[:, None] <= i[None, :]).astype(np.float32)
    lt = (i[:, None] < i[None, :]).astype(np.float32)
    blk = ((i[:, None] // 8) == (i[None, :] // 8)).astype(np.float32)
    c["m_le"] = le
    c["m_lt"] = lt
    c["m_gt"] = lt.T.copy()
    c["m_le_s"] = le * blk
    c["m_lt_s"] = lt * blk
    c["m_gt_s"] = lt.T * blk
    c["m_le_b"] = le.astype(ml_dtypes.bfloat16)
    rm = np.ones((NT,), np.float32)
    for (s0, n) in TT[:-1]:
        rm[s0] = 0.0
    rm[NPR::8] = 0.0
    c["reset"] = np.broadcast_to(rm[None, :], (128, NT)).copy()
    bo = np.zeros((128, 128), np.float32)
    bo[:64, :64] = 1.0
    bo[64:, 64:] = 1.0
    c["blockones"] = bo
    c["ones_f"] = np.ones((128, 128), np.float32)
    cm = np.zeros((128, 16, 128), np.float32)
    for b in range(16):
        cm[:, b, b * 8:(b + 1) * 8] = 1.0
    c["colmask"] = cm.reshape(128, 16 * 128)
    rmk = np.zeros((128, 16), np.float32)
    for b in range(16):
        rmk[b * 8:(b + 1) * 8, b] = 1.0
    c["rowmask"] = rmk
    nm = np.zeros((8, 16, 8), np.float32)
    for sk in range(8):
        nm[sk, :, sk:] = 1.0
    c["newmask"] = nm.reshape(8, 128).astype(ml_dtypes.bfloat16)
    return c


CONST_DT = {"ident_b": BF16, "m_le_b": BF16, "newmask": BF16}

def col_tiles():
    ct = []
    for i in range(4):
        ct.append((f"q{i}", [(0, 128 * i, 128)], 128, "copy"))
    for i in range(2):
        ct.append((f"kv{i}", [(0, 512 + 128 * i, 128)], 128, "copy"))
    ct.append(("kr", [(64, 768, 32)], 96, "copy"))
    ct.append(("krs", [(64, 784, 16), (80, 768, 16)], 96, "copy"))
    for nm, off in (("r", 800), ("k", 1824), ("v", 2848)):
        for i in range(8):
            ct.append((f"{nm}{i}", [(0, off + 128 * i, 128)], 128, "copy"))
    ct.append(("wl", [(0, 3872, 96)], 96, "copy"))
    ct.append(("al", [(0, 3968, 96)], 96, "copy"))
    ct.append(("gl0", [(0, 4064, 128)], 128, "copy"))
    ct.append(("gl1", [(0, 4192, 128)], 128, "copy"))
    for i in range(16):
        ct.append((f"ga{i}", [(0, 4320 + 128 * i, 128)], 128, "sig"))
    for i in range(16):
        ct.append((f"gb{i}", [(0, 6368 + 128 * i, 128)], 128, "sig"))
    return ct


CT = col_tiles()
CTI = {c[0]: i for i, c in enumerate(CT)}

IN_SPECS = [
    ("xp", [2048, D], F32), ("xs", [NS, D], F32), ("meta", [16, D], F32),
    ("state_wkv", [16, 16, 64, 64], F32), ("state_shift", [16, RWKV_COLS], F32),
    ("page_table", [16, 128], I32),
    ("norm_mix_g", [1, D], F32), ("w_in", [D, IN_COLS], F32), ("rwkv_mu", [RWKV_COLS, 1], F32),
    ("rwkv_w0", [1024, 1], F32), ("rwkv_w2", [96, 1024], F32), ("rwkv_a0", [1024, 1], F32),
    ("rwkv_a2", [96, 1024], F32), ("rwkv_g2", [256, 1024], F32), ("rwkv_k_k", [1024, 1], F32),
    ("rwkv_k_a", [1024, 1], F32), ("rwkv_r_k", [1024, 1], F32), ("rwkv_ln_g", [1024, 1], F32),
    ("rwkv_ln_b", [1024, 1], F32), ("mla_q_norm_g", [512, 1], F32), ("mla_w_uq", [512, 1536], F32),
    ("mla_kv_norm_g", [256, 1], F32), ("mla_w_uk", [256, 1024], F32), ("mla_w_uv", [256, 1024], F32),
    ("w_up_a", [1024, D], F32), ("w_up_b", [1024, D], F32), ("w_o", [D, D], F32),
    ("norm_ffn_g", [1, D], F32), ("router_w", [D, 36], F32), ("router_b", [1, 36], F32),
    ("expert_w_gate", [32, D, 512], F32), ("expert_w_up", [32, D, 512], F32),
    ("expert_w_down", [32, 512, D], F32), ("norm_final_g", [1, D], F32),
]
OUT_SPECS = [
    ("y_prompt", [2048, D]), ("y_sample", [NS, D]), ("ckv_p", [NPR, 256]), ("kr_p", [NPR, 32]),
    ("wkv_p", [16, 64, 64]), ("sh_p", [1, RWKV_COLS]), ("ckv_s", [NS, 256]), ("kr_s", [NS, 32]),
    ("wkv_s", [16, 16, 64, 64]), ("sh_s", [16, RWKV_COLS]),
]


class Ctx:
    pass


def build(n_pool=20480, upto="all", debug=(), start=None, feed=()):
    nc = bass.Bass("TRN2", target_bir_lowering=False)
    S = Sched(nc)
    X = Ctx()
    X.nc, X.S = nc, S
    X.debug, X.upto = debug, upto
    import os as _os
    X.nct = int(_os.environ.get('KB_NCT', '1000'))
    X.use_fp32r = _os.environ.get('KB_FP32R', '1') == '1'
    I = {}
    for name, shape, dt in IN_SPECS:
        I[name] = nc.dram_tensor(name, shape, dt, kind="ExternalInput").ap()
    I["cache_ckv"] = nc.dram_tensor("cache_ckv", [n_pool, 128, 256], F32, kind="ExternalInput").ap()
    I["cache_krope"] = nc.dram_tensor("cache_krope", [n_pool, 128, 32], F32, kind="ExternalInput").ap()
    consts = make_consts()
    for k, v in consts.items():
        I[k] = nc.dram_tensor(k, list(v.shape), CONST_DT.get(k, F32), kind="ExternalInput").ap()
    O = {}
    for name, shape in OUT_SPECS:
        O[name] = nc.dram_tensor(name, shape, F32, kind="ExternalOutput").ap()
    X.I, X.O = I, O

    def scratch(name, shape, dt):
        kind = "ExternalOutput" if name in debug else ("ExternalInput" if name in feed else "Internal")
        return T(nc.dram_tensor(name, shape, dt, kind=kind).ap())
    X.scratch = scratch

    X.V = lambda fn, r=(), w=(): S.op("vector", fn, r, w)
    X.A = lambda fn, r=(), w=(): S.op("scalar", fn, r, w)
    X.G = lambda fn, r=(), w=(): S.op("gpsimd", fn, r, w)
    X.P = lambda fn, r=(), w=(): S.op("tensor", fn, r, w)
    X.DS = lambda fn, r=(), w=(): S.dma("sync", fn, r, w)
    X.DG = lambda fn, r=(), w=(): S.dma("gpsimd", fn, r, w)

    X.PFM = scratch("PFM", [len(CT), 128, NT], F32)
    X.RW = scratch("RW", [8, 8, 128, NT], F32)
    X.OAT = scratch("OAT", [8, 128, NT], BF16)
    X.OBT = scratch("OBT", [8, 128, NT], BF16)
    X.X2 = scratch("X2", [NT, D], F32)
    X.XN2T = scratch("XN2T", [D, NT], BF16)
    X.COMB = scratch("COMB", [NT, 32], F32)
    X.MTD = scratch("MTD", [128, 16, NT], BF16)

    stages = [stage_ab, stage_c1, stage_c2, stage_d, stage_e, stage_f]
    started = start is None
    for st in stages:
        if not started:
            if st.__name__ != start:
                continue
            started = True
        st(X)
        S.barrier()
        if upto == st.__name__:
            break
    S.finish()
    return nc, consts


class Stage:
    N = 0

    def __init__(self, X):
        self.X = X
        self.es = contextlib.ExitStack()
        self.n = 0

    def __enter__(self):
        self.es.__enter__()
        return self

    def __exit__(self, *a):
        self.X.S.barrier()
        return self.es.__exit__(*a)

    def sb(self, shape, dt=F32, name=None):
        Stage.N += 1
        return T(self.es.enter_context(self.X.nc.sbuf_tensor(f"{name or 'sb'}_{Stage.N}", list(shape), dt)))

    def ps(self, shape, dt=F32, name=None):
        Stage.N += 1
        return T(self.es.enter_context(self.X.nc.psum_tensor(f"{name or 'ps'}_{Stage.N}", list(shape), dt)))

    def load(self, ap_dram, shape, dt=F32, eng="sync", **kw):
        t = self.sb(shape, dt)
        (self.X.DS if eng == "sync" else self.X.DG)(lambda E: E.dma_start(out=t[:], in_=ap_dram, **kw), (), [t])
        return t


class RR:
    def __init__(self, items):
        self.items = items
        self.i = 0

    def next(self):
        x = self.items[self.i % len(self.items)]
        self.i += 1
        return x


def stage_ab(X):
    I, V, A, G, P, DS, DG = X.I, X.V, X.A, X.G, X.P, X.DS, X.DG
    with Stage(X) as st:
        hT = st.sb([128, 16, NT], BF16, "hT")
        identb = st.load(I["ident_b"], [128, 128], BF16)
        with Stage(X) as sa:
            gm = sa.load(I["norm_mix_g"].to_broadcast([128, D]), [128, D])
            xin = RR([sa.sb([128, D]) for _ in range(2)])
            junk = sa.sb([128, D])
            ssq = RR([sa.sb([128, 1]) for _ in range(2)])
            hb = RR([sa.sb([128, D], BF16) for _ in range(2)])
            pst = RR([sa.ps([128, 16, 128], BF16) for _ in range(2)])
            for ti, (g0, n) in enumerate(TT):
                x = xin.next()
                if ti == 0:
                    src = I["meta"]
                elif ti <= 16:
                    src = I["xp"][(ti - 1) * 128: ti * 128, :]
                else:
                    src = I["xs"]
                DS(lambda E, x=x, src=src, n=n: E.dma_start(out=x[0:n, :], in_=src), (), [x])
                sq = ssq.next()
                G(lambda E, x=x, n=n: E.tensor_tensor(out=junk[0:n, :], in0=x[0:n, :], in1=x[0:n, :], op=ALU.mult),
                  [x], [junk])
                V(lambda E, sq=sq, n=n: E.tensor_reduce(out=sq[0:n, :], in_=junk[0:n, :], axis=AX.X, op=ALU.add),
                  [junk], [sq])
                A(lambda E, sq=sq, n=n: E.activation(out=sq[0:n, :], in_=sq[0:n, :], func=AF.Sqrt,
                                                     bias=1e-6, scale=1.0 / D), [sq], [sq])
                V(lambda E, sq=sq, n=n: E.reciprocal(out=sq[0:n, :], in_=sq[0:n, :]), [sq], [sq])
                h = hb.next()
                V(lambda E, x=x, sq=sq, h=h, n=n: E.scalar_tensor_tensor(
                    out=h[0:n, :], in0=x[0:n, :], scalar=sq[0:n, 0:1], in1=gm[0:n, :],
                    op0=ALU.mult, op1=ALU.mult), [x, sq, gm], [h])
                pt = pst.next()
                for kc in range(16):
                    P(lambda E, h=h, pt=pt, kc=kc, n=n: E.transpose(
                        out=pt[:, kc, 0:n], in_=h[0:n, kc * 128:(kc + 1) * 128], identity=identb[0:n, 0:n]),
                      [h, identb], [pt])
                A(lambda E, pt=pt, g0=g0, n=n: E.copy(out=hT[:, :, g0:g0 + n], in_=pt[:, :, 0:n]), [pt], [hT])
        X.S.barrier()
        if "HT" in X.debug:
            HTd = X.scratch("HT", [128, 16, NT], BF16)
            DS(lambda E: E.dma_start(out=HTd[:], in_=hT[:]), [hT], [HTd])
        if X.upto == "A":
            return
        with Stage(X) as sb_:
            wb = RR([sb_.sb([128, 16, 128], BF16) for _ in range(3)])
            stg = RR([sb_.sb([128, NT]) for _ in range(2)])
            pss = RR([sb_.ps([128, 512]) for _ in range(4)])
            w_in = I["w_in"].rearrange("(kc p) c -> p kc c", p=128)
            for ci, (name, pieces, M, kind) in enumerate(CT):
                if ci >= X.nct:
                    break
                w = wb.next()
                if pieces[0][0] != 0:
                    G(lambda E, w=w: E.memset(w[:, :, 0:64], 0.0), (), [w])
                for (dc, sc, n) in pieces:
                    DG(lambda E, w=w, dc=dc, sc=sc, n=n: E.dma_start(out=w[:, :, dc:dc + n], in_=w_in[:, :, sc:sc + n]),
                       (), [w])
                sg = stg.next()
                for (t0, tn) in GROUPS:
                    ps = pss.next()
                    for kc in range(16):
                        P(lambda E, ps=ps, w=w, kc=kc, t0=t0, tn=tn, M=M: E.matmul(
                            ps[0:M, 0:tn], lhsT=w[:, kc, 0:M], rhs=hT[:, kc, t0:t0 + tn],
                            start=(kc == 0), stop=(kc == 15)), [w], [ps])
                    func, scl = (AF.Tanh, 0.5) if kind == "sig" else (AF.Copy, 1.0)
                    A(lambda E, ps=ps, sg=sg, t0=t0, tn=tn, M=M, func=func, scl=scl: E.activation(
                        out=sg[0:M, t0:t0 + tn], in_=ps[0:M, 0:tn], func=func, scale=scl), [ps], [sg])
                DS(lambda E, sg=sg, ci=ci, M=M: E.dma_start(out=X.PFM[ci, 0:M, :], in_=sg[0:M, :]), [sg], [X.PFM])


NLW = -0.30326533


def stage_c1(X):
    I, O, V, A, G, P, DS, DG = X.I, X.O, X.V, X.A, X.G, X.P, X.DS, X.DG
    with Stage(X) as st:
        blockones = st.load(I["blockones"], [128, 128])
        reset = st.load(I["reset"], [128, NT])
        tw = st.sb([128, NT])
        xa = st.sb([128, NT])
        sg = [st.sb([128, NT]), st.sb([128, NT])]
        pbuf = RR([st.sb([128, NT]) for _ in range(2)])
        dbuf = RR([st.sb([128, NT]) for _ in range(2)])
        small = RR([st.sb([128, 32]) for _ in range(8)])
        pss = RR([st.ps([128, 512]) for _ in range(6)])

        def colvec(name, c0, M):
            t = small.next()
            DS(lambda E: E.dma_start(out=t[0:M, 0:1], in_=I[name][c0:c0 + M, :]), (), [t])
            return t

        def xs_tile(name, out):
            ci = CTI[name]
            _, pieces, M, _ = CT[ci]
            rc0 = pieces[0][1] - OFF_RWKV
            p = pbuf.next()
            DS(lambda E: E.dma_start(out=p[0:M, :], in_=X.PFM[ci, 0:M, :]), [X.PFM], [p])
            mu = colvec("rwkv_mu", rc0, M)
            sh = small.next()
            DS(lambda E: E.dma_start(out=sh[0:M, 0:16], in_=I["state_shift"][:, rc0:rc0 + M].rearrange("b c -> c b"),
                                     allow_slow_non_contiguous=True), (), [sh])
            DS(lambda E: E.dma_start(out=O["sh_p"][0:1, rc0:rc0 + M].rearrange("o c -> c o"), in_=p[0:M, NPR - 1:NPR],
                                     allow_slow_non_contiguous=True), [p], [])
            ps_ = p[0:M, NPR:NT].rearrange("p (b s) -> p b s", s=8)
            DS(lambda E: E.dma_start(out=O["sh_s"][:, rc0:rc0 + M].rearrange("b c -> c b"), in_=ps_[:, :, 7],
                                     allow_slow_non_contiguous=True), [p], [])
            d = dbuf.next()
            ds_ = d[0:M, NPR:NT].rearrange("p (b s) -> p b s", s=8)
            G(lambda E: E.tensor_tensor(out=d[0:M, 1:NPR], in0=p[0:M, 0:NPR - 1], in1=p[0:M, 1:NPR], op=ALU.subtract), [p], [d])
            V(lambda E: E.tensor_scalar(out=d[0:M, 0:1], in0=p[0:M, 0:1], scalar1=-1.0, scalar2=None, op0=ALU.mult), [p], [d])
            V(lambda E: E.tensor_tensor(out=ds_[:, :, 1:8], in0=ps_[:, :, 0:7], in1=ps_[:, :, 1:8], op=ALU.subtract), [p], [d])
            V(lambda E: E.tensor_tensor(out=ds_[:, :, 0], in0=sh[0:M, 0:16], in1=ps_[:, :, 0], op=ALU.subtract), [p, sh], [d])
            V(lambda E: E.scalar_tensor_tensor(out=out[0:M, :], in0=d[0:M, :], scalar=mu[0:M, 0:1], in1=p[0:M, :],
                                               op0=ALU.mult, op1=ALU.add), [d, mu, p], [out])

        xs_tile("wl", tw)
        A(lambda E: E.activation(out=tw[0:96, :], in_=tw[0:96, :], func=AF.Tanh), [tw], [tw])
        xs_tile("al", xa)
        for i in range(2):
            xs_tile(f"gl{i}", sg[i])
            A(lambda E, i=i: E.activation(out=sg[i][:], in_=sg[i][:], func=AF.Tanh, scale=0.5), [sg[i]], [sg[i]])
            V(lambda E, i=i: E.tensor_scalar(out=sg[i][:], in0=sg[i][:], scalar1=0.5, scalar2=0.5, op0=ALU.mult, op1=ALU.add),
              [sg[i]], [sg[i]])
        w2 = st.load(I["rwkv_w2"], [96, 1024])
        a2 = st.load(I["rwkv_a2"], [96, 1024])
        g2 = st.load(I["rwkv_g2"].rearrange("(kc p) c -> p kc c", p=128), [128, 2, 1024])
        big = RR([st.sb([128, NT]) for _ in range(12)])
        for j in range(8):
            c0 = j * 128
            xr, xk, xv = big.next(), big.next(), big.next()
            xs_tile(f"r{j}", xr)
            xs_tile(f"k{j}", xk)
            xs_tile(f"v{j}", xv)
            w0 = colvec("rwkv_w0", c0, 128)
            a0 = colvec("rwkv_a0", c0, 128)
            V(lambda E, w0=w0: E.tensor_scalar(out=w0[:, 0:1], in0=w0[:, 0:1], scalar1=0.5, scalar2=None, op0=ALU.mult), [w0], [w0])
            V(lambda E, a0=a0: E.tensor_scalar(out=a0[:, 0:1], in0=a0[:, 0:1], scalar1=0.5, scalar2=None, op0=ALU.mult), [a0], [a0])
            kkv = colvec("rwkv_k_k", c0, 128)
            kav = colvec("rwkv_k_a", c0, 128)
            rkv = colvec("rwkv_r_k", c0, 128)
            logw, alpha, gg = big.next(), big.next(), big.next()
            for (t0, tn) in GROUPS:
                ps = pss.next()
                P(lambda E, ps=ps, t0=t0, tn=tn: E.matmul(ps[:, 0:tn], lhsT=w2[0:96, c0:c0 + 128], rhs=tw[0:96, t0:t0 + tn],
                                                          start=True, stop=True), [w2, tw], [ps])
                A(lambda E, ps=ps, t0=t0, tn=tn: E.activation(out=logw[:, t0:t0 + tn], in_=ps[:, 0:tn], func=AF.Tanh,
                                                              bias=w0[:, 0:1], scale=0.5), [ps, w0], [logw])
                ps = pss.next()
                P(lambda E, ps=ps, t0=t0, tn=tn: E.matmul(ps[:, 0:tn], lhsT=a2[0:96, c0:c0 + 128], rhs=xa[0:96, t0:t0 + tn],
                                                          start=True, stop=True), [a2, xa], [ps])
                A(lambda E, ps=ps, t0=t0, tn=tn: E.activation(out=alpha[:, t0:t0 + tn], in_=ps[:, 0:tn], func=AF.Tanh,
                                                              bias=a0[:, 0:1], scale=0.5), [ps, a0], [alpha])
                ps = pss.next()
                for kc in range(2):
                    P(lambda E, ps=ps, t0=t0, tn=tn, kc=kc: E.matmul(ps[:, 0:tn], lhsT=g2[:, kc, c0:c0 + 128],
                                                                     rhs=sg[kc][:, t0:t0 + tn], start=(kc == 0), stop=(kc == 1)),
                      [g2, sg[kc]], [ps])
                A(lambda E, ps=ps, t0=t0, tn=tn: E.copy(out=gg[:, t0:t0 + tn], in_=ps[:, 0:tn]), [ps], [gg])
            V(lambda E: E.tensor_scalar(out=logw[:], in0=logw[:], scalar1=NLW, scalar2=NLW, op0=ALU.mult, op1=ALU.add), [logw], [logw])
            G(lambda E: E.tensor_scalar(out=alpha[:], in0=alpha[:], scalar1=0.5, scalar2=0.5, op0=ALU.mult, op1=ALU.add), [alpha], [alpha])
            kk, sq, rs = big.next(), big.next(), big.next()
            V(lambda E: E.tensor_scalar(out=kk[:], in0=xk[:], scalar1=kkv[:, 0:1], scalar2=None, op0=ALU.mult), [xk, kkv], [kk])
            G(lambda E: E.tensor_tensor(out=sq[:], in0=kk[:], in1=kk[:], op=ALU.mult), [kk], [sq])
            for (t0, tn) in GROUPS:
                ps = pss.next()
                P(lambda E, ps=ps, t0=t0, tn=tn: E.matmul(ps[:, 0:tn], lhsT=blockones[:], rhs=sq[:, t0:t0 + tn], start=True, stop=True),
                  [blockones, sq], [ps])
                V(lambda E, ps=ps, t0=t0, tn=tn: E.tensor_scalar(out=rs[:, t0:t0 + tn], in0=ps[:, 0:tn], scalar1=1e-24, scalar2=None,
                                                                 op0=ALU.max), [ps], [rs])
            A(lambda E: E.activation(out=rs[:], in_=rs[:], func=AF.Sqrt), [rs], [rs])
            V(lambda E: E.reciprocal(out=rs[:], in_=rs[:]), [rs], [rs])
            V(lambda E: E.tensor_tensor(out=kk[:], in0=kk[:], in1=rs[:], op=ALU.mult), [kk, rs], [kk])
            kmod = big.next()
            V(lambda E: E.tensor_scalar(out=kmod[:], in0=alpha[:], scalar1=-1.0, scalar2=kav[:, 0:1], op0=ALU.add, op1=ALU.mult),
              [alpha, kav], [kmod])
            V(lambda E: E.scalar_tensor_tensor(out=kmod[:], in0=kmod[:], scalar=1.0, in1=xk[:], op0=ALU.add, op1=ALU.mult),
              [kmod, xk], [kmod])
            cum = big.next()
            V(lambda E: E.tensor_tensor_scan(out=cum[:], data0=reset[:], data1=logw[:], initial=0.0, op0=ALU.mult, op1=ALU.add),
              [reset, logw], [cum])
            ew, ewm, ewi = big.next(), sq, rs
            A(lambda E: E.activation(out=ew[:], in_=cum[:], func=AF.Exp), [cum], [ew])
            A(lambda E: E.activation(out=ewi[:], in_=cum[:], func=AF.Exp, scale=-1.0), [cum], [ewi])
            G(lambda E: E.tensor_tensor(out=ewm[:], in0=cum[:], in1=logw[:], op=ALU.subtract), [cum, logw], [ewm])
            A(lambda E: E.activation(out=ewm[:], in_=ewm[:], func=AF.Exp), [ewm], [ewm])
            RWj = lambda a: X.RW[j, a, :, :]
            V(lambda E: E.scalar_tensor_tensor(out=ewm[:], in0=kk[:], scalar=-1.0, in1=ewm[:], op0=ALU.mult, op1=ALU.mult), [kk, ewm], [ewm])
            DS(lambda E: E.dma_start(out=RWj(0), in_=ewm[:]), [ewm], [X.RW])
            V(lambda E: E.scalar_tensor_tensor(out=cum[:], in0=xr[:], scalar=rkv[:, 0:1], in1=kmod[:], op0=ALU.mult, op1=ALU.mult),
              [xr, rkv, kmod], [cum])
            G(lambda E: E.tensor_tensor(out=xr[:], in0=xr[:], in1=ew[:], op=ALU.mult), [xr, ew], [xr])
            DS(lambda E: E.dma_start(out=RWj(1), in_=xr[:]), [xr], [X.RW])
            G(lambda E: E.tensor_tensor(out=kk[:], in0=kk[:], in1=alpha[:], op=ALU.mult), [kk, alpha], [kk])
            V(lambda E: E.tensor_tensor(out=kk[:], in0=kk[:], in1=ewi[:], op=ALU.mult), [kk, ewi], [kk])
            DS(lambda E: E.dma_start(out=RWj(2), in_=kk[:]), [kk], [X.RW])
            G(lambda E: E.tensor_tensor(out=kmod[:], in0=kmod[:], in1=ewi[:], op=ALU.mult), [kmod, ewi], [kmod])
            DS(lambda E: E.dma_start(out=RWj(3), in_=kmod[:]), [kmod], [X.RW])
            DS(lambda E: E.dma_start(out=RWj(4), in_=xv[:]), [xv], [X.RW])
            for (t0, tn) in GROUPS:
                ps = pss.next()
                P(lambda E, ps=ps, t0=t0, tn=tn: E.matmul(ps[:, 0:tn], lhsT=blockones[:], rhs=cum[:, t0:t0 + tn], start=True, stop=True),
                  [blockones, cum], [ps])
                V(lambda E, ps=ps, t0=t0, tn=tn: E.tensor_tensor(out=logw[:, t0:t0 + tn], in0=ps[:, 0:tn], in1=xv[:, t0:t0 + tn],
                                                                 op=ALU.mult), [ps, xv], [logw])
            DS(lambda E: E.dma_start(out=RWj(5), in_=logw[:]), [logw], [X.RW])
            DS(lambda E: E.dma_start(out=RWj(6), in_=gg[:]), [gg], [X.RW])
            DS(lambda E: E.dma_start(out=RWj(7), in_=ew[:]), [ew], [X.RW])


def stage_c2(X):
    R = (lambda ap: ap.bitcast(F32R)) if X.use_fp32r else (lambda ap: ap)
    I, O, V, A, G, P, DS, DG = X.I, X.O, X.V, X.A, X.G, X.P, X.DS, X.DG
    with Stage(X) as st:
        identf = st.load(I["ident_f"], [128, 128])
        mk4 = {}
        mk1 = {}
        for sfx in ("", "_s"):
            m4 = st.sb([128, 4, 128])
            for a, nm in enumerate(("m_lt", "m_gt", "m_lt", "m_le")):
                DS(lambda E: E.dma_start(out=m4[:, a, :], in_=I[nm + sfx]), (), [m4])
            mk4[sfx] = m4
            mk1[sfx] = st.load(I["m_le" + sfx], [128, 128])
        colmask = st.load(I["colmask"], [128, 2048])
        rowmask = st.load(I["rowmask"], [128, 16])
        lng = st.load(I["rwkv_ln_g"].rearrange("(j p) o -> p (j o)", p=128), [128, 8])
        lnb = st.load(I["rwkv_ln_b"].rearrange("(j p) o -> p (j o)", p=128), [128, 8])
        STp = [st.sb([128, 64]) for _ in range(8)]
        for j in range(8):
            G(lambda E: E.memset(STp[j][:], 0.0), (), [STp[j]])
            V(lambda E: E.tensor_scalar(out=R(STp[j][:]), in0=STp[j][:], scalar1=1.0, scalar2=None, op0=ALU.mult), [STp[j]], [STp[j]])
        STs = [st.sb([128, 16, 64]) for _ in range(8)]
        pss = RR([st.ps([128, 512]) for _ in range(8)])
        with Stage(X) as s0:
            sin = RR([s0.sb([64, 16, 128]) for _ in range(2)])
            for j in range(8):
                si = sin.next()
                for h2 in range(2):
                    DS(lambda E: E.dma_start(out=si[:, :, h2 * 64:h2 * 64 + 64],
                                             in_=I["state_wkv"][:, 2 * j + h2, :, :].rearrange("b v k -> v b k")), (), [si])
                for half in range(2):
                    ps = pss.next()
                    for bb in range(8):
                        b = half * 8 + bb
                        P(lambda E: E.transpose(out=ps[:, bb * 64:(bb + 1) * 64], in_=si[0:64, b, :], identity=identf[0:64, 0:64]),
                          [si, identf], [ps])
                    A(lambda E: E.copy(out=R(STs[j][:, half * 8:half * 8 + 8, :]), in_=ps[:, :].rearrange("p (b v) -> p b v", v=64)),
                      [ps], [STs[j]])
        inpool = RR([st.sb([128, 8, 5, 128]) for _ in range(1)])
        INparts = {id(t): [T(t.t) for _ in range(5)] for t in inpool.items}
        tmpool = RR([st.sb([128, 8, 3, 128]) for _ in range(1)])
        bgpool = RR([st.sb([128, 8, 2, 128]) for _ in range(1)])
        BGparts = {id(t): [T(t.t) for _ in range(2)] for t in bgpool.items}
        wcpool = RR([st.sb([128, 8, 16]) for _ in range(2)])
        M4 = [st.sb([128, 4, 128]) for _ in range(8)]
        MKR = [st.sb([128, 128]) for _ in range(8)]
        LV = [[st.sb([128, 2, 128]) for _ in range(2)] for _ in range(8)]
        PM = [[st.sb([128, 128]) for _ in range(2)] for _ in range(8)]
        XT = st.sb([128, 16, 64])
        UT = st.sb([128, 16, 64])
        Y = st.sb([128, 16, 64])
        cen = st.sb([128, 16, 64])
        sqv = st.sb([128, 16, 64])
        stat = RR([st.sb([128, 16]) for _ in range(4)])
        bdp = RR([st.sb([128, 16, 128]) for _ in range(2)])
        ubd = RR([st.sb([128, 16, 64]) for _ in range(2)])
        tmpf = RR([st.sb([128, 128]) for _ in range(3)])
        oap = RR([st.sb([128, 8, 128], BF16) for _ in range(2)])
        stmp = RR([st.sb([128, 16, 64]) for _ in range(2)])

        X.S.fp32r = X.use_fp32r
        for ci, (g0, n) in enumerate(TT):
            is_s = (ci == len(TT) - 1)
            sfx = "_s" if is_s else ""
            L = 3 if is_s else (4 if n == 16 else 7)
            IN = inpool.next()
            INa = INparts[id(IN)]
            for a in range(5):
                DS(lambda E: E.dma_start(out=IN[:, :, a, 0:n], in_=X.RW[:, a, :, g0:g0 + n].rearrange("j p t -> p j t")), [X.RW], [INa[a]])
            BG = bgpool.next()
            BGa = BGparts[id(BG)]
            for a in range(2):
                DS(lambda E: E.dma_start(out=BG[:, :, a, 0:n], in_=X.RW[:, 5 + a, :, g0:g0 + n].rearrange("j p t -> p j t")), [X.RW], [BGa[a]])
            WC = wcpool.next()
            if not is_s:
                DS(lambda E: E.dma_start(out=WC[:, :, 0], in_=X.RW[:, 7, :, g0 + n - 1].rearrange("j p -> p j"),
                                         allow_slow_non_contiguous=True), [X.RW], [WC])
            else:
                for j in range(8):
                    DS(lambda E: E.dma_start(out=WC[:, j, :], in_=X.RW[j, 7, :, NPR:NT].rearrange("p (b s) -> p b s", s=8)[:, :, 7],
                                             allow_slow_non_contiguous=True), [X.RW], [WC])
            TM = tmpool.next()
            for j in range(8):
                ps = pss.next()
                for a3, a in enumerate((2, 3, 4)):
                    P(lambda E: E.transpose(out=ps[0:n, a3 * 128:(a3 + 1) * 128], in_=IN[:, j, a, 0:n], identity=identf[:, :]),
                      [*INa, identf], [ps])
                A(lambda E: E.copy(out=R(TM[0:n, j, :, :]), in_=ps[0:n, 0:384].rearrange("p (a t) -> p a t", a=3)), [ps], [TM])
            npb = 1 if is_s else 4
            for hb in range(8 // npb):
                heads = [(hb * npb + jj, h2) for jj in range(npb) for h2 in range(2)]
                NH = len(heads)
                for hi, (j, h2) in enumerate(heads):
                    rows = slice(h2 * 64, h2 * 64 + 64)
                    at, rt, bt, kt = (IN[rows, j, a, 0:n] for a in range(4))
                    ps = pss.next()
                    for a, (l, r) in enumerate(((bt, at), (at, bt), (kt, at), (bt, rt))):
                        P(lambda E: E.matmul(ps[0:n, a * 128:a * 128 + n], lhsT=l, rhs=r, start=True, stop=True), [*INa], [ps])
                    V(lambda E: E.tensor_tensor(out=R(M4[hi][0:n, :, 0:n]), in0=ps[0:n, :].rearrange("p (a t) -> p a t", a=4)[:, :, 0:n],
                                                in1=mk4[sfx][0:n, :, 0:n], op=ALU.mult), [ps, mk4[sfx]], [M4[hi]])
                    ps = pss.next()
                    P(lambda E: E.matmul(ps[0:n, 0:n], lhsT=kt, rhs=rt, start=True, stop=True), [*INa], [ps])
                    V(lambda E: E.tensor_tensor(out=R(MKR[hi][0:n, 0:n]), in0=ps[0:n, 0:n], in1=mk1[sfx][0:n, 0:n], op=ALU.mult),
                      [ps, mk1[sfx]], [MKR[hi]])
                    G(lambda E: E.tensor_tensor(out=R(PM[hi][0][0:n, 0:n]), in0=M4[hi][0:n, 0, 0:n], in1=identf[0:n, 0:n], op=ALU.add),
                      [M4[hi], identf], [PM[hi][0]])
                for i in range(1, L):
                    for hi in range(NH):
                        if i == 1:
                            Ap, ATp, srcT = M4[hi][0:n, 0, 0:n], M4[hi][0:n, 1, 0:n], M4[hi]
                        else:
                            srcT = LV[hi][(i - 1) % 2]
                            Ap, ATp = srcT[0:n, 0, 0:n], srcT[0:n, 1, 0:n]
                        ps = pss.next()
                        P(lambda E: E.matmul(ps[0:n, 0:n], lhsT=ATp, rhs=Ap, start=True, stop=True), [srcT], [ps])
                        P(lambda E: E.matmul(ps[0:n, 128:128 + n], lhsT=Ap, rhs=ATp, start=True, stop=True), [srcT], [ps])
                        dst = LV[hi][i % 2]
                        A(lambda E: E.copy(out=R(dst[0:n, :, 0:n]), in_=ps[0:n, 0:256].rearrange("p (a t) -> p a t", a=2)[:, :, 0:n]),
                          [ps], [dst])
                    for hi in range(NH):
                        cur = LV[hi][i % 2]
                        Pp, Pn = PM[hi][(i - 1) % 2], PM[hi][i % 2]
                        ps = pss.next()
                        P(lambda E: E.matmul(ps[0:n, 0:n], lhsT=cur[0:n, 1, 0:n], rhs=Pp[0:n, 0:n], start=True, stop=True), [cur, Pp], [ps])
                        V(lambda E: E.tensor_tensor(out=R(Pn[0:n, 0:n]), in0=ps[0:n, 0:n], in1=Pp[0:n, 0:n], op=ALU.add), [ps, Pp], [Pn])
                Pf = [PM[hi][(L - 1) % 2] for hi in range(NH)]
                bd = {}
                if is_s:
                    for jj in range(npb):
                        j = hb * npb + jj
                        for a in range(2):
                            t = bdp.next()
                            V(lambda E: E.tensor_tensor(out=R(t[:]), in0=IN[:, j, a, :].unsqueeze(1).to_broadcast([128, 16, 128]),
                                                        in1=colmask[:, :].rearrange("p (b t) -> p b t", b=16), op=ALU.mult),
                              [*INa, colmask], [t])
                            bd[(j, a)] = t
                for hi, (j, h2) in enumerate(heads):
                    h = 2 * j + h2
                    rows = slice(h2 * 64, h2 * 64 + 64)
                    vtm = TM[0:n, j, 2, h2 * 64:h2 * 64 + 64]
                    ps = pss.next()
                    if not is_s:
                        P(lambda E: E.matmul(ps[0:n, 0:64], lhsT=IN[rows, j, 0, 0:n], rhs=STp[j][rows, :], start=True, stop=False),
                          [*INa, STp[j]], [ps])
                    else:
                        for b in range(16):
                            P(lambda E: E.matmul(ps[0:n, 0:64], lhsT=bd[(j, 0)][rows, b, :], rhs=STs[j][rows, b, :],
                                                 start=(b == 0), stop=False), [bd[(j, 0)], STs[j]], [ps])
                    P(lambda E: E.matmul(ps[0:n, 0:64], lhsT=M4[hi][0:n, 2, 0:n], rhs=vtm, start=False, stop=True), [M4[hi], TM], [ps])
                    A(lambda E: E.copy(out=R(XT[0:n, h, :]), in_=ps[0:n, 0:64]), [ps], [XT])
                for hi, (j, h2) in enumerate(heads):
                    h = 2 * j + h2
                    ps = pss.next()
                    P(lambda E: E.matmul(ps[0:n, 0:64], lhsT=Pf[hi][0:n, 0:n], rhs=XT[0:n, h, :], start=True, stop=True), [Pf[hi], XT], [ps])
                    A(lambda E: E.copy(out=R(UT[0:n, h, :]), in_=ps[0:n, 0:64]), [ps], [UT])
                for hi, (j, h2) in enumerate(heads):
                    h = 2 * j + h2
                    rows = slice(h2 * 64, h2 * 64 + 64)
                    vtm = TM[0:n, j, 2, h2 * 64:h2 * 64 + 64]
                    ps = pss.next()
                    if not is_s:
                        P(lambda E: E.matmul(ps[0:n, 0:64], lhsT=IN[rows, j, 1, 0:n], rhs=STp[j][rows, :], start=True, stop=False),
                          [*INa, STp[j]], [ps])
                    else:
                        for b in range(16):
                            P(lambda E: E.matmul(ps[0:n, 0:64], lhsT=bd[(j, 1)][rows, b, :], rhs=STs[j][rows, b, :],
                                                 start=(b == 0), stop=False), [bd[(j, 1)], STs[j]], [ps])
                    P(lambda E: E.matmul(ps[0:n, 0:64], lhsT=M4[hi][0:n, 3, 0:n], rhs=UT[0:n, h, :], start=False, stop=False), [M4[hi], UT], [ps])
                    P(lambda E: E.matmul(ps[0:n, 0:64], lhsT=MKR[hi][0:n, 0:n], rhs=vtm, start=False, stop=True), [MKR[hi], TM], [ps])
                    A(lambda E: E.copy(out=Y[0:n, h, :], in_=ps[0:n, 0:64]), [ps], [Y])
                for hi, (j, h2) in enumerate(heads):
                    h = 2 * j + h2
                    rows = slice(h2 * 64, h2 * 64 + 64)
                    vtm = TM[0:n, j, 2, h2 * 64:h2 * 64 + 64]
                    if not is_s:
                        ps = pss.next()
                        P(lambda E: E.matmul(ps[:, 0:64], lhsT=TM[0:n, j, 0, :], rhs=UT[0:n, h, :], start=True, stop=False), [TM, UT], [ps])
                        P(lambda E: E.matmul(ps[:, 0:64], lhsT=TM[0:n, j, 1, :], rhs=vtm, start=False, stop=True), [TM], [ps])
                        V(lambda E: E.tensor_tensor(out=R(STp[j][rows, :]), in0=ps[rows, 0:64], in1=STp[j][rows, :], op=ALU.add), [ps, STp[j]], [STp[j]])
                        V(lambda E: E.tensor_scalar(out=R(STp[j][rows, :]), in0=STp[j][rows, :], scalar1=WC[rows, j, 0:1], scalar2=None,
                                                    op0=ALU.mult), [STp[j], WC], [STp[j]])
                    else:
                        ub, vb = ubd.next(), ubd.next()
                        rmb = rowmask[:, :].unsqueeze(2).to_broadcast([128, 16, 64])
                        V(lambda E: E.tensor_tensor(out=R(ub[:]), in0=UT[:, h, :].unsqueeze(1).to_broadcast([128, 16, 64]), in1=rmb, op=ALU.mult),
                          [UT, rowmask], [ub])
                        V(lambda E: E.tensor_tensor(out=R(vb[:]), in0=vtm.unsqueeze(1).to_broadcast([128, 16, 64]), in1=rmb, op=ALU.mult),
                          [TM, rowmask], [vb])
                        for half in range(2):
                            ps = pss.next()
                            bs = slice(half * 8, half * 8 + 8)
                            P(lambda E: E.matmul(ps[:, :], lhsT=TM[:, j, 0, :], rhs=ub[:, bs, :].rearrange("p b v -> p (b v)"),
                                                 start=True, stop=False), [TM, ub], [ps])
                            P(lambda E: E.matmul(ps[:, :], lhsT=TM[:, j, 1, :], rhs=vb[:, bs, :].rearrange("p b v -> p (b v)"),
                                                 start=False, stop=True), [TM, vb], [ps])
                            tt = stmp.next()
                            V(lambda E: E.tensor_tensor(out=tt[rows, 0:8, :], in0=ps[rows, :].rearrange("p (b v) -> p b v", v=64),
                                                        in1=STs[j][rows, bs, :], op=ALU.add), [ps, STs[j]], [tt])
                            V(lambda E: E.tensor_tensor(out=R(STs[j][rows, bs, :]), in0=tt[rows, 0:8, :],
                                                        in1=WC[rows, j, bs].unsqueeze(2).to_broadcast([64, 8, 64]), op=ALU.mult),
                              [tt, WC], [STs[j]])
            mu, var = stat.next(), stat.next()
            V(lambda E: E.tensor_reduce(out=mu[0:n, :], in_=Y[0:n, :, :], axis=AX.X, op=ALU.add), [Y], [mu])
            V(lambda E: E.tensor_scalar(out=mu[0:n, :], in0=mu[0:n, :], scalar1=1.0 / 64, scalar2=None, op0=ALU.mult), [mu], [mu])
            G(lambda E: E.tensor_tensor(out=cen[0:n], in0=Y[0:n], in1=mu[0:n, :].unsqueeze(2).to_broadcast([n, 16, 64]), op=ALU.subtract),
              [Y, mu], [cen])
            G(lambda E: E.tensor_tensor(out=sqv[0:n], in0=cen[0:n], in1=cen[0:n], op=ALU.mult), [cen], [sqv])
            V(lambda E: E.tensor_reduce(out=var[0:n, :], in_=sqv[0:n, :, :], axis=AX.X, op=ALU.add), [sqv], [var])
            A(lambda E: E.activation(out=var[0:n, :], in_=var[0:n, :], func=AF.Sqrt, bias=64e-5, scale=1.0 / 64), [var], [var])
            V(lambda E: E.reciprocal(out=var[0:n, :], in_=var[0:n, :]), [var], [var])
            V(lambda E: E.tensor_tensor(out=cen[0:n], in0=cen[0:n], in1=var[0:n, :].unsqueeze(2).to_broadcast([n, 16, 64]), op=ALU.mult),
              [cen, var], [cen])
            OA = oap.next()
            for j in range(8):
                ps = pss.next()
                P(lambda E: E.transpose(out=ps[:, 0:n], in_=cen[0:n, 2 * j:2 * j + 2, :].rearrange("p h v -> p (h v)"),
                                        identity=identf[0:n, 0:n]), [cen, identf], [ps])
                tf = tmpf.next()
                V(lambda E: E.tensor_scalar(out=tf[:, 0:n], in0=ps[:, 0:n], scalar1=lng[:, j:j + 1], scalar2=lnb[:, j:j + 1],
                                            op0=ALU.mult, op1=ALU.add), [ps, lng, lnb], [tf])
                G(lambda E: E.tensor_tensor(out=tf[:, 0:n], in0=tf[:, 0:n], in1=BG[:, j, 0, 0:n], op=ALU.add), [tf, *BGa], [tf])
                G(lambda E: E.tensor_tensor(out=OA[:, j, 0:n], in0=tf[:, 0:n], in1=BG[:, j, 1, 0:n], op=ALU.mult), [tf, *BGa], [OA])
            DS(lambda E: E.dma_start(out=X.OAT[:, :, g0:g0 + n].rearrange("j p t -> p j t"), in_=OA[:, :, 0:n]), [OA], [X.OAT])
        X.S.fp32r = False
        with Stage(X) as s1:
            so = s1.sb([64, 8, 128])
            for j in range(8):
                ps = pss.next()
                P(lambda E: E.transpose(out=ps[0:64, 0:128], in_=STp[j][:, :], identity=identf[:, :]), [STp[j], identf], [ps])
                A(lambda E: E.copy(out=so[:, j, :], in_=ps[0:64, 0:128]), [ps], [so])
            for h2 in range(2):
                DS(lambda E: E.dma_start(out=O["wkv_p"].rearrange("(j h) v k -> h v j k", h=2)[h2],
                                         in_=so[:, :, h2 * 64:h2 * 64 + 64]), [so], [])
            sos = RR([s1.sb([64, 16, 128]) for _ in range(1)])
            for j in range(8):
                sj = sos.next()
                for q in range(4):
                    ps = pss.next()
                    for bb in range(4):
                        b = q * 4 + bb
                        P(lambda E: E.transpose(out=ps[0:64, bb * 128:(bb + 1) * 128], in_=STs[j][:, b, :], identity=identf[:, :]),
                          [STs[j], identf], [ps])
                    A(lambda E: E.copy(out=sj[:, q * 4:q * 4 + 4, :], in_=ps[0:64, :].rearrange("p (b k) -> p b k", b=4)), [ps], [sj])
                for h2 in range(2):
                    DS(lambda E: E.dma_start(out=O["wkv_s"][:, 2 * j + h2, :, :].rearrange("b v k -> v b k"),
                                             in_=sj[:, :, h2 * 64:h2 * 64 + 64]), [sj], [])


def stage_d(X):
    I, O, V, A, G, P, DS, DG = X.I, X.O, X.V, X.A, X.G, X.P, X.DS, X.DG
    with Stage(X) as st:
        identb = st.load(I["ident_b"], [128, 128], BF16)
        identf = st.load(I["ident_f"], [128, 128])
        onesf = st.load(I["ones_f"], [128, 128])
        mleb = st.load(I["m_le_b"], [128, 128], BF16)
        newmask = st.load(I["newmask"], [8, 128], BF16)
        ropec = st.load(I["rope_c"], [128, NT])
        ropes = st.load(I["rope_s"], [128, NT])
        CQ = st.sb([128, 4, NT], BF16)
        CKVb = st.sb([128, 2, NT], BF16)
        KRb = st.sb([128, NT], BF16)
        KCN = st.sb([8, 16, 257], BF16)
        with Stage(X) as s1:
            pss = RR([s1.ps([128, 512]) for _ in range(4)])
            pbuf = [s1.sb([128, NT]) for _ in range(4)]
            sq = s1.sb([128, NT])
            rq = s1.sb([128, NT])
            CKV = s1.sb([128, 2, NT])
            KR = s1.sb([128, NT])
            gv = s1.sb([128, 8])

            def norm(names, gname, width, outs):
                nt_ = len(names)
                for i, nm in enumerate(names):
                    DS(lambda E: E.dma_start(out=pbuf[i][:], in_=X.PFM[CTI[nm], :, :]), [X.PFM], [pbuf[i]])
                DS(lambda E: E.dma_start(out=gv[:, 0:nt_], in_=I[gname].rearrange("(j p) o -> p (j o)", p=128)), (), [gv])
                for (t0, tn) in GROUPS:
                    ps = pss.next()
                    for i in range(nt_):
                        G(lambda E: E.tensor_tensor(out=sq[:, t0:t0 + tn], in0=pbuf[i][:, t0:t0 + tn], in1=pbuf[i][:, t0:t0 + tn], op=ALU.mult),
                          [pbuf[i]], [sq])
                        P(lambda E: E.matmul(ps[:, 0:tn], lhsT=onesf[:], rhs=sq[:, t0:t0 + tn], start=(i == 0), stop=(i == nt_ - 1)),
                          [onesf, sq], [ps])
                    A(lambda E: E.activation(out=rq[:, t0:t0 + tn], in_=ps[:, 0:tn], func=AF.Sqrt, bias=1e-6, scale=1.0 / width), [ps], [rq])
                V(lambda E: E.reciprocal(out=rq[:], in_=rq[:]), [rq], [rq])
                for i in range(nt_):
                    for o in outs:
                        V(lambda E: E.scalar_tensor_tensor(out=o[:, i, :], in0=pbuf[i][:], scalar=gv[:, i:i + 1], in1=rq[:],
                                                           op0=ALU.mult, op1=ALU.mult), [pbuf[i], gv, rq], [o])

            norm([f"q{i}" for i in range(4)], "mla_q_norm_g", 512, [CQ])
            norm(["kv0", "kv1"], "mla_kv_norm_g", 256, [CKV, CKVb])
            DS(lambda E: E.dma_start(out=pbuf[0][64:96, :], in_=X.PFM[CTI["kr"], 64:96, :]), [X.PFM], [pbuf[0]])
            DS(lambda E: E.dma_start(out=pbuf[1][64:96, :], in_=X.PFM[CTI["krs"], 64:96, :]), [X.PFM], [pbuf[1]])
            r_ = slice(64, 96)
            V(lambda E: E.tensor_tensor(out=KR[r_, :], in0=pbuf[0][r_, :], in1=ropec[r_, :], op=ALU.mult), [pbuf[0], ropec], [KR])
            G(lambda E: E.tensor_tensor(out=sq[r_, :], in0=pbuf[1][r_, :], in1=ropes[r_, :], op=ALU.mult), [pbuf[1], ropes], [sq])
            V(lambda E: E.tensor_tensor(out=KR[r_, :], in0=KR[r_, :], in1=sq[r_, :], op=ALU.add), [KR, sq], [KR])
            A(lambda E: E.copy(out=KRb[r_, :], in_=KR[r_, :]), [KR], [KRb])
            otm = RR([s1.sb([128, 288]) for _ in range(2)])
            for ti, (g0, n) in enumerate(TT):
                ps = pss.next()
                for kc in range(2):
                    P(lambda E: E.transpose(out=ps[0:n, kc * 128:(kc + 1) * 128], in_=CKV[:, kc, g0:g0 + n], identity=identf[:, :]),
                      [CKV, identf], [ps])
                P(lambda E: E.transpose(out=ps[0:n, 256:288], in_=KR[r_, g0:g0 + n], identity=identf[r_, r_]), [KR, identf], [ps])
                ot = otm.next()
                A(lambda E: E.copy(out=ot[0:n, :], in_=ps[0:n, 0:288]), [ps], [ot])
                if ti < 17:
                    DS(lambda E: E.dma_start(out=O["ckv_p"][g0:g0 + n, :], in_=ot[0:n, 0:256]), [ot], [])
                    DS(lambda E: E.dma_start(out=O["kr_p"][g0:g0 + n, :], in_=ot[0:n, 256:288]), [ot], [])
                else:
                    wdep = Buf()
                    DS(lambda E: E.dma_start(out=O["ckv_s"][:, :], in_=ot[0:n, 0:256]), [ot], [wdep])
                    DS(lambda E: E.dma_start(out=O["kr_s"][:, :], in_=ot[0:n, 256:288]), [ot], [])
                    kcf = s1.sb([8, 16, 256])
                    DS(lambda E: E.dma_start(out=kcf[:], in_=O["ckv_s"].rearrange("(b s) c -> s b c", s=8)), [wdep], [kcf])
                    V(lambda E: E.tensor_copy(out=KCN[:, :, 0:256], in_=kcf[:]), [kcf], [KCN])
                    V(lambda E: E.memset(KCN[:, :, 256:257], 1.0), (), [KCN])
        QLAT = st.sb([128, 2, 16, 16, 8], BF16)
        QR = st.sb([128, 16, 16, 8], BF16)
        WUVb = st.sb([128, 2, 1024], BF16)
        DG(lambda E: E.dma_start(out=WUVb[:], in_=I["mla_w_uv"].rearrange("(kc p) c -> p kc c", p=128)), (), [WUVb])
        with Stage(X) as s2:
            WQ = s2.sb([128, 4, 1536], BF16)
            DG(lambda E: E.dma_start(out=WQ[:], in_=I["mla_w_uq"].rearrange("(kc p) c -> p kc c", p=128)), (), [WQ])
            WQS = s2.sb([128, 4, 16, 32], BF16)
            wq4 = I["mla_w_uq"].rearrange("(kc p) (h c) -> p kc h c", p=128, c=96)
            for kc in range(4):
                DG(lambda E: E.dma_start(out=WQS[:, kc, :, 0:16], in_=wq4[:, kc, :, 80:96]), (), [WQS])
                DG(lambda E: E.dma_start(out=WQS[:, kc, :, 16:32], in_=wq4[:, kc, :, 64:80]), (), [WQS])
            WUKb = s2.sb([128, 2, 1024], BF16)
            DG(lambda E: E.dma_start(out=WUKb[:], in_=I["mla_w_uk"].rearrange("(kc p) c -> p kc c", p=128)), (), [WUKb])
            WUKT = s2.sb([64, 16, 2, 128], BF16)
            psb = RR([s2.ps([128, 1024], BF16) for _ in range(1)])
            psq = RR([s2.ps([128, 512]) for _ in range(2)])
            pss_ = RR([s2.ps([128, 512]) for _ in range(2)])
            pso = RR([s2.ps([128, 512]) for _ in range(2)])
            for h in range(16):
                ps = psb.next()
                for kc in range(2):
                    P(lambda E: E.transpose(out=ps[0:64, kc * 128:(kc + 1) * 128], in_=WUKb[:, kc, h * 64:(h + 1) * 64], identity=identb[:, :]),
                      [WUKb, identb], [ps])
                A(lambda E: E.copy(out=WUKT[:, h, :, :], in_=ps[0:64, 0:256].rearrange("p (a c) -> p a c", a=2)), [ps], [WUKT])
            Qp = RR([s2.sb([96, NT], BF16) for _ in range(2)])
            Kp = RR([s2.sb([96, NPR], BF16) for _ in range(2)])
            Vp = RR([s2.sb([128, 17, 65], BF16) for _ in range(2)])
            OBp = RR([s2.sb([64, NPR], BF16) for _ in range(2)])
            tmp_r = RR([s2.sb([96, 512]) for _ in range(2)])
            tmp_r2 = RR([s2.sb([96, 512]) for _ in range(2)])
            PTp = RR([s2.sb([128, 4, 128], BF16) for _ in range(3)])
            otp = RR([s2.sb([128, 64], BF16) for _ in range(2)])
            rlp = RR([s2.sb([128, 1]) for _ in range(2)])
            for h in range(16):
                Qh, Kh, Vh, OBh = Qp.next(), Kp.next(), Vp.next(), OBp.next()
                for (t0, tn) in GROUPS:
                    psA, psB = psq.next(), psq.next()
                    for kc in range(4):
                        P(lambda E: E.matmul(psA[0:96, 0:tn], lhsT=WQ[:, kc, h * 96:(h + 1) * 96], rhs=CQ[:, kc, t0:t0 + tn],
                                             start=(kc == 0), stop=(kc == 3)), [WQ, CQ], [psA])
                    for kc in range(4):
                        P(lambda E: E.matmul(psB[0:32, 0:tn], lhsT=WQS[:, kc, h, :], rhs=CQ[:, kc, t0:t0 + tn],
                                             start=(kc == 0), stop=(kc == 3)), [WQS, CQ], [psB])
                    A(lambda E: E.copy(out=Qh[0:64, t0:t0 + tn], in_=psA[0:64, 0:tn]), [psA], [Qh])
                    t1, t2 = tmp_r.next(), tmp_r2.next()
                    V(lambda E: E.tensor_tensor(out=t1[64:96, 0:tn], in0=psB[0:32, 0:tn], in1=ropes[0:32, t0:t0 + tn], op=ALU.mult),
                      [psB, ropes], [t1])
                    V(lambda E: E.tensor_tensor(out=t2[64:96, 0:tn], in0=psA[64:96, 0:tn], in1=ropec[64:96, t0:t0 + tn], op=ALU.mult),
                      [psA, ropec], [t2])
                    G(lambda E: E.tensor_tensor(out=Qh[64:96, t0:t0 + tn], in0=t1[64:96, 0:tn], in1=t2[64:96, 0:tn], op=ALU.add),
                      [t1, t2], [Qh])
                    if t0 < NPR:
                        kn = min(tn, NPR - t0)
                        psK = psq.next()
                        for kc in range(2):
                            P(lambda E: E.matmul(psK[0:64, 0:kn], lhsT=WUKb[:, kc, h * 64:(h + 1) * 64], rhs=CKVb[:, kc, t0:t0 + kn],
                                                 start=(kc == 0), stop=(kc == 1)), [WUKb, CKVb], [psK])
                        A(lambda E: E.copy(out=Kh[0:64, t0:t0 + kn], in_=psK[0:64, 0:kn]), [psK], [Kh])
                G(lambda E: E.tensor_copy(out=Kh[64:96, :], in_=KRb[64:96, 0:NPR]), [KRb], [Kh])
                G(lambda E: E.memset(Vh[:, :, 64:65], 1.0), (), [Vh])
                for ti in range(17):
                    g0, n = TT[ti]
                    psV = psq.next()
                    for kc in range(2):
                        P(lambda E: E.matmul(psV[0:n, 0:64], lhsT=CKVb[:, kc, g0:g0 + n], rhs=WUVb[:, kc, h * 64:(h + 1) * 64],
                                             start=(kc == 0), stop=(kc == 1)), [CKVb, WUVb], [psV])
                    A(lambda E: E.copy(out=Vh[0:n, ti, 0:64], in_=psV[0:n, 0:64]), [psV], [Vh])
                for kc in range(2):
                    psL = psq.next()
                    P(lambda E: E.matmul(psL[:, 0:128], lhsT=WUKT[0:64, h, kc, :], rhs=Qh[0:64, NPR:NT], start=True, stop=True), [WUKT, Qh], [psL])
                    A(lambda E: E.copy(out=QLAT[:, kc, :, h, :], in_=psL[:, 0:128].rearrange("p (b s) -> p b s", s=8)), [psL], [QLAT])
                G(lambda E: E.tensor_copy(out=QR[64:96, :, h, :], in_=Qh[64:96, NPR:NT].rearrange("p (b s) -> p b s", s=8)), [Qh], [QR])
                for qi in range(17):
                    q0, nq = TT[qi]
                    po = pso.next()
                    klist = list(range(qi + 1))
                    groups = [[0]] + [klist[1:][i:i + 4] for i in range(0, len(klist) - 1, 4)]
                    nk_total = len(klist)
                    done = 0
                    for grp in groups:
                        psS = pss_.next()
                        PT = PTp.next()
                        nk = TT[grp[0]][1]
                        for a, kt in enumerate(grp):
                            k0, _ = TT[kt]
                            P(lambda E: E.matmul(psS[0:nk, a * 128:a * 128 + nq], lhsT=Kh[:, k0:k0 + nk], rhs=Qh[:, q0:q0 + nq],
                                                 start=True, stop=True), [Kh, Qh], [psS])
                        na = len(grp)
                        A(lambda E: E.activation(out=PT[0:nk, 0:na, 0:nq], in_=psS[0:nk, 0:na * 128].rearrange("p (a q) -> p a q", a=na)[:, :, 0:nq],
                                                 func=AF.Exp, scale=SCALE), [psS], [PT])
                        if grp[-1] == qi:
                            a = na - 1
                            G(lambda E: E.tensor_tensor(out=PT[0:nk, a, 0:nq], in0=PT[0:nk, a, 0:nq], in1=mleb[0:nk, 0:nq], op=ALU.mult),
                              [PT, mleb], [PT])
                        for a, kt in enumerate(grp):
                            P(lambda E: E.matmul(po[0:nq, 0:65], lhsT=PT[0:nk, a, 0:nq], rhs=Vh[0:nk, kt, :], start=(done == 0),
                                                 stop=(done == nk_total - 1)), [PT, Vh], [po])
                            done += 1
                    rl = rlp.next()
                    V(lambda E: E.reciprocal(out=rl[0:nq, :], in_=po[0:nq, 64:65]), [po], [rl])
                    ot = otp.next()
                    V(lambda E: E.tensor_scalar(out=ot[0:nq, :], in0=po[0:nq, 0:64], scalar1=rl[0:nq, 0:1], scalar2=None, op0=ALU.mult),
                      [po, rl], [ot])
                    pt_ = psb.next()
                    P(lambda E: E.transpose(out=pt_[0:64, 0:nq], in_=ot[0:nq, :], identity=identb[0:nq, 0:nq]), [ot, identb], [pt_])
                    A(lambda E: E.copy(out=OBh[:, q0:q0 + nq], in_=pt_[0:64, 0:nq]), [pt_], [OBh])
                DS(lambda E: E.dma_start(out=X.OBT[h // 2, (h % 2) * 64:(h % 2) * 64 + 64, 0:NPR], in_=OBh[:, :]), [OBh], [X.OBT])
        with Stage(X) as s3:
            ptb = s3.sb([128, 16], I32)
            DS(lambda E: E.dma_start(out=ptb[:], in_=I["page_table"].rearrange("b j -> j b")), (), [ptb])
            idx = s3.sb([128, 16, 16], I32)
            for g in range(16):
                V(lambda E: E.tensor_scalar(out=idx[:, :, g], in0=ptb[:], scalar1=16, scalar2=g, op0=ALU.mult, op1=ALU.add), [ptb], [idx])
            ckv_v = I["cache_ckv"].rearrange("n (g r) c -> (n g) (r c)", g=16)
            kr_v = I["cache_krope"].rearrange("n (g r) c -> (n g) (r c)", g=16)
            Gk = RR([s3.sb([128, 8, 256]) for _ in range(3)])
            Gr = RR([s3.sb([128, 8, 32]) for _ in range(3)])
            KCb = RR([s3.sb([128, 8, 257], BF16) for _ in range(2)])
            KRc = RR([s3.sb([128, 8, 96], BF16) for _ in range(2)])
            KCh = {}
            for t in KCb.items:
                G(lambda E: E.memset(t[:, :, 256:257], 1.0), (), [t])
                KCh[id(t)] = (T(t.t), T(t.t))
            for t in KRc.items:
                G(lambda E: E.memset(t[:, :, 0:64], 0.0), (), [t])
            KT = RR([s3.sb([128, 2, 4, 128], BF16) for _ in range(2)])
            KTh = {id(t): (T(t.t), T(t.t)) for t in KT.items}
            KTR = RR([s3.sb([96, 4, 128], BF16) for _ in range(2)])
            PTs = RR([s3.sb([128, 4, 128], BF16) for _ in range(2)])
            PN = RR([s3.sb([8, 128], BF16) for _ in range(2)])
            OL = RR([s3.sb([128, 256], BF16) for _ in range(2)])
            rlp = RR([s3.sb([128, 1]) for _ in range(2)])
            OLT = s3.sb([128, 2, 16, 16, 8], BF16)
            psA = RR([s3.ps([128, 512]) for _ in range(2)])
            psS = RR([s3.ps([128, 512]) for _ in range(2)])
            psT = RR([s3.ps([128, 2, 4, 128], BF16) for _ in range(1)])
            psR = RR([s3.ps([128, 4, 128], BF16) for _ in range(2)])
            for b in range(16):
                pa = psA.next()
                first = True
                for g in range(16):
                    gk, gr = Gk.next(), Gr.next()
                    DG(lambda E: E.indirect_dma_start(out=gk[:].rearrange("p r c -> p (r c)"), out_offset=None, in_=ckv_v,
                                                      in_offset=bass.IndirectOffsetOnAxis(ap=idx[:, b, g:g + 1], axis=0)), [idx], [gk])
                    DG(lambda E: E.indirect_dma_start(out=gr[:].rearrange("p r c -> p (r c)"), out_offset=None, in_=kr_v,
                                                      in_offset=bass.IndirectOffsetOnAxis(ap=idx[:, b, g:g + 1], axis=0)), [idx], [gr])
                    kcb, krc = KCb.next(), KRc.next()
                    kh = KCh[id(kcb)]
                    G(lambda E: E.tensor_copy(out=kcb[:, 0:4, 0:256], in_=gk[:, 0:4, :]), [gk, kcb], [kh[0]])
                    A(lambda E: E.copy(out=kcb[:, 4:8, 0:256], in_=gk[:, 4:8, :]), [gk, kcb], [kh[1]])
                    V(lambda E: E.tensor_copy(out=krc[:, :, 64:96], in_=gr[:]), [gr], [krc])
                    for r4 in range(2):
                        pT, pR = psT.next(), psR.next()
                        for rr in range(4):
                            r = r4 * 4 + rr
                            for kc in range(2):
                                P(lambda E: E.transpose(out=pT[:, kc, rr, :], in_=kcb[:, r, kc * 128:(kc + 1) * 128], identity=identb[:, :]),
                                  [kh[r4], identb], [pT])
                            P(lambda E: E.transpose(out=pR[0:96, rr, :], in_=krc[:, r, :], identity=identb[:, :]), [krc, identb], [pR])
                        kt, ktr = KT.next(), KTR.next()
                        A(lambda E: E.copy(out=kt[:, 0], in_=pT[:, 0]), [pT], [kt])
                        V(lambda E: E.tensor_copy(out=kt[:, 1], in_=pT[:, 1]), [pT], [kt])
                        V(lambda E: E.tensor_copy(out=ktr[64:96], in_=pR[64:96]), [pR], [ktr])
                        pS = psS.next()
                        for rr in range(4):
                            for kc in range(2):
                                P(lambda E: E.matmul(pS[:, rr * 128:(rr + 1) * 128], lhsT=kt[:, kc, rr, :],
                                                     rhs=QLAT[:, kc, b].rearrange("p h s -> p (h s)"), start=(kc == 0), stop=False),
                                  [kt, QLAT], [pS])
                            P(lambda E: E.matmul(pS[:, rr * 128:(rr + 1) * 128], lhsT=ktr[64:96, rr, :],
                                                 rhs=QR[64:96, b].rearrange("p h s -> p (h s)"), start=False, stop=True), [ktr, QR], [pS])
                        pts = PTs.next()
                        A(lambda E: E.activation(out=pts[:].rearrange("p a q -> p (a q)"), in_=pS[:, :], func=AF.Exp, scale=SCALE), [pS], [pts])
                        for rr in range(4):
                            r = r4 * 4 + rr
                            P(lambda E: E.matmul(pa[:, 0:257], lhsT=pts[:, rr, :], rhs=kcb[:, r, :], start=first, stop=False), [pts, kh[r4]], [pa])
                            first = False
                pS = psS.next()
                c0 = NPR + 8 * b
                for kc in range(2):
                    P(lambda E: E.matmul(pS[0:8, 0:128], lhsT=CKVb[:, kc, c0:c0 + 8], rhs=QLAT[:, kc, b].rearrange("p h s -> p (h s)"),
                                         start=(kc == 0), stop=False), [CKVb, QLAT], [pS])
                P(lambda E: E.matmul(pS[0:8, 0:128], lhsT=KRb[64:96, c0:c0 + 8], rhs=QR[64:96, b].rearrange("p h s -> p (h s)"),
                                     start=False, stop=True), [KRb, QR], [pS])
                pn = PN.next()
                A(lambda E: E.activation(out=pn[:], in_=pS[0:8, 0:128], func=AF.Exp, scale=SCALE), [pS], [pn])
                V(lambda E: E.tensor_tensor(out=pn[:], in0=pn[:], in1=newmask[:], op=ALU.mult), [pn, newmask], [pn])
                P(lambda E: E.matmul(pa[:, 0:257], lhsT=pn[:], rhs=KCN[:, b, :], start=False, stop=True), [pn, KCN], [pa])
                rl = rlp.next()
                V(lambda E: E.reciprocal(out=rl[:], in_=pa[:, 256:257]), [pa], [rl])
                ol = OL.next()
                V(lambda E: E.tensor_scalar(out=ol[:], in0=pa[:, 0:256], scalar1=rl[:, 0:1], scalar2=None, op0=ALU.mult), [pa, rl], [ol])
                pR = psR.next()
                for kc in range(2):
                    P(lambda E: E.transpose(out=pR[:, kc, :], in_=ol[:, kc * 128:(kc + 1) * 128], identity=identb[:, :]), [ol, identb], [pR])
                A(lambda E: E.copy(out=OLT[:, :, b].rearrange("p k h s -> p k (h s)"), in_=pR[:, 0:2, :]), [pR], [OLT])
            obs = RR([s3.sb([64, 128], BF16) for _ in range(2)])
            for h in range(16):
                pS = psS.next()
                for kc in range(2):
                    P(lambda E: E.matmul(pS[0:64, 0:128], lhsT=WUVb[:, kc, h * 64:(h + 1) * 64], rhs=OLT[:, kc, :, h, :],
                                         start=(kc == 0), stop=(kc == 1)), [WUVb, OLT], [pS])
                ob = obs.next()
                A(lambda E: E.copy(out=ob[:], in_=pS[0:64, 0:128]), [pS], [ob])
                DS(lambda E: E.dma_start(out=X.OBT[h // 2, (h % 2) * 64:(h % 2) * 64 + 64, NPR:NT], in_=ob[:]), [ob], [X.OBT])


EGROUPS = [(16, [1, 2, 3, 4]), (528, [5, 6, 7, 8]), (1040, [9, 10, 11, 12]), (1552, [13, 14, 15, 16]), (2064, [17])]


def x_rows(X, ti):
    return X.I["xp"][(ti - 1) * 128: ti * 128, :] if ti <= 16 else X.I["xs"]


def stage_e(X):
    I, O, V, A, G, P, DS, DG = X.I, X.O, X.V, X.A, X.G, X.P, X.DS, X.DG
    with Stage(X) as st:
        WUA = st.sb([128, 8, D], BF16)
        WUB = st.sb([128, 8, D], BF16)
        for kc in range(8):
            DG(lambda E: E.dma_start(out=WUA[:, kc, :], in_=I["w_up_a"][kc * 128:(kc + 1) * 128, :]), (), [WUA])
            DG(lambda E: E.dma_start(out=WUB[:, kc, :], in_=I["w_up_b"][kc * 128:(kc + 1) * 128, :]), (), [WUB])
        OAg = RR([st.sb([128, 8, 512], BF16) for _ in range(2)])
        OBg = RR([st.sb([128, 8, 512], BF16) for _ in range(2)])
        MTg = RR([st.sb([128, 16, 512], BF16) for _ in range(2)])
        gat = RR([st.sb([128, 2, 512]) for _ in range(3)])
        mtmp = RR([st.sb([128, 2, 512]) for _ in range(2)])
        psm = RR([st.ps([128, 512]) for _ in range(4)])
        for (t0, tiles) in EGROUPS:
            tn = 128 * len(tiles)
            oa, ob, mt = OAg.next(), OBg.next(), MTg.next()
            DS(lambda E: E.dma_start(out=oa[:, :, 0:tn], in_=X.OAT[:, :, t0:t0 + tn].rearrange("j p t -> p j t")), [X.OAT], [oa])
            DS(lambda E: E.dma_start(out=ob[:, :, 0:tn], in_=X.OBT[:, :, t0:t0 + tn].rearrange("j p t -> p j t")), [X.OBT], [ob])
            for dt in range(16):
                gt = gat.next()
                DS(lambda E: E.dma_start(out=gt[:, 0, 0:tn], in_=X.PFM[CTI[f"ga{dt}"], :, t0:t0 + tn]), [X.PFM], [gt])
                DS(lambda E: E.dma_start(out=gt[:, 1, 0:tn], in_=X.PFM[CTI[f"gb{dt}"], :, t0:t0 + tn]), [X.PFM], [gt])
                pa, pb = psm.next(), psm.next()
                for kc in range(8):
                    P(lambda E: E.matmul(pa[:, 0:tn], lhsT=WUA[:, kc, dt * 128:(dt + 1) * 128], rhs=oa[:, kc, 0:tn], start=(kc == 0), stop=(kc == 7)),
                      [WUA, oa], [pa])
                for kc in range(8):
                    P(lambda E: E.matmul(pb[:, 0:tn], lhsT=WUB[:, kc, dt * 128:(dt + 1) * 128], rhs=ob[:, kc, 0:tn], start=(kc == 0), stop=(kc == 7)),
                      [WUB, ob], [pb])
                m_ = mtmp.next()
                V(lambda E: E.scalar_tensor_tensor(out=m_[:, 0, 0:tn], in0=gt[:, 0, 0:tn], scalar=1.0, in1=pa[:, 0:tn], op0=ALU.add, op1=ALU.mult),
                  [gt, pa], [m_])
                V(lambda E: E.scalar_tensor_tensor(out=m_[:, 1, 0:tn], in0=gt[:, 1, 0:tn], scalar=1.0, in1=pb[:, 0:tn], op0=ALU.add, op1=ALU.mult),
                  [gt, pb], [m_])
                G(lambda E: E.tensor_tensor(out=mt[:, dt, 0:tn], in0=m_[:, 0, 0:tn], in1=m_[:, 1, 0:tn], op=ALU.add), [m_], [mt])
            DS(lambda E: E.dma_start(out=X.MTD[:, :, t0:t0 + tn], in_=mt[:, :, 0:tn]), [mt], [X.MTD])
    with Stage(X) as st:
        identf = st.load(I["ident_f"], [128, 128])
        WO = st.sb([128, 16, D], BF16)
        for kc in range(16):
            DG(lambda E: E.dma_start(out=WO[:, kc, :], in_=I["w_o"][kc * 128:(kc + 1) * 128, :]), (), [WO])
        RWt = st.load(I["router_w"].rearrange("(kc p) c -> p kc c", p=128), [128, 16, 36])
        RB = st.load(I["router_b"].to_broadcast([128, 36]), [128, 36])
        g2 = st.load(I["norm_ffn_g"].to_broadcast([128, D]), [128, D])
        MTt = RR([st.sb([128, 16, 128], BF16) for _ in range(2)])
        xin = RR([st.sb([128, D]) for _ in range(2)])
        x2p = RR([st.sb([128, D]) for _ in range(2)])
        xn = RR([st.sb([128, D]) for _ in range(2)])
        junk = st.sb([128, D])
        XT32 = RR([st.sb([128, 16, 128]) for _ in range(2)])
        XTb = RR([st.sb([128, 16, 128], BF16) for _ in range(2)])
        sm = RR([st.sb([128, 64]) for _ in range(24)])
        cmb = RR([st.sb([128, 32]) for _ in range(2)])
        psm = RR([st.ps([128, 512]) for _ in range(4)])
        pst = RR([st.ps([128, 512]) for _ in range(4)])
        for (t0, tiles) in EGROUPS:
            for li, ti in enumerate(tiles):
                g0, n = TT[ti]
                mt = MTt.next()
                DS(lambda E: E.dma_start(out=mt[:], in_=X.MTD[:, :, g0:g0 + n]), [X.MTD], [mt])
                li = 0
                x = xin.next()
                DS(lambda E: E.dma_start(out=x[:], in_=x_rows(X, ti)), (), [x])
                x2 = x2p.next()
                for cc in range(4):
                    ps = psm.next()
                    for kc in range(16):
                        P(lambda E: E.matmul(ps[:, :], lhsT=mt[:, kc, li * 128:(li + 1) * 128], rhs=WO[:, kc, cc * 512:(cc + 1) * 512],
                                             start=(kc == 0), stop=(kc == 15)), [mt, WO], [ps])
                    V(lambda E: E.scalar_tensor_tensor(out=x2[:, cc * 512:(cc + 1) * 512], in0=ps[:, :], scalar=0.5,
                                                       in1=x[:, cc * 512:(cc + 1) * 512], op0=ALU.mult, op1=ALU.add), [ps, x], [x2])
                DS(lambda E: E.dma_start(out=X.X2[g0:g0 + n, :], in_=x2[:]), [x2], [X.X2])
                sq = sm.next()
                G(lambda E: E.tensor_tensor(out=junk[:], in0=x2[:], in1=x2[:], op=ALU.mult), [x2], [junk])
                V(lambda E: E.tensor_reduce(out=sq[:, 0:1], in_=junk[:], axis=AX.X, op=ALU.add), [junk], [sq])
                A(lambda E: E.activation(out=sq[:, 0:1], in_=sq[:, 0:1], func=AF.Sqrt, bias=1e-6, scale=1.0 / D), [sq], [sq])
                V(lambda E: E.reciprocal(out=sq[:, 0:1], in_=sq[:, 0:1]), [sq], [sq])
                xn2 = xn.next()
                V(lambda E: E.scalar_tensor_tensor(out=xn2[:], in0=x2[:], scalar=sq[:, 0:1], in1=g2[:], op0=ALU.mult, op1=ALU.mult),
                  [x2, sq, g2], [xn2])
                xt32, xtb = XT32.next(), XTb.next()
                for q4 in range(4):
                    ps = pst.next()
                    for a in range(4):
                        kc = q4 * 4 + a
                        P(lambda E: E.transpose(out=ps[:, a * 128:(a + 1) * 128], in_=xn2[:, kc * 128:(kc + 1) * 128], identity=identf[:, :]),
                          [xn2, identf], [ps])
                    A(lambda E: E.copy(out=xt32[:, q4 * 4:q4 * 4 + 4, :], in_=ps[:, :].rearrange("p (a t) -> p a t", a=4)), [ps], [xt32])
                G(lambda E: E.tensor_copy(out=xtb[:], in_=xt32[:]), [xt32], [xtb])
                DS(lambda E: E.dma_start(out=X.XN2T.t.rearrange("(kc p) t -> p kc t", p=128)[:, :, g0:g0 + n], in_=xtb[:]), [xtb], [X.XN2T])
                ps = psm.next()
                for kc in range(16):
                    P(lambda E: E.matmul(ps[:, 0:36], lhsT=xt32[:, kc, :], rhs=RWt[:, kc, :], start=(kc == 0), stop=(kc == 15)), [xt32, RWt], [ps])
                lg = sm.next()
                A(lambda E: E.copy(out=lg[:, 0:36], in_=ps[:, 0:36]), [ps], [lg])
                gl = lg[:, 0:4]
                el = lg[:, 4:36].rearrange("p (g e) -> p g e", g=4)
                mx, e4, s4, pr = sm.next(), sm.next(), sm.next(), sm.next()
                V(lambda E: E.tensor_reduce(out=mx[:, 0:1], in_=gl, axis=AX.X, op=ALU.max), [lg], [mx])
                V(lambda E: E.tensor_scalar(out=e4[:, 0:4], in0=gl, scalar1=mx[:, 0:1], scalar2=None, op0=ALU.subtract), [lg, mx], [e4])
                A(lambda E: E.activation(out=e4[:, 0:4], in_=e4[:, 0:4], func=AF.Exp), [e4], [e4])
                V(lambda E: E.tensor_reduce(out=s4[:, 0:1], in_=e4[:, 0:4], axis=AX.X, op=ALU.add), [e4], [s4])
                V(lambda E: E.reciprocal(out=s4[:, 0:1], in_=s4[:, 0:1]), [s4], [s4])
                V(lambda E: E.tensor_scalar(out=pr[:, 0:4], in0=e4[:, 0:4], scalar1=s4[:, 0:1], scalar2=None, op0=ALU.mult), [e4, s4], [pr])
                glb, mxb, oh = sm.next(), sm.next(), sm.next()
                V(lambda E: E.tensor_tensor(out=glb[:, 0:4], in0=gl, in1=RB[:, 0:4], op=ALU.add), [lg, RB], [glb])
                V(lambda E: E.tensor_reduce(out=mxb[:, 0:1], in_=glb[:, 0:4], axis=AX.X, op=ALU.max), [glb], [mxb])
                V(lambda E: E.tensor_scalar(out=oh[:, 0:4], in0=glb[:, 0:4], scalar1=mxb[:, 0:1], scalar2=None, op0=ALU.is_ge), [glb, mxb], [oh])
                pg, t4 = sm.next(), sm.next()
                V(lambda E: E.tensor_tensor(out=t4[:, 0:4], in0=pr[:, 0:4], in1=oh[:, 0:4], op=ALU.mult), [pr, oh], [t4])
                V(lambda E: E.tensor_reduce(out=pg[:, 0:1], in_=t4[:, 0:4], axis=AX.X, op=ALU.add), [t4], [pg])
                ohb = oh[:, 0:4].unsqueeze(2).to_broadcast([128, 4, 8])
                t32, ein, eb = sm.next(), sm.next(), sm.next()
                V(lambda E: E.tensor_tensor(out=t32[:, 0:32].rearrange("p (g e) -> p g e", g=4), in0=el, in1=ohb, op=ALU.mult), [lg, oh], [t32])
                V(lambda E: E.tensor_reduce(out=ein[:, 0:8], in_=t32[:, 0:32].rearrange("p (g e) -> p e g", g=4), axis=AX.X, op=ALU.add), [t32], [ein])
                t32b = sm.next()
                V(lambda E: E.tensor_tensor(out=t32b[:, 0:32].rearrange("p (g e) -> p g e", g=4),
                                            in0=RB[:, 4:36].rearrange("p (g e) -> p g e", g=4), in1=ohb, op=ALU.mult), [RB, oh], [t32b])
                V(lambda E: E.tensor_reduce(out=eb[:, 0:8], in_=t32b[:, 0:32].rearrange("p (g e) -> p e g", g=4), axis=AX.X, op=ALU.add), [t32b], [eb])
                V(lambda E: E.tensor_tensor(out=eb[:, 0:8], in0=eb[:, 0:8], in1=ein[:, 0:8], op=ALU.add), [eb, ein], [eb])
                m1, oh1, eb2, m2, oh2 = sm.next(), sm.next(), sm.next(), sm.next(), sm.next()
                V(lambda E: E.tensor_reduce(out=m1[:, 0:1], in_=eb[:, 0:8], axis=AX.X, op=ALU.max), [eb], [m1])
                V(lambda E: E.tensor_scalar(out=oh1[:, 0:8], in0=eb[:, 0:8], scalar1=m1[:, 0:1], scalar2=None, op0=ALU.is_ge), [eb, m1], [oh1])
                V(lambda E: E.scalar_tensor_tensor(out=eb2[:, 0:8], in0=oh1[:, 0:8], scalar=-1e30, in1=eb[:, 0:8], op0=ALU.mult, op1=ALU.add),
                  [oh1, eb], [eb2])
                V(lambda E: E.tensor_reduce(out=m2[:, 0:1], in_=eb2[:, 0:8], axis=AX.X, op=ALU.max), [eb2], [m2])
                V(lambda E: E.tensor_scalar(out=oh2[:, 0:8], in0=eb2[:, 0:8], scalar1=m2[:, 0:1], scalar2=None, op0=ALU.is_ge), [eb2, m2], [oh2])
                v1, v2, tt8 = sm.next(), sm.next(), sm.next()
                V(lambda E: E.tensor_tensor(out=tt8[:, 0:8], in0=ein[:, 0:8], in1=oh1[:, 0:8], op=ALU.mult), [ein, oh1], [tt8])
                V(lambda E: E.tensor_reduce(out=v1[:, 0:1], in_=tt8[:, 0:8], axis=AX.X, op=ALU.add), [tt8], [v1])
                V(lambda E: E.tensor_tensor(out=tt8[:, 8:16], in0=ein[:, 0:8], in1=oh2[:, 0:8], op=ALU.mult), [ein, oh2], [tt8])
                V(lambda E: E.tensor_reduce(out=v2[:, 0:1], in_=tt8[:, 8:16], axis=AX.X, op=ALU.add), [tt8], [v2])
                V(lambda E: E.tensor_tensor(out=v2[:, 0:1], in0=v2[:, 0:1], in1=v1[:, 0:1], op=ALU.subtract), [v2, v1], [v2])
                A(lambda E: E.activation(out=v2[:, 0:1], in_=v2[:, 0:1], func=AF.Exp), [v2], [v2])
                V(lambda E: E.tensor_scalar(out=v1[:, 0:1], in0=v2[:, 0:1], scalar1=1.0, scalar2=None, op0=ALU.add), [v2], [v1])
                V(lambda E: E.reciprocal(out=v1[:, 0:1], in_=v1[:, 0:1]), [v1], [v1])
                V(lambda E: E.tensor_tensor(out=v1[:, 0:1], in0=v1[:, 0:1], in1=pg[:, 0:1], op=ALU.mult), [v1, pg], [v1])
                V(lambda E: E.tensor_tensor(out=v2[:, 0:1], in0=v2[:, 0:1], in1=v1[:, 0:1], op=ALU.mult), [v2, v1], [v2])
                c8 = sm.next()
                V(lambda E: E.tensor_scalar(out=c8[:, 0:8], in0=oh1[:, 0:8], scalar1=v1[:, 0:1], scalar2=None, op0=ALU.mult), [oh1, v1], [c8])
                V(lambda E: E.scalar_tensor_tensor(out=c8[:, 0:8], in0=oh2[:, 0:8], scalar=v2[:, 0:1], in1=c8[:, 0:8], op0=ALU.mult, op1=ALU.add),
                  [oh2, v2, c8], [c8])
                cb = cmb.next()
                V(lambda E: E.tensor_tensor(out=cb[:, :].rearrange("p (g e) -> p g e", g=4), in0=c8[:, 0:8].unsqueeze(1).to_broadcast([128, 4, 8]),
                                            in1=ohb, op=ALU.mult), [c8, oh], [cb])
                DS(lambda E: E.dma_start(out=X.COMB[g0:g0 + n, :], in_=cb[:]), [cb], [X.COMB])


FGROUPS = [[1, 2, 3, 4, 5, 6], [7, 8, 9, 10, 11, 12], [13, 14, 15, 16, 17]]


def stage_f(X):
    I, O, V, A, G, P, DS, DG = X.I, X.O, X.V, X.A, X.G, X.P, X.DS, X.DG
    with Stage(X) as st:
        gf = st.load(I["norm_final_g"].to_broadcast([128, D]), [128, D])
        XTg = st.sb([128, 16, 768], BF16)
        YACC = [st.sb([128, D]) for _ in range(6)]
        CMB = st.sb([128, 6, 32])
        Wg = RR([st.sb([128, 16, 512], BF16) for _ in range(2)])
        Wu = RR([st.sb([128, 16, 512], BF16) for _ in range(2)])
        Wd = RR([st.sb([128, 4, D], BF16) for _ in range(2)])
        act = RR([st.sb([128, 4, 768], BF16) for _ in range(2)])
        sgp = RR([st.sb([128, 384]) for _ in range(3)])
        small = RR([st.sb([128, 1]) for _ in range(2)])
        x2f = RR([st.sb([128, D]) for _ in range(1)])
        psg = RR([st.ps([128, 512]) for _ in range(4)])
        psd = RR([st.ps([128, 512]) for _ in range(4)])
        for tiles in FGROUPS:
            t0 = TT[tiles[0]][0]
            nt_ = len(tiles)
            tn = 128 * nt_
            hn = tn // 2
            DS(lambda E: E.dma_start(out=XTg[:, :, 0:tn], in_=X.XN2T.t.rearrange("(p k) t -> p k t", k=16)[:, :, t0:t0 + tn]), [X.XN2T], [XTg])
            for li in range(nt_):
                DS(lambda E: E.dma_start(out=CMB[:, li, :], in_=X.COMB[t0 + li * 128:t0 + (li + 1) * 128, :]), [X.COMB], [CMB])
            for e in range(32):
                wg, wu, wd = Wg.next(), Wu.next(), Wd.next()
                DG(lambda E: E.dma_start(out=wg[:], in_=I["expert_w_gate"][e].rearrange("(p k) f -> p k f", k=16)), (), [wg])
                DG(lambda E: E.dma_start(out=wu[:], in_=I["expert_w_up"][e].rearrange("(p k) f -> p k f", k=16)), (), [wu])
                DG(lambda E: E.dma_start(out=wd[:], in_=I["expert_w_down"][e].rearrange("(p k) d -> p k d", k=4)), (), [wd])
                ac = act.next()
                for half in range(2):
                    hs = slice(half * hn, (half + 1) * hn)
                    for ft in range(4):
                        pg_, pu_ = psg.next(), psg.next()
                        for kc in range(16):
                            P(lambda E: E.matmul(pg_[:, 0:hn], lhsT=wg[:, kc, ft:512:4], rhs=XTg[:, kc, hs], start=(kc == 0), stop=(kc == 15)),
                              [wg, XTg], [pg_])
                        for kc in range(16):
                            P(lambda E: E.matmul(pu_[:, 0:hn], lhsT=wu[:, kc, ft:512:4], rhs=XTg[:, kc, hs], start=(kc == 0), stop=(kc == 15)),
                              [wu, XTg], [pu_])
                        sg = sgp.next()
                        A(lambda E: E.activation(out=sg[:, 0:hn], in_=pg_[:, 0:hn], func=AF.Silu), [pg_], [sg])
                        V(lambda E: E.tensor_tensor(out=ac[:, ft, hs], in0=sg[:, 0:hn], in1=pu_[:, 0:hn], op=ALU.mult), [sg, pu_], [ac])
                for li in range(nt_):
                    for cc in range(4):
                        pd = psd.next()
                        for ft in range(4):
                            P(lambda E: E.matmul(pd[:, :], lhsT=ac[:, ft, li * 128:(li + 1) * 128], rhs=wd[:, ft, cc * 512:(cc + 1) * 512],
                                                 start=(ft == 0), stop=(ft == 3)), [ac, wd], [pd])
                        ya = YACC[li]
                        if e == 0:
                            V(lambda E: E.tensor_scalar(out=ya[:, cc * 512:(cc + 1) * 512], in0=pd[:, :], scalar1=CMB[:, li, e:e + 1], scalar2=None,
                                                        op0=ALU.mult), [pd, CMB], [ya])
                        else:
                            V(lambda E: E.scalar_tensor_tensor(out=ya[:, cc * 512:(cc + 1) * 512], in0=pd[:, :], scalar=CMB[:, li, e:e + 1],
                                                               in1=ya[:, cc * 512:(cc + 1) * 512], op0=ALU.mult, op1=ALU.add), [pd, CMB, ya], [ya])
            for li, ti in enumerate(tiles):
                g0, n = TT[ti]
                ya = YACC[li]
                x2t = x2f.next()
                DS(lambda E: E.dma_start(out=x2t[:], in_=X.X2[g0:g0 + n, :]), [X.X2], [x2t])
                G(lambda E: E.tensor_tensor(out=ya[:], in0=ya[:], in1=x2t[:], op=ALU.add), [ya, x2t], [ya])
                G(lambda E: E.tensor_tensor(out=x2t[:], in0=ya[:], in1=ya[:], op=ALU.mult), [ya], [x2t])
                sq = small.next()
                V(lambda E: E.tensor_reduce(out=sq[:, 0:1], in_=x2t[:], axis=AX.X, op=ALU.add), [x2t], [sq])
                A(lambda E: E.activation(out=sq[:, 0:1], in_=sq[:, 0:1], func=AF.Sqrt, bias=1e-6, scale=1.0 / D), [sq], [sq])
                V(lambda E: E.reciprocal(out=sq[:, 0:1], in_=sq[:, 0:1]), [sq], [sq])
                V(lambda E: E.scalar_tensor_tensor(out=x2t[:], in0=ya[:], scalar=sq[:, 0:1], in1=gf[:], op0=ALU.mult, op1=ALU.mult),
                  [ya, sq, gf], [x2t])
                dst = O["y_prompt"][(ti - 1) * 128: ti * 128, :] if ti <= 16 else O["y_sample"]
                DS(lambda E: E.dma_start(out=dst, in_=x2t[:]), [x2t], [])


def shard_inputs(inputs, n_cores=8, n_pool=None):
    f = lambda a: np.ascontiguousarray(np.asarray(a))
    g = {k: np.asarray(v) for k, v in inputs.items()}
    col = lambda a: f(a.reshape(-1, 1))
    shared = {
        "meta": f(g["meta_tokens"]),
        "norm_mix_g": f(g["norm_mix_g"].reshape(1, D)), "w_in": f(g["w_in"][0]),
        "rwkv_mu": col(g["rwkv_mu"][0]), "rwkv_w0": col(g["rwkv_w0"][0]), "rwkv_w2": f(g["rwkv_w2"][0]),
        "rwkv_a0": col(g["rwkv_a0"][0]), "rwkv_a2": f(g["rwkv_a2"][0]), "rwkv_g2": f(g["rwkv_g2"][0]),
        "rwkv_k_k": col(g["rwkv_k_k"][0]), "rwkv_k_a": col(g["rwkv_k_a"][0]), "rwkv_r_k": col(g["rwkv_r_k"][0]),
        "rwkv_ln_g": col(g["rwkv_ln_g"][0]), "rwkv_ln_b": col(g["rwkv_ln_b"][0]),
        "mla_q_norm_g": col(g["mla_q_norm_g"][0]), "mla_w_uq": f(g["mla_w_uq"][0]),
        "mla_kv_norm_g": col(g["mla_kv_norm_g"][0]), "mla_w_uk": f(g["mla_w_uk"][0].reshape(256, 1024)),
        "mla_w_uv": f(g["mla_w_uv"][0].reshape(256, 1024)),
        "w_up_a": f(g["w_up_a"][0]), "w_up_b": f(g["w_up_b"][0]), "w_o": f(g["w_o"][0]),
        "norm_ffn_g": f(g["norm_ffn_g"].reshape(1, D)),
        "router_w": f(np.concatenate([g["router_group_w"][0], g["router_expert_w"][0]], axis=1)),
        "router_b": f(np.concatenate([g["router_group_b"][0], g["router_expert_b"][0]]).reshape(1, 36)),
        "expert_w_gate": f(g["expert_w_gate"][0]), "expert_w_up": f(g["expert_w_up"][0]),
        "expert_w_down": f(g["expert_w_down"][0]), "norm_final_g": f(g["norm_final_g"].reshape(1, D)),
        "cache_ckv": f(g["cache_ckv"][0]), "cache_krope": f(g["cache_krope"][0]),
    }
    shared.update(make_consts())
    maps = []
    for c in range(n_cores):
        m = dict(shared)
        m["xp"] = f(g["x_prompt"][c])
        m["xs"] = f(g["x_sample"][16 * c:16 * c + 16].reshape(NS, D))
        m["state_wkv"] = f(g["state_wkv"][0, 16 * c:16 * c + 16])
        m["state_shift"] = f(g["state_shift"][0, 16 * c:16 * c + 16])
        m["page_table"] = f(g["page_table"][16 * c:16 * c + 16]).astype(np.int32)
        maps.append(m)
    return maps


def kernel(**inputs):
    n_pool = int(np.asarray(inputs["cache_ckv"]).shape[1])
    nc, _ = build(n_pool=n_pool)
    maps = shard_inputs(inputs)
    res = run_bass_kernel_spmd(nc, maps, core_ids=list(range(8)))
    R = res.results
    cat = lambda k: np.stack([r[k] for r in R], axis=0)
    y_prompt = cat("y_prompt")
    y_sample = np.concatenate([r["y_sample"].reshape(16, 8, D) for r in R], axis=0)
    ckv_p = cat("ckv_p")[None]
    kr_p = cat("kr_p")[None]
    wkv_p = cat("wkv_p")[None]
    sh_p = np.concatenate([r["sh_p"] for r in R], axis=0)[None]
    ckv_s = np.concatenate([r["ckv_s"].reshape(16, 8, 256) for r in R], axis=0)[None]
    kr_s = np.concatenate([r["kr_s"].reshape(16, 8, 32) for r in R], axis=0)[None]
    wkv_s = np.concatenate([r["wkv_s"] for r in R], axis=0)[None]
    sh_s = np.concatenate([r["sh_s"] for r in R], axis=0)[None]
    return (y_prompt, y_sample, ckv_p, kr_p, wkv_p, sh_p, ckv_s, kr_s, wkv_s, sh_s)
```

```python
import contextlib
import numpy as np
import ml_dtypes
import concourse.bass as bass
import concourse.mybir as mybir
from concourse.bass_utils import run_bass_kernel_spmd

F32 = mybir.dt.float32
BF16 = mybir.dt.bfloat16
F32R = mybir.dt.float32r
I32 = mybir.dt.int32
AF = mybir.ActivationFunctionType
ALU = mybir.AluOpType
AX = mybir.AxisListType

NT = 2192
NPR = 2064
NS = 128
D = 2048
TT = [(0, 16)] + [(16 + 128 * i, 128) for i in range(16)] + [(2064, 128)]
GROUPS = [(0, 512), (512, 512), (1024, 512), (1536, 512), (2048, 144)]
RWKV_COLS = 3520
OFF_RWKV = 800
IN_COLS = 8416
SCALE = 96 ** -0.5


class Buf:
    __slots__ = ("writers", "readers")

    def __init__(self):
        self.writers = {}
        self.readers = {}


class T:
    __slots__ = ("t", "b")

    def __init__(self, t, b=None):
        self.t = t
        self.b = b if b is not None else Buf()

    def __getitem__(self, k):
        return self.t[k]


def _bufs(xs):
    out = []
    for x in xs:
        if x is None:
            continue
        out.append(x.b if isinstance(x, T) else x)
    return out


class _Rec:
    def __getattr__(self, name):
        return lambda *a, **k: (name, a, k)


_REC = _Rec()


def _replay(E, call):
    kw = call[2]
    if call[0] == "dma_start" and "allow_slow_non_contiguous" not in kw:
        kw = dict(kw, allow_slow_non_contiguous=True)
    return getattr(E, call[0])(*call[1], **kw)


class _Op:
    __slots__ = ("eng", "call", "needed", "key", "val", "is_dma")

    def __init__(self, eng, call, is_dma=False):
        self.eng, self.call, self.is_dma = eng, call, is_dma
        self.needed = False
        self.key = None
        self.val = None


class _Wait:
    __slots__ = ("op",)

    def __init__(self, op):
        self.op = op


class Sched:
    ENGS = ("sync", "scalar", "vector", "gpsimd", "tensor")
    EPOCH = 30000

    def __init__(self, nc):
        self.nc = nc
        self.streams = {e: [] for e in self.ENGS}
        self.sems = {}
        self.nsem = 0
        self.ninst = 0
        self.fp32r = False
        self.last = {e: None for e in self.ENGS}
        self.ndma = {e: 0 for e in self.ENGS}
        self.n_dma_sems = 8
        self.dma_slot_last = {}

    def _alloc(self, key):
        h = self.nc.alloc_semaphore(name=f"s{self.nsem}")
        self.nsem += 1
        self.sems[key] = h
        return key

    def _wait(self, eng, op):
        if op.eng == eng and eng == "tensor" and not op.is_dma:
            return
        op.needed = True
        self.streams[eng].append(_Wait(op))

    def _deps(self, eng, reads, writes):
        for b in reads:
            for d in b.writers.values():
                self._wait(eng, d)
        for b in writes:
            for d in b.writers.values():
                self._wait(eng, d)
            for d in b.readers.values():
                self._wait(eng, d)

    def _mark(self, op, tag, reads, writes):
        for b in reads:
            b.readers[tag] = op
        for b in writes:
            b.writers = {tag: op}
            b.readers = {}

    def op(self, eng, fn, reads=(), writes=()):
        reads = _bufs(reads)
        writes = _bufs(writes)
        self._deps(eng, reads, writes)
        call = fn(_REC)
        if self.fp32r and call[0] == "matmul":
            kw = dict(call[2])
            for k in ("lhsT", "rhs"):
                if kw[k].dtype == F32:
                    kw[k] = kw[k].bitcast(F32R)
            call = (call[0], call[1], kw)
        o = _Op(eng, call)
        self.streams[eng].append(o)
        self.last[eng] = o
        self._mark(o, eng, reads, writes)
        self.ninst += 1
        return o

    def dma(self, eng, fn, reads=(), writes=()):
        reads = _bufs(reads)
        writes = _bufs(writes)
        self._deps(eng, reads, writes)
        slot = self.ndma[eng] % self.n_dma_sems
        self.ndma[eng] += 1
        prev = self.dma_slot_last.get((eng, slot))
        if prev is not None:
            self._wait(eng, prev)
        o = _Op(eng, fn(_REC), is_dma=True)
        o.key = f"{eng}_dma{slot}"
        o.needed = True
        self.dma_slot_last[(eng, slot)] = o
        self.streams[eng].append(o)
        self._mark(o, o.key, reads, writes)
        self.ninst += 1
        return o

    def barrier(self):
        deps = [o for o in self.last.values() if o is not None] + list(self.dma_slot_last.values())
        for e in self.ENGS:
            for d in deps:
                if not (d.eng == e and not d.is_dma):
                    d.needed = True
                    self.streams[e].append(_Wait(d))

    def finish(self):
        self.barrier()
        cnt = {}
        for e in self.ENGS:
            key = None
            for it in self.streams[e]:
                if isinstance(it, _Wait):
                    continue
                if it.is_dma:
                    if it.key not in self.sems:
                        self._alloc(it.key)
                    cnt[it.key] = cnt.get(it.key, 0) + 16
                    it.val = cnt[it.key]
                elif it.needed:
                    if key is None or cnt[key] >= self.EPOCH:
                        key = self._alloc(f"{e}_ep{self.nsem}")
                        cnt[key] = 0
                    cnt[key] += 1
                    it.key, it.val = key, cnt[key]
        nc = self.nc
        sems = self.sems

        def emit(E, stream):
            waited = {}
            for it in stream:
                if isinstance(it, _Wait):
                    o = it.op
                    if waited.get(o.key, 0) >= o.val:
                        continue
                    waited[o.key] = o.val
                    E.wait_ge(sems[o.key], o.val)
                elif it.is_dma:
                    _replay(E, it.call).then_inc(sems[it.key], 16)
                elif it.needed:
                    _replay(E, it.call).then_inc(sems[it.key], 1)
                else:
                    _replay(E, it.call)

        st = self.streams
        with nc.Block() as block:
            @block.sync
            def _(E):
                emit(E, st["sync"])

            @block.scalar
            def _(E):
                emit(E, st["scalar"])

            @block.vector
            def _(E):
                emit(E, st["vector"])

            @block.gpsimd
            def _(E):
                emit(E, st["gpsimd"])

            @block.tensor
            def _(E):
                emit(E, st["tensor"])


def make_consts():
    c = {}
    c["ident_f"] = np.eye(128, dtype=np.float32)
    c["ident_b"] = np.eye(128, dtype=np.float32).astype(ml_dtypes.bfloat16)
    half = 16
    inv = (np.float32(10000.0) ** (-np.arange(half, dtype=np.float32) / np.float32(half))).astype(np.float32)
    pos = np.concatenate([np.arange(NPR), 16384 + np.tile(np.arange(8), 16)]).astype(np.float32)
    ang = (pos[:, None] * inv[None, :]).astype(np.float32)
    cos = np.cos(ang).astype(np.float32).T
    sin = np.sin(ang).astype(np.float32).T
    rc = np.zeros((128, NT), np.float32)
    rs = np.zeros((128, NT), np.float32)
    for p in range(128):
        j = p % 16
        rc[p] = cos[j]
        rs[p] = -sin[j] if (p % 32) < 16 else sin[j]
    c["rope_c"] = rc
    c["rope_s"] = rs
    i = np.arange(128)
    le = (i[:, None] <= i[None, :]).astype(np.float32)
    lt = (i[:, None] < i[None, :]).astype(np.float32)
    blk = ((i[:, None] // 8) == (i[None, :] // 8)).astype(np.float32)
    c["m_le"] = le
    c["m_lt"] = lt
    c["m_gt"] = lt.T.copy()
    c["m_le_s"] = le * blk
    c["m_lt_s"] = lt * blk
    c["m_gt_s"] = lt.T * blk
    c["m_le_b"] = le.astype(ml_dtypes.bfloat16)
    rm = np.ones((NT,), np.float32)
    for (s0, n) in TT[:-1]:
        rm[s0] = 0.0
    rm[NPR::8] = 0.0
    c["reset"] = np.broadcast_to(rm[None, :], (128, NT)).copy()
    bo = np.zeros((128, 128), np.float32)
    bo[:64, :64] = 1.0
    bo[64:, 64:] = 1.0
    c["blockones"] = bo
    c["ones_f"] = np.ones((128, 128), np.float32)
    cm = np.zeros((128, 16, 128), np.float32)
    for b in range(16):
        cm[:, b, b * 8:(b + 1) * 8] = 1.0
    c["colmask"] = cm.reshape(128, 16 * 128)
    rmk = np.zeros((128, 16), np.float32)
    for b in range(16):
        rmk[b * 8:(b + 1) * 8, b] = 1.0
    c["rowmask"] = rmk
    nm = np.zeros((8, 16, 8), np.float32)
    for sk in range(8):
        nm[sk, :, sk:] = 1.0
    c["newmask"] = nm.reshape(8, 128).astype(ml_dtypes.bfloat16)
    return c


CONST_DT = {"ident_b": BF16, "m_le_b": BF16, "newmask": BF16}

def col_tiles():
    ct = []
    for i in range(4):
        ct.append((f"q{i}", [(0, 128 * i, 128)], 128, "copy"))
    for i in range(2):
        ct.append((f"kv{i}", [(0, 512 + 128 * i, 128)], 128, "copy"))
    ct.append(("kr", [(64, 768, 32)], 96, "copy"))
    ct.append(("krs", [(64, 784, 16), (80, 768, 16)], 96, "copy"))
    for nm, off in (("r", 800), ("k", 1824), ("v", 2848)):
        for i in range(8):
            ct.append((f"{nm}{i}", [(0, off + 128 * i, 128)], 128, "copy"))
    ct.append(("wl", [(0, 3872, 96)], 96, "copy"))
    ct.append(("al", [(0, 3968, 96)], 96, "copy"))
    ct.append(("gl0", [(0, 4064, 128)], 128, "copy"))
    ct.append(("gl1", [(0, 4192, 128)], 128, "copy"))
    for i in range(16):
        ct.append((f"ga{i}", [(0, 4320 + 128 * i, 128)], 128, "sig"))
    for i in range(16):
        ct.append((f"gb{i}", [(0, 6368 + 128 * i, 128)], 128, "sig"))
    return ct


CT = col_tiles()
CTI = {c[0]: i for i, c in enumerate(CT)}

IN_SPECS = [
    ("xp", [2048, D], F32), ("xs", [NS, D], F32), ("meta", [16, D], F32),
    ("state_wkv", [16, 16, 64, 64], F32), ("state_shift", [16, RWKV_COLS], F32),
    ("page_table", [16, 128], I32),
    ("norm_mix_g", [1, D], F32), ("w_in", [D, IN_COLS], F32), ("rwkv_mu", [RWKV_COLS, 1], F32),
    ("rwkv_w0", [1024, 1], F32), ("rwkv_w2", [96, 1024], F32), ("rwkv_a0", [1024, 1], F32),
    ("rwkv_a2", [96, 1024], F32), ("rwkv_g2", [256, 1024], F32), ("rwkv_k_k", [1024, 1], F32),
    ("rwkv_k_a", [1024, 1], F32), ("rwkv_r_k", [1024, 1], F32), ("rwkv_ln_g", [1024, 1], F32),
    ("rwkv_ln_b", [1024, 1], F32), ("mla_q_norm_g", [512, 1], F32), ("mla_w_uq", [512, 1536], F32),
    ("mla_kv_norm_g", [256, 1], F32), ("mla_w_uk", [256, 1024], F32), ("mla_w_uv", [256, 1024], F32),
    ("w_up_a", [1024, D], F32), ("w_up_b", [1024, D], F32), ("w_o", [D, D], F32),
    ("norm_ffn_g", [1, D], F32), ("router_w", [D, 36], F32), ("router_b", [1, 36], F32),
    ("expert_w_gate", [32, D, 512], F32), ("expert_w_up", [32, D, 512], F32),
    ("expert_w_down", [32, 512, D], F32), ("norm_final_g", [1, D], F32),
]
OUT_SPECS = [
    ("y_prompt", [2048, D]), ("y_sample", [NS, D]), ("ckv_p", [NPR, 256]), ("kr_p", [NPR, 32]),
    ("wkv_p", [16, 64, 64]), ("sh_p", [1, RWKV_COLS]), ("ckv_s", [NS, 256]), ("kr_s", [NS, 32]),
    ("wkv_s", [16, 16, 64, 64]), ("sh_s", [16, RWKV_COLS]),
]


class Ctx:
    pass


def build(n_pool=20480, upto="all", debug=(), start=None, feed=()):
    nc = bass.Bass("TRN2", target_bir_lowering=False)
    S = Sched(nc)
    X = Ctx()
    X.nc, X.S = nc, S
    X.debug, X.upto = debug, upto
    import os as _os
    X.nct = int(_os.environ.get('KB_NCT', '1000'))
    X.use_fp32r = _os.environ.get('KB_FP32R', '1') == '1'
    I = {}
    for name, shape, dt in IN_SPECS:
        I[name] = nc.dram_tensor(name, shape, dt, kind="ExternalInput").ap()
    I["cache_ckv"] = nc.dram_tensor("cache_ckv", [n_pool, 128, 256], F32, kind="ExternalInput").ap()
    I["cache_krope"] = nc.dram_tensor("cache_krope", [n_pool, 128, 32], F32, kind="ExternalInput").ap()
    consts = make_consts()
    for k, v in consts.items():
        I[k] = nc.dram_tensor(k, list(v.shape), CONST_DT.get(k, F32), kind="ExternalInput").ap()
    O = {}
    for name, shape in OUT_SPECS:
        O[name] = nc.dram_tensor(name, shape, F32, kind="ExternalOutput").ap()
    X.I, X.O = I, O

    def scratch(name, shape, dt):
        kind = "ExternalOutput" if name in debug else ("ExternalInput" if name in feed else "Internal")
        return T(nc.dram_tensor(name, shape, dt, kind=kind).ap())
    X.scratch = scratch

    X.V = lambda fn, r=(), w=(): S.op("vector", fn, r, w)
    X.A = lambda fn, r=(), w=(): S.op("scalar", fn, r, w)
    X.G = lambda fn, r=(), w=(): S.op("gpsimd", fn, r, w)
    X.P = lambda fn, r=(), w=(): S.op("tensor", fn, r, w)
    X.DS = lambda fn, r=(), w=(): S.dma("sync", fn, r, w)
    X.DG = lambda fn, r=(), w=(): S.dma("gpsimd", fn, r, w)

    X.PFM = scratch("PFM", [len(CT), 128, NT], F32)
    X.RW = scratch("RW", [8, 8, 128, NT], F32)
    X.OAT = scratch("OAT", [8, 128, NT], BF16)
    X.OBT = scratch("OBT", [8, 128, NT], BF16)
    X.X2 = scratch("X2", [NT, D], F32)
    X.XN2T = scratch("XN2T", [D, NT], BF16)
    X.COMB = scratch("COMB", [NT, 32], F32)
    X.MTD = scratch("MTD", [128, 16, NT], BF16)

    stages = [stage_ab, stage_c1, stage_c2, stage_d, stage_e, stage_f]
    started = start is None
    for st in stages:
        if not started:
            if st.__name__ != start:
                continue
            started = True
        st(X)
        S.barrier()
        if upto == st.__name__:
            break
    S.finish()
    return nc, consts


class Stage:
    N = 0

    def __init__(self, X):
        self.X = X
        self.es = contextlib.ExitStack()
        self.n = 0

    def __enter__(self):
        self.es.__enter__()
        return self

    def __exit__(self, *a):
        self.X.S.barrier()
        return self.es.__exit__(*a)

    def sb(self, shape, dt=F32, name=None):
        Stage.N += 1
        return T(self.es.enter_context(self.X.nc.sbuf_tensor(f"{name or 'sb'}_{Stage.N}", list(shape), dt)))

    def ps(self, shape, dt=F32, name=None):
        Stage.N += 1
        return T(self.es.enter_context(self.X.nc.psum_tensor(f"{name or 'ps'}_{Stage.N}", list(shape), dt)))

    def load(self, ap_dram, shape, dt=F32, eng="sync", **kw):
        t = self.sb(shape, dt)
        (self.X.DS if eng == "sync" else self.X.DG)(lambda E: E.dma_start(out=t[:], in_=ap_dram, **kw), (), [t])
        return t


class RR:
    def __init__(self, items):
        self.items = items
        self.i = 0

    def next(self):
        x = self.items[self.i % len(self.items)]
        self.i += 1
        return x


def stage_ab(X):
    I, V, A, G, P, DS, DG = X.I, X.V, X.A, X.G, X.P, X.DS, X.DG
    with Stage(X) as st:
        hT = st.sb([128, 16, NT], BF16, "hT")
        identb = st.load(I["ident_b"], [128, 128], BF16)
        with Stage(X) as sa:
            gm = sa.load(I["norm_mix_g"].to_broadcast([128, D]), [128, D])
            xin = RR([sa.sb([128, D]) for _ in range(2)])
            junk = sa.sb([128, D])
            ssq = RR([sa.sb([128, 1]) for _ in range(2)])
            hb = RR([sa.sb([128, D], BF16) for _ in range(2)])
            pst = RR([sa.ps([128, 16, 128], BF16) for _ in range(2)])
            for ti, (g0, n) in enumerate(TT):
                x = xin.next()
                if ti == 0:
                    src = I["meta"]
                elif ti <= 16:
                    src = I["xp"][(ti - 1) * 128: ti * 128, :]
                else:
                    src = I["xs"]
                DS(lambda E, x=x, src=src, n=n: E.dma_start(out=x[0:n, :], in_=src), (), [x])
                sq = ssq.next()
                G(lambda E, x=x, n=n: E.tensor_tensor(out=junk[0:n, :], in0=x[0:n, :], in1=x[0:n, :], op=ALU.mult),
                  [x], [junk])
                V(lambda E, sq=sq, n=n: E.tensor_reduce(out=sq[0:n, :], in_=junk[0:n, :], axis=AX.X, op=ALU.add),
                  [junk], [sq])
                A(lambda E, sq=sq, n=n: E.activation(out=sq[0:n, :], in_=sq[0:n, :], func=AF.Sqrt,
                                                     bias=1e-6, scale=1.0 / D), [sq], [sq])
                V(lambda E, sq=sq, n=n: E.reciprocal(out=sq[0:n, :], in_=sq[0:n, :]), [sq], [sq])
                h = hb.next()
                V(lambda E, x=x, sq=sq, h=h, n=n: E.scalar_tensor_tensor(
                    out=h[0:n, :], in0=x[0:n, :], scalar=sq[0:n, 0:1], in1=gm[0:n, :],
                    op0=ALU.mult, op1=ALU.mult), [x, sq, gm], [h])
                pt = pst.next()
                for kc in range(16):
                    P(lambda E, h=h, pt=pt, kc=kc, n=n: E.transpose(
                        out=pt[:, kc, 0:n], in_=h[0:n, kc * 128:(kc + 1) * 128], identity=identb[0:n, 0:n]),
                      [h, identb], [pt])
                A(lambda E, pt=pt, g0=g0, n=n: E.copy(out=hT[:, :, g0:g0 + n], in_=pt[:, :, 0:n]), [pt], [hT])
        X.S.barrier()
        if "HT" in X.debug:
            HTd = X.scratch("HT", [128, 16, NT], BF16)
            DS(lambda E: E.dma_start(out=HTd[:], in_=hT[:]), [hT], [HTd])
        if X.upto == "A":
            return
        with Stage(X) as sb_:
            wb = RR([sb_.sb([128, 16, 128], BF16) for _ in range(3)])
            stg = RR([sb_.sb([128, NT]) for _ in range(2)])
            pss = RR([sb_.ps([128, 512]) for _ in range(4)])
            w_in = I["w_in"].rearrange("(kc p) c -> p kc c", p=128)
            for ci, (name, pieces, M, kind) in enumerate(CT):
                if ci >= X.nct:
                    break
                w = wb.next()
                if pieces[0][0] != 0:
                    G(lambda E, w=w: E.memset(w[:, :, 0:64], 0.0), (), [w])
                for (dc, sc, n) in pieces:
                    DG(lambda E, w=w, dc=dc, sc=sc, n=n: E.dma_start(out=w[:, :, dc:dc + n], in_=w_in[:, :, sc:sc + n]),
                       (), [w])
                sg = stg.next()
                for (t0, tn) in GROUPS:
                    ps = pss.next()
                    for kc in range(16):
                        P(lambda E, ps=ps, w=w, kc=kc, t0=t0, tn=tn, M=M: E.matmul(
                            ps[0:M, 0:tn], lhsT=w[:, kc, 0:M], rhs=hT[:, kc, t0:t0 + tn],
                            start=(kc == 0), stop=(kc == 15)), [w], [ps])
                    func, scl = (AF.Tanh, 0.5) if kind == "sig" else (AF.Copy, 1.0)
                    A(lambda E, ps=ps, sg=sg, t0=t0, tn=tn, M=M, func=func, scl=scl: E.activation(
                        out=sg[0:M, t0:t0 + tn], in_=ps[0:M, 0:tn], func=func, scale=scl), [ps], [sg])
                DS(lambda E, sg=sg, ci=ci, M=M: E.dma_start(out=X.PFM[ci, 0:M, :], in_=sg[0:M, :]), [sg], [X.PFM])


NLW = -0.30326533


def stage_c1(X):
    I, O, V, A, G, P, DS, DG = X.I, X.O, X.V, X.A, X.G, X.P, X.DS, X.DG
    with Stage(X) as st:
        blockones = st.load(I["blockones"], [128, 128])
        reset = st.load(I["reset"], [128, NT])
        tw = st.sb([128, NT])
        xa = st.sb([128, NT])
        sg = [st.sb([128, NT]), st.sb([128, NT])]
        pbuf = RR([st.sb([128, NT]) for _ in range(2)])
        dbuf = RR([st.sb([128, NT]) for _ in range(2)])
        small = RR([st.sb([128, 32]) for _ in range(8)])
        pss = RR([st.ps([128, 512]) for _ in range(6)])

        def colvec(name, c0, M):
            t = small.next()
            DS(lambda E: E.dma_start(out=t[0:M, 0:1], in_=I[name][c0:c0 + M, :]), (), [t])
            return t

        def xs_tile(name, out):
            ci = CTI[name]
            _, pieces, M, _ = CT[ci]
            rc0 = pieces[0][1] - OFF_RWKV
            p = pbuf.next()
            DS(lambda E: E.dma_start(out=p[0:M, :], in_=X.PFM[ci, 0:M, :]), [X.PFM], [p])
            mu = colvec("rwkv_mu", rc0, M)
            sh = small.next()
            DS(lambda E: E.dma_start(out=sh[0:M, 0:16], in_=I["state_shift"][:, rc0:rc0 + M].rearrange("b c -> c b"),
                                     allow_slow_non_contiguous=True), (), [sh])
            DS(lambda E: E.dma_start(out=O["sh_p"][0:1, rc0:rc0 + M].rearrange("o c -> c o"), in_=p[0:M, NPR - 1:NPR],
                                     allow_slow_non_contiguous=True), [p], [])
            ps_ = p[0:M, NPR:NT].rearrange("p (b s) -> p b s", s=8)
            DS(lambda E: E.dma_start(out=O["sh_s"][:, rc0:rc0 + M].rearrange("b c -> c b"), in_=ps_[:, :, 7],
                                     allow_slow_non_contiguous=True), [p], [])
            d = dbuf.next()
            ds_ = d[0:M, NPR:NT].rearrange("p (b s) -> p b s", s=8)
            G(lambda E: E.tensor_tensor(out=d[0:M, 1:NPR], in0=p[0:M, 0:NPR - 1], in1=p[0:M, 1:NPR], op=ALU.subtract), [p], [d])
            V(lambda E: E.tensor_scalar(out=d[0:M, 0:1], in0=p[0:M, 0:1], scalar1=-1.0, scalar2=None, op0=ALU.mult), [p], [d])
            V(lambda E: E.tensor_tensor(out=ds_[:, :, 1:8], in0=ps_[:, :, 0:7], in1=ps_[:, :, 1:8], op=ALU.subtract), [p], [d])
            V(lambda E: E.tensor_tensor(out=ds_[:, :, 0], in0=sh[0:M, 0:16], in1=ps_[:, :, 0], op=ALU.subtract), [p, sh], [d])
            V(lambda E: E.scalar_tensor_tensor(out=out[0:M, :], in0=d[0:M, :], scalar=mu[0:M, 0:1], in1=p[0:M, :],
                                               op0=ALU.mult, op1=ALU.add), [d, mu, p], [out])

        xs_tile("wl", tw)
        A(lambda E: E.activation(out=tw[0:96, :], in_=tw[0:96, :], func=AF.Tanh), [tw], [tw])
        xs_tile("al", xa)
        for i in range(2):
            xs_tile(f"gl{i}", sg[i])
            A(lambda E, i=i: E.activation(out=sg[i][:], in_=sg[i][:], func=AF.Tanh, scale=0.5), [sg[i]], [sg[i]])
            V(lambda E, i=i: E.tensor_scalar(out=sg[i][:], in0=sg[i][:], scalar1=0.5, scalar2=0.5, op0=ALU.mult, op1=ALU.add),
              [sg[i]], [sg[i]])
        w2 = st.load(I["rwkv_w2"], [96, 1024])
        a2 = st.load(I["rwkv_a2"], [96, 1024])
        g2 = st.load(I["rwkv_g2"].rearrange("(kc p) c -> p kc c", p=128), [128, 2, 1024])
        big = RR([st.sb([128, NT]) for _ in range(12)])
        for j in range(8):
            c0 = j * 128
            xr, xk, xv = big.next(), big.next(), big.next()
            xs_tile(f"r{j}", xr)
            xs_tile(f"k{j}", xk)
            xs_tile(f"v{j}", xv)
            w0 = colvec("rwkv_w0", c0, 128)
            a0 = colvec("rwkv_a0", c0, 128)
            V(lambda E, w0=w0: E.tensor_scalar(out=w0[:, 0:1], in0=w0[:, 0:1], scalar1=0.5, scalar2=None, op0=ALU.mult), [w0], [w0])
            V(lambda E, a0=a0: E.tensor_scalar(out=a0[:, 0:1], in0=a0[:, 0:1], scalar1=0.5, scalar2=None, op0=ALU.mult), [a0], [a0])
            kkv = colvec("rwkv_k_k", c0, 128)
            kav = colvec("rwkv_k_a", c0, 128)
            rkv = colvec("rwkv_r_k", c0, 128)
            logw, alpha, gg = big.next(), big.next(), big.next()
            for (t0, tn) in GROUPS:
                ps = pss.next()
                P(lambda E, ps=ps, t0=t0, tn=tn: E.matmul(ps[:, 0:tn], lhsT=w2[0:96, c0:c0 + 128], rhs=tw[0:96, t0:t0 + tn],
                                                          start=True, stop=True), [w2, tw], [ps])
                A(lambda E, ps=ps, t0=t0, tn=tn: E.activation(out=logw[:, t0:t0 + tn], in_=ps[:, 0:tn], func=AF.Tanh,
                                                              bias=w0[:, 0:1], scale=0.5), [ps, w0], [logw])
                ps = pss.next()
                P(lambda E, ps=ps, t0=t0, tn=tn: E.matmul(ps[:, 0:tn], lhsT=a2[0:96, c0:c0 + 128], rhs=xa[0:96, t0:t0 + tn],
                                                          start=True, stop=True), [a2, xa], [ps])
                A(lambda E, ps=ps, t0=t0, tn=tn: E.activation(out=alpha[:, t0:t0 + tn], in_=ps[:, 0:tn], func=AF.Tanh,
                                                              bias=a0[:, 0:1], scale=0.5), [ps, a0], [alpha])
                ps = pss.next()
                for kc in range(2):
                    P(lambda E, ps=ps, t0=t0, tn=tn, kc=kc: E.matmul(ps[:, 0:tn], lhsT=g2[:, kc, c0:c0 + 128],
                                                                     rhs=sg[kc][:, t0:t0 + tn], start=(kc == 0), stop=(kc == 1)),
                      [g2, sg[kc]], [ps])
                A(lambda E, ps=ps, t0=t0, tn=tn: E.copy(out=gg[:, t0:t0 + tn], in_=ps[:, 0:tn]), [ps], [gg])
            V(lambda E: E.tensor_scalar(out=logw[:], in0=logw[:], scalar1=NLW, scalar2=NLW, op0=ALU.mult, op1=ALU.add), [logw], [logw])
            G(lambda E: E.tensor_scalar(out=alpha[:], in0=alpha[:], scalar1=0.5, scalar2=0.5, op0=ALU.mult, op1=ALU.add), [alpha], [alpha])
            kk, sq, rs = big.next(), big.next(), big.next()
            V(lambda E: E.tensor_scalar(out=kk[:], in0=xk[:], scalar1=kkv[:, 0:1], scalar2=None, op0=ALU.mult), [xk, kkv], [kk])
            G(lambda E: E.tensor_tensor(out=sq[:], in0=kk[:], in1=kk[:], op=ALU.mult), [kk], [sq])
            for (t0, tn) in GROUPS:
                ps = pss.next()
                P(lambda E, ps=ps, t0=t0, tn=tn: E.matmul(ps[:, 0:tn], lhsT=blockones[:], rhs=sq[:, t0:t0 + tn], start=True, stop=True),
                  [blockones, sq], [ps])
                V(lambda E, ps=ps, t0=t0, tn=tn: E.tensor_scalar(out=rs[:, t0:t0 + tn], in0=ps[:, 0:tn], scalar1=1e-24, scalar2=None,
                                                                 op0=ALU.max), [ps], [rs])
            A(lambda E: E.activation(out=rs[:], in_=rs[:], func=AF.Sqrt), [rs], [rs])
            V(lambda E: E.reciprocal(out=rs[:], in_=rs[:]), [rs], [rs])
            V(lambda E: E.tensor_tensor(out=kk[:], in0=kk[:], in1=rs[:], op=ALU.mult), [kk, rs], [kk])
            kmod = big.next()
            V(lambda E: E.tensor_scalar(out=kmod[:], in0=alpha[:], scalar1=-1.0, scalar2=kav[:, 0:1], op0=ALU.add, op1=ALU.mult),
              [alpha, kav], [kmod])
            V(lambda E: E.scalar_tensor_tensor(out=kmod[:], in0=kmod[:], scalar=1.0, in1=xk[:], op0=ALU.add, op1=ALU.mult),
              [kmod, xk], [kmod])
            cum = big.next()
            V(lambda E: E.tensor_tensor_scan(out=cum[:], data0=reset[:], data1=logw[:], initial=0.0, op0=ALU.mult, op1=ALU.add),
              [reset, logw], [cum])
            ew, ewm, ewi = big.next(), sq, rs
            A(lambda E: E.activation(out=ew[:], in_=cum[:], func=AF.Exp), [cum], [ew])
            A(lambda E: E.activation(out=ewi[:], in_=cum[:], func=AF.Exp, scale=-1.0), [cum], [ewi])
            G(lambda E: E.tensor_tensor(out=ewm[:], in0=cum[:], in1=logw[:], op=ALU.subtract), [cum, logw], [ewm])
            A(lambda E: E.activation(out=ewm[:], in_=ewm[:], func=AF.Exp), [ewm], [ewm])
            RWj = lambda a: X.RW[j, a, :, :]
            V(lambda E: E.scalar_tensor_tensor(out=ewm[:], in0=kk[:], scalar=-1.0, in1=ewm[:], op0=ALU.mult, op1=ALU.mult), [kk, ewm], [ewm])
            DS(lambda E: E.dma_start(out=RWj(0), in_=ewm[:]), [ewm], [X.RW])
            V(lambda E: E.scalar_tensor_tensor(out=cum[:], in0=xr[:], scalar=rkv[:, 0:1], in1=kmod[:], op0=ALU.mult, op1=ALU.mult),
              [xr, rkv, kmod], [cum])
            G(lambda E: E.tensor_tensor(out=xr[:], in0=xr[:], in1=ew[:], op=ALU.mult), [xr, ew], [xr])
            DS(lambda E: E.dma_start(out=RWj(1), in_=xr[:]), [xr], [X.RW])
            G(lambda E: E.tensor_tensor(out=kk[:], in0=kk[:], in1=alpha[:], op=ALU.mult), [kk, alpha], [kk])
            V(lambda E: E.tensor_tensor(out=kk[:], in0=kk[:], in1=ewi[:], op=ALU.mult), [kk, ewi], [kk])
            DS(lambda E: E.dma_start(out=RWj(2), in_=kk[:]), [kk], [X.RW])
            G(lambda E: E.tensor_tensor(out=kmod[:], in0=kmod[:], in1=ewi[:], op=ALU.mult), [kmod, ewi], [kmod])
            DS(lambda E: E.dma_start(out=RWj(3), in_=kmod[:]), [kmod], [X.RW])
            DS(lambda E: E.dma_start(out=RWj(4), in_=xv[:]), [xv], [X.RW])
            for (t0, tn) in GROUPS:
                ps = pss.next()
                P(lambda E, ps=ps, t0=t0, tn=tn: E.matmul(ps[:, 0:tn], lhsT=blockones[:], rhs=cum[:, t0:t0 + tn], start=True, stop=True),
                  [blockones, cum], [ps])
                V(lambda E, ps=ps, t0=t0, tn=tn: E.tensor_tensor(out=logw[:, t0:t0 + tn], in0=ps[:, 0:tn], in1=xv[:, t0:t0 + tn],
                                                                 op=ALU.mult), [ps, xv], [logw])
            DS(lambda E: E.dma_start(out=RWj(5), in_=logw[:]), [logw], [X.RW])
            DS(lambda E: E.dma_start(out=RWj(6), in_=gg[:]), [gg], [X.RW])
            DS(lambda E: E.dma_start(out=RWj(7), in_=ew[:]), [ew], [X.RW])


def stage_c2(X):
    R = (lambda ap: ap.bitcast(F32R)) if X.use_fp32r else (lambda ap: ap)
    I, O, V, A, G, P, DS, DG = X.I, X.O, X.V, X.A, X.G, X.P, X.DS, X.DG
    with Stage(X) as st:
        identf = st.load(I["ident_f"], [128, 128])
        mk4 = {}
        mk1 = {}
        for sfx in ("", "_s"):
            m4 = st.sb([128, 4, 128])
            for a, nm in enumerate(("m_lt", "m_gt", "m_lt", "m_le")):
                DS(lambda E: E.dma_start(out=m4[:, a, :], in_=I[nm + sfx]), (), [m4])
            mk4[sfx] = m4
            mk1[sfx] = st.load(I["m_le" + sfx], [128, 128])
        colmask = st.load(I["colmask"], [128, 2048])
        rowmask = st.load(I["rowmask"], [128, 16])
        lng = st.load(I["rwkv_ln_g"].rearrange("(j p) o -> p (j o)", p=128), [128, 8])
        lnb = st.load(I["rwkv_ln_b"].rearrange("(j p) o -> p (j o)", p=128), [128, 8])
        STp = [st.sb([128, 64]) for _ in range(8)]
        for j in range(8):
            G(lambda E: E.memset(STp[j][:], 0.0), (), [STp[j]])
            V(lambda E: E.tensor_scalar(out=R(STp[j][:]), in0=STp[j][:], scalar1=1.0, scalar2=None, op0=ALU.mult), [STp[j]], [STp[j]])
        STs = [st.sb([128, 16, 64]) for _ in range(8)]
        pss = RR([st.ps([128, 512]) for _ in range(8)])
        with Stage(X) as s0:
            sin = RR([s0.sb([64, 16, 128]) for _ in range(2)])
            for j in range(8):
                si = sin.next()
                for h2 in range(2):
                    DS(lambda E: E.dma_start(out=si[:, :, h2 * 64:h2 * 64 + 64],
                                             in_=I["state_wkv"][:, 2 * j + h2, :, :].rearrange("b v k -> v b k")), (), [si])
                for half in range(2):
                    ps = pss.next()
                    for bb in range(8):
                        b = half * 8 + bb
                        P(lambda E: E.transpose(out=ps[:, bb * 64:(bb + 1) * 64], in_=si[0:64, b, :], identity=identf[0:64, 0:64]),
                          [si, identf], [ps])
                    A(lambda E: E.copy(out=R(STs[j][:, half * 8:half * 8 + 8, :]), in_=ps[:, :].rearrange("p (b v) -> p b v", v=64)),
                      [ps], [STs[j]])
        inpool = RR([st.sb([128, 8, 5, 128]) for _ in range(1)])
        INparts = {id(t): [T(t.t) for _ in range(5)] for t in inpool.items}
        tmpool = RR([st.sb([128, 8, 3, 128]) for _ in range(1)])
        bgpool = RR([st.sb([128, 8, 2, 128]) for _ in range(1)])
        BGparts = {id(t): [T(t.t) for _ in range(2)] for t in bgpool.items}
        wcpool = RR([st.sb([128, 8, 16]) for _ in range(2)])
        M4 = [st.sb([128, 4, 128]) for _ in range(8)]
        MKR = [st.sb([128, 128]) for _ in range(8)]
        LV = [[st.sb([128, 2, 128]) for _ in range(2)] for _ in range(8)]
        PM = [[st.sb([128, 128]) for _ in range(2)] for _ in range(8)]
        XT = st.sb([128, 16, 64])
        UT = st.sb([128, 16, 64])
        Y = st.sb([128, 16, 64])
        cen = st.sb([128, 16, 64])
        sqv = st.sb([128, 16, 64])
        stat = RR([st.sb([128, 16]) for _ in range(4)])
        bdp = RR([st.sb([128, 16, 128]) for _ in range(2)])
        ubd = RR([st.sb([128, 16, 64]) for _ in range(2)])
        tmpf = RR([st.sb([128, 128]) for _ in range(3)])
        oap = RR([st.sb([128, 8, 128], BF16) for _ in range(2)])
        stmp = RR([st.sb([128, 16, 64]) for _ in range(2)])

        X.S.fp32r = X.use_fp32r
        for ci, (g0, n) in enumerate(TT):
            is_s = (ci == len(TT) - 1)
            sfx = "_s" if is_s else ""
            L = 3 if is_s else (4 if n == 16 else 7)
            IN = inpool.next()
            INa = INparts[id(IN)]
            for a in range(5):
                DS(lambda E: E.dma_start(out=IN[:, :, a, 0:n], in_=X.RW[:, a, :, g0:g0 + n].rearrange("j p t -> p j t")), [X.RW], [INa[a]])
            BG = bgpool.next()
            BGa = BGparts[id(BG)]
            for a in range(2):
                DS(lambda E: E.dma_start(out=BG[:, :, a, 0:n], in_=X.RW[:, 5 + a, :, g0:g0 + n].rearrange("j p t -> p j t")), [X.RW], [BGa[a]])
            WC = wcpool.next()
            if not is_s:
                DS(lambda E: E.dma_start(out=WC[:, :, 0], in_=X.RW[:, 7, :, g0 + n - 1].rearrange("j p -> p j"),
                                         allow_slow_non_contiguous=True), [X.RW], [WC])
            else:
                for j in range(8):
                    DS(lambda E: E.dma_start(out=WC[:, j, :], in_=X.RW[j, 7, :, NPR:NT].rearrange("p (b s) -> p b s", s=8)[:, :, 7],
                                             allow_slow_non_contiguous=True), [X.RW], [WC])
            TM = tmpool.next()
            for j in range(8):
                ps = pss.next()
                for a3, a in enumerate((2, 3, 4)):
                    P(lambda E: E.transpose(out=ps[0:n, a3 * 128:(a3 + 1) * 128], in_=IN[:, j, a, 0:n], identity=identf[:, :]),
                      [*INa, identf], [ps])
                A(lambda E: E.copy(out=R(TM[0:n, j, :, :]), in_=ps[0:n, 0:384].rearrange("p (a t) -> p a t", a=3)), [ps], [TM])
            npb = 1 if is_s else 4
            for hb in range(8 // npb):
                heads = [(hb * npb + jj, h2) for jj in range(npb) for h2 in range(2)]
                NH = len(heads)
                for hi, (j, h2) in enumerate(heads):
                    rows = slice(h2 * 64, h2 * 64 + 64)
                    at, rt, bt, kt = (IN[rows, j, a, 0:n] for a in range(4))
                    ps = pss.next()
                    for a, (l, r) in enumerate(((bt, at), (at, bt), (kt, at), (bt, rt))):
                        P(lambda E: E.matmul(ps[0:n, a * 128:a * 128 + n], lhsT=l, rhs=r, start=True, stop=True), [*INa], [ps])
                    V(lambda E: E.tensor_tensor(out=R(M4[hi][0:n, :, 0:n]), in0=ps[0:n, :].rearrange("p (a t) -> p a t", a=4)[:, :, 0:n],
                                                in1=mk4[sfx][0:n, :, 0:n], op=ALU.mult), [ps, mk4[sfx]], [M4[hi]])
                    ps = pss.next()
                    P(lambda E: E.matmul(ps[0:n, 0:n], lhsT=kt, rhs=rt, start=True, stop=True), [*INa], [ps])
                    V(lambda E: E.tensor_tensor(out=R(MKR[hi][0:n, 0:n]), in0=ps[0:n, 0:n], in1=mk1[sfx][0:n, 0:n], op=ALU.mult),
                      [ps, mk1[sfx]], [MKR[hi]])
                    G(lambda E: E.tensor_tensor(out=R(PM[hi][0][0:n, 0:n]), in0=M4[hi][0:n, 0, 0:n], in1=identf[0:n, 0:n], op=ALU.add),
                      [M4[hi], identf], [PM[hi][0]])
                for i in range(1, L):
                    for hi in range(NH):
                        if i == 1:
                            Ap, ATp, srcT = M4[hi][0:n, 0, 0:n], M4[hi][0:n, 1, 0:n], M4[hi]
                        else:
                            srcT = LV[hi][(i - 1) % 2]
                            Ap, ATp = srcT[0:n, 0, 0:n], srcT[0:n, 1, 0:n]
                        ps = pss.next()
                        P(lambda E: E.matmul(ps[0:n, 0:n], lhsT=ATp, rhs=Ap, start=True, stop=True), [srcT], [ps])
                        P(lambda E: E.matmul(ps[0:n, 128:128 + n], lhsT=Ap, rhs=ATp, start=True, stop=True), [srcT], [ps])
                        dst = LV[hi][i % 2]
                        A(lambda E: E.copy(out=R(dst[0:n, :, 0:n]), in_=ps[0:n, 0:256].rearrange("p (a t) -> p a t", a=2)[:, :, 0:n]),
                          [ps], [dst])
                    for hi in range(NH):
                        cur = LV[hi][i % 2]
                        Pp, Pn = PM[hi][(i - 1) % 2], PM[hi][i % 2]
                        ps = pss.next()
                        P(lambda E: E.matmul(ps[0:n, 0:n], lhsT=cur[0:n, 1, 0:n], rhs=Pp[0:n, 0:n], start=True, stop=True), [cur, Pp], [ps])
                        V(lambda E: E.tensor_tensor(out=R(Pn[0:n, 0:n]), in0=ps[0:n, 0:n], in1=Pp[0:n, 0:n], op=ALU.add), [ps, Pp], [Pn])
                Pf = [PM[hi][(L - 1) % 2] for hi in range(NH)]
                bd = {}
                if is_s:
                    for jj in range(npb):
                        j = hb * npb + jj
                        for a in range(2):
                            t = bdp.next()
                            V(lambda E: E.tensor_tensor(out=R(t[:]), in0=IN[:, j, a, :].unsqueeze(1).to_broadcast([128, 16, 128]),
                                                        in1=colmask[:, :].rearrange("p (b t) -> p b t", b=16), op=ALU.mult),
                              [*INa, colmask], [t])
                            bd[(j, a)] = t
                for hi, (j, h2) in enumerate(heads):
                    h = 2 * j + h2
                    rows = slice(h2 * 64, h2 * 64 + 64)
                    vtm = TM[0:n, j, 2, h2 * 64:h2 * 64 + 64]
                    ps = pss.next()
                    if not is_s:
                        P(lambda E: E.matmul(ps[0:n, 0:64], lhsT=IN[rows, j, 0, 0:n], rhs=STp[j][rows, :], start=True, stop=False),
                          [*INa, STp[j]], [ps])
                    else:
                        for b in range(16):
                            P(lambda E: E.matmul(ps[0:n, 0:64], lhsT=bd[(j, 0)][rows, b, :], rhs=STs[j][rows, b, :],
                                                 start=(b == 0), stop=False), [bd[(j, 0)], STs[j]], [ps])
                    P(lambda E: E.matmul(ps[0:n, 0:64], lhsT=M4[hi][0:n, 2, 0:n], rhs=vtm, start=False, stop=True), [M4[hi], TM], [ps])
                    A(lambda E: E.copy(out=R(XT[0:n, h, :]), in_=ps[0:n, 0:64]), [ps], [XT])
                for hi, (j, h2) in enumerate(heads):
                    h = 2 * j + h2
                    ps = pss.next()
                    P(lambda E: E.matmul(ps[0:n, 0:64], lhsT=Pf[hi][0:n, 0:n], rhs=XT[0:n, h, :], start=True, stop=True), [Pf[hi], XT], [ps])
                    A(lambda E: E.copy(out=R(UT[0:n, h, :]), in_=ps[0:n, 0:64]), [ps], [UT])
                for hi, (j, h2) in enumerate(heads):
                    h = 2 * j + h2
                    rows = slice(h2 * 64, h2 * 64 + 64)
                    vtm = TM[0:n, j, 2, h2 * 64:h2 * 64 + 64]
                    ps = pss.next()
                    if not is_s:
                        P(lambda E: E.matmul(ps[0:n, 0:64], lhsT=IN[rows, j, 1, 0:n], rhs=STp[j][rows, :], start=True, stop=False),
                          [*INa, STp[j]], [ps])
                    else:
                        for b in range(16):
                            P(lambda E: E.matmul(ps[0:n, 0:64], lhsT=bd[(j, 1)][rows, b, :], rhs=STs[j][rows, b, :],
                                                 start=(b == 0), stop=False), [bd[(j, 1)], STs[j]], [ps])
                    P(lambda E: E.matmul(ps[0:n, 0:64], lhsT=M4[hi][0:n, 3, 0:n], rhs=UT[0:n, h, :], start=False, stop=False), [M4[hi], UT], [ps])
                    P(lambda E: E.matmul(ps[0:n, 0:64], lhsT=MKR[hi][0:n, 0:n], rhs=vtm, start=False, stop=True), [MKR[hi], TM], [ps])
                    A(lambda E: E.copy(out=Y[0:n, h, :], in_=ps[0:n, 0:64]), [ps], [Y])
                for hi, (j, h2) in enumerate(heads):
                    h = 2 * j + h2
                    rows = slice(h2 * 64, h2 * 64 + 64)
                    vtm = TM[0:n, j, 2, h2 * 64:h2 * 64 + 64]
                    if not is_s:
                        ps = pss.next()
                        P(lambda E: E.matmul(ps[:, 0:64], lhsT=TM[0:n, j, 0, :], rhs=UT[0:n, h, :], start=True, stop=False), [TM, UT], [ps])
                        P(lambda E: E.matmul(ps[:, 0:64], lhsT=TM[0:n, j, 1, :], rhs=vtm, start=False, stop=True), [TM], [ps])
                        V(lambda E: E.tensor_tensor(out=R(STp[j][rows, :]), in0=ps[rows, 0:64], in1=STp[j][rows, :], op=ALU.add), [ps, STp[j]], [STp[j]])
                        V(lambda E: E.tensor_scalar(out=R(STp[j][rows, :]), in0=STp[j][rows, :], scalar1=WC[rows, j, 0:1], scalar2=None,
                                                    op0=ALU.mult), [STp[j], WC], [STp[j]])
                    else:
                        ub, vb = ubd.next(), ubd.next()
                        rmb = rowmask[:, :].unsqueeze(2).to_broadcast([128, 16, 64])
                        V(lambda E: E.tensor_tensor(out=R(ub[:]), in0=UT[:, h, :].unsqueeze(1).to_broadcast([128, 16, 64]), in1=rmb, op=ALU.mult),
                          [UT, rowmask], [ub])
                        V(lambda E: E.tensor_tensor(out=R(vb[:]), in0=vtm.unsqueeze(1).to_broadcast([128, 16, 64]), in1=rmb, op=ALU.mult),
                          [TM, rowmask], [vb])
                        for half in range(2):
                            ps = pss.next()
                            bs = slice(half * 8, half * 8 + 8)
                            P(lambda E: E.matmul(ps[:, :], lhsT=TM[:, j, 0, :], rhs=ub[:, bs, :].rearrange("p b v -> p (b v)"),
                                                 start=True, stop=False), [TM, ub], [ps])
                            P(lambda E: E.matmul(ps[:, :], lhsT=TM[:, j, 1, :], rhs=vb[:, bs, :].rearrange("p b v -> p (b v)"),
                                                 start=False, stop=True), [TM, vb], [ps])
                            tt = stmp.next()
                            V(lambda E: E.tensor_tensor(out=tt[rows, 0:8, :], in0=ps[rows, :].rearrange("p (b v) -> p b v", v=64),
                                                        in1=STs[j][rows, bs, :], op=ALU.add), [ps, STs[j]], [tt])
                            V(lambda E: E.tensor_tensor(out=R(STs[j][rows, bs, :]), in0=tt[rows, 0:8, :],
                                                        in1=WC[rows, j, bs].unsqueeze(2).to_broadcast([64, 8, 64]), op=ALU.mult),
                              [tt, WC], [STs[j]])
            mu, var = stat.next(), stat.next()
            V(lambda E: E.tensor_reduce(out=mu[0:n, :], in_=Y[0:n, :, :], axis=AX.X, op=ALU.add), [Y], [mu])
            V(lambda E: E.tensor_scalar(out=mu[0:n, :], in0=mu[0:n, :], scalar1=1.0 / 64, scalar2=None, op0=ALU.mult), [mu], [mu])
            G(lambda E: E.tensor_tensor(out=cen[0:n], in0=Y[0:n], in1=mu[0:n, :].unsqueeze(2).to_broadcast([n, 16, 64]), op=ALU.subtract),
              [Y, mu], [cen])
            G(lambda E: E.tensor_tensor(out=sqv[0:n], in0=cen[0:n], in1=cen[0:n], op=ALU.mult), [cen], [sqv])
            V(lambda E: E.tensor_reduce(out=var[0:n, :], in_=sqv[0:n, :, :], axis=AX.X, op=ALU.add), [sqv], [var])
            A(lambda E: E.activation(out=var[0:n, :], in_=var[0:n, :], func=AF.Sqrt, bias=64e-5, scale=1.0 / 64), [var], [var])
            V(lambda E: E.reciprocal(out=var[0:n, :], in_=var[0:n, :]), [var], [var])
            V(lambda E: E.tensor_tensor(out=cen[0:n], in0=cen[0:n], in1=var[0:n, :].unsqueeze(2).to_broadcast([n, 16, 64]), op=ALU.mult),
              [cen, var], [cen])
            OA = oap.next()
            for j in range(8):
                ps = pss.next()
                P(lambda E: E.transpose(out=ps[:, 0:n], in_=cen[0:n, 2 * j:2 * j + 2, :].rearrange("p h v -> p (h v)"),
                                        identity=identf[0:n, 0:n]), [cen, identf], [ps])
                tf = tmpf.next()
                V(lambda E: E.tensor_scalar(out=tf[:, 0:n], in0=ps[:, 0:n], scalar1=lng[:, j:j + 1], scalar2=lnb[:, j:j + 1],
                                            op0=ALU.mult, op1=ALU.add), [ps, lng, lnb], [tf])
                G(lambda E: E.tensor_tensor(out=tf[:, 0:n], in0=tf[:, 0:n], in1=BG[:, j, 0, 0:n], op=ALU.add), [tf, *BGa], [tf])
                G(lambda E: E.tensor_tensor(out=OA[:, j, 0:n], in0=tf[:, 0:n], in1=BG[:, j, 1, 0:n], op=ALU.mult), [tf, *BGa], [OA])
            DS(lambda E: E.dma_start(out=X.OAT[:, :, g0:g0 + n].rearrange("j p t -> p j t"), in_=OA[:, :, 0:n]), [OA], [X.OAT])
        X.S.fp32r = False
        with Stage(X) as s1:
            so = s1.sb([64, 8, 128])
            for j in range(8):
                ps = pss.next()
                P(lambda E: E.transpose(out=ps[0:64, 0:128], in_=STp[j][:, :], identity=identf[:, :]), [STp[j], identf], [ps])
                A(lambda E: E.copy(out=so[:, j, :], in_=ps[0:64, 0:128]), [ps], [so])
            for h2 in range(2):
                DS(lambda E: E.dma_start(out=O["wkv_p"].rearrange("(j h) v k -> h v j k", h=2)[h2],
                                         in_=so[:, :, h2 * 64:h2 * 64 + 64]), [so], [])
            sos = RR([s1.sb([64, 16, 128]) for _ in range(1)])
            for j in range(8):
                sj = sos.next()
                for q in range(4):
                    ps = pss.next()
                    for bb in range(4):
                        b = q * 4 + bb
                        P(lambda E: E.transpose(out=ps[0:64, bb * 128:(bb + 1) * 128], in_=STs[j][:, b, :], identity=identf[:, :]),
                          [STs[j], identf], [ps])
                    A(lambda E: E.copy(out=sj[:, q * 4:q * 4 + 4, :], in_=ps[0:64, :].rearrange("p (b k) -> p b k", b=4)), [ps], [sj])
                for h2 in range(2):
                    DS(lambda E: E.dma_start(out=O["wkv_s"][:, 2 * j + h2, :, :].rearrange("b v k -> v b k"),
                                             in_=sj[:, :, h2 * 64:h2 * 64 + 64]), [sj], [])


def stage_d(X):
    I, O, V, A, G, P, DS, DG = X.I, X.O, X.V, X.A, X.G, X.P, X.DS, X.DG
    with Stage(X) as st:
        identb = st.load(I["ident_b"], [128, 128], BF16)
        identf = st.load(I["ident_f"], [128, 128])
        onesf = st.load(I["ones_f"], [128, 128])
        mleb = st.load(I["m_le_b"], [128, 128], BF16)
        newmask = st.load(I["newmask"], [8, 128], BF16)
        ropec = st.load(I["rope_c"], [128, NT])
        ropes = st.load(I["rope_s"], [128, NT])
        CQ = st.sb([128, 4, NT], BF16)
        CKVb = st.sb([128, 2, NT], BF16)
        KRb = st.sb([128, NT], BF16)
        KCN = st.sb([8, 16, 257], BF16)
        with Stage(X) as s1:
            pss = RR([s1.ps([128, 512]) for _ in range(4)])
            pbuf = [s1.sb([128, NT]) for _ in range(4)]
            sq = s1.sb([128, NT])
            rq = s1.sb([128, NT])
            CKV = s1.sb([128, 2, NT])
            KR = s1.sb([128, NT])
            gv = s1.sb([128, 8])

            def norm(names, gname, width, outs):
                nt_ = len(names)
                for i, nm in enumerate(names):
                    DS(lambda E: E.dma_start(out=pbuf[i][:], in_=X.PFM[CTI[nm], :, :]), [X.PFM], [pbuf[i]])
                DS(lambda E: E.dma_start(out=gv[:, 0:nt_], in_=I[gname].rearrange("(j p) o -> p (j o)", p=128)), (), [gv])
                for (t0, tn) in GROUPS:
                    ps = pss.next()
                    for i in range(nt_):
                        G(lambda E: E.tensor_tensor(out=sq[:, t0:t0 + tn], in0=pbuf[i][:, t0:t0 + tn], in1=pbuf[i][:, t0:t0 + tn], op=ALU.mult),
                          [pbuf[i]], [sq])
                        P(lambda E: E.matmul(ps[:, 0:tn], lhsT=onesf[:], rhs=sq[:, t0:t0 + tn], start=(i == 0), stop=(i == nt_ - 1)),
                          [onesf, sq], [ps])
                    A(lambda E: E.activation(out=rq[:, t0:t0 + tn], in_=ps[:, 0:tn], func=AF.Sqrt, bias=1e-6, scale=1.0 / width), [ps], [rq])
                V(lambda E: E.reciprocal(out=rq[:], in_=rq[:]), [rq], [rq])
                for i in range(nt_):
                    for o in outs:
                        V(lambda E: E.scalar_tensor_tensor(out=o[:, i, :], in0=pbuf[i][:], scalar=gv[:, i:i + 1], in1=rq[:],
                                                           op0=ALU.mult, op1=ALU.mult), [pbuf[i], gv, rq], [o])

            norm([f"q{i}" for i in range(4)], "mla_q_norm_g", 512, [CQ])
            norm(["kv0", "kv1"], "mla_kv_norm_g", 256, [CKV, CKVb])
            DS(lambda E: E.dma_start(out=pbuf[0][64:96, :], in_=X.PFM[CTI["kr"], 64:96, :]), [X.PFM], [pbuf[0]])
            DS(lambda E: E.dma_start(out=pbuf[1][64:96, :], in_=X.PFM[CTI["krs"], 64:96, :]), [X.PFM], [pbuf[1]])
            r_ = slice(64, 96)
            V(lambda E: E.tensor_tensor(out=KR[r_, :], in0=pbuf[0][r_, :], in1=ropec[r_, :], op=ALU.mult), [pbuf[0], ropec], [KR])
            G(lambda E: E.tensor_tensor(out=sq[r_, :], in0=pbuf[1][r_, :], in1=ropes[r_, :], op=ALU.mult), [pbuf[1], ropes], [sq])
            V(lambda E: E.tensor_tensor(out=KR[r_, :], in0=KR[r_, :], in1=sq[r_, :], op=ALU.add), [KR, sq], [KR])
            A(lambda E: E.copy(out=KRb[r_, :], in_=KR[r_, :]), [KR], [KRb])
            otm = RR([s1.sb([128, 288]) for _ in range(2)])
            for ti, (g0, n) in enumerate(TT):
                ps = pss.next()
                for kc in range(2):
                    P(lambda E: E.transpose(out=ps[0:n, kc * 128:(kc + 1) * 128], in_=CKV[:, kc, g0:g0 + n], identity=identf[:, :]),
                      [CKV, identf], [ps])
                P(lambda E: E.transpose(out=ps[0:n, 256:288], in_=KR[r_, g0:g0 + n], identity=identf[r_, r_]), [KR, identf], [ps])
                ot = otm.next()
                A(lambda E: E.copy(out=ot[0:n, :], in_=ps[0:n, 0:288]), [ps], [ot])
                if ti < 17:
                    DS(lambda E: E.dma_start(out=O["ckv_p"][g0:g0 + n, :], in_=ot[0:n, 0:256]), [ot], [])
                    DS(lambda E: E.dma_start(out=O["kr_p"][g0:g0 + n, :], in_=ot[0:n, 256:288]), [ot], [])
                else:
                    wdep = Buf()
                    DS(lambda E: E.dma_start(out=O["ckv_s"][:, :], in_=ot[0:n, 0:256]), [ot], [wdep])
                    DS(lambda E: E.dma_start(out=O["kr_s"][:, :], in_=ot[0:n, 256:288]), [ot], [])
                    kcf = s1.sb([8, 16, 256])
                    DS(lambda E: E.dma_start(out=kcf[:], in_=O["ckv_s"].rearrange("(b s) c -> s b c", s=8)), [wdep], [kcf])
                    V(lambda E: E.tensor_copy(out=KCN[:, :, 0:256], in_=kcf[:]), [kcf], [KCN])
                    V(lambda E: E.memset(KCN[:, :, 256:257], 1.0), (), [KCN])
        QLAT = st.sb([128, 2, 16, 16, 8], BF16)
        QR = st.sb([128, 16, 16, 8], BF16)
        WUVb = st.sb([128, 2, 1024], BF16)
        DG(lambda E: E.dma_start(out=WUVb[:], in_=I["mla_w_uv"].rearrange("(kc p) c -> p kc c", p=128)), (), [WUVb])
        with Stage(X) as s2:
            WQ = s2.sb([128, 4, 1536], BF16)
            DG(lambda E: E.dma_start(out=WQ[:], in_=I["mla_w_uq"].rearrange("(kc p) c -> p kc c", p=128)), (), [WQ])
            WQS = s2.sb([128, 4, 16, 32], BF16)
            wq4 = I["mla_w_uq"].rearrange("(kc p) (h c) -> p kc h c", p=128, c=96)
            for kc in range(4):
                DG(lambda E: E.dma_start(out=WQS[:, kc, :, 0:16], in_=wq4[:, kc, :, 80:96]), (), [WQS])
                DG(lambda E: E.dma_start(out=WQS[:, kc, :, 16:32], in_=wq4[:, kc, :, 64:80]), (), [WQS])
            WUKb = s2.sb([128, 2, 1024], BF16)
            DG(lambda E: E.dma_start(out=WUKb[:], in_=I["mla_w_uk"].rearrange("(kc p) c -> p kc c", p=128)), (), [WUKb])
            WUKT = s2.sb([64, 16, 2, 128], BF16)
            psb = RR([s2.ps([128, 1024], BF16) for _ in range(1)])
            psq = RR([s2.ps([128, 512]) for _ in range(2)])
            pss_ = RR([s2.ps([128, 512]) for _ in range(2)])
            pso = RR([s2.ps([128, 512]) for _ in range(2)])
            for h in range(16):
                ps = psb.next()
                for kc in range(2):
                    P(lambda E: E.transpose(out=ps[0:64, kc * 128:(kc + 1) * 128], in_=WUKb[:, kc, h * 64:(h + 1) * 64], identity=identb[:, :]),
                      [WUKb, identb], [ps])
                A(lambda E: E.copy(out=WUKT[:, h, :, :], in_=ps[0:64, 0:256].rearrange("p (a c) -> p a c", a=2)), [ps], [WUKT])
            Qp = RR([s2.sb([96, NT], BF16) for _ in range(2)])
            Kp = RR([s2.sb([96, NPR], BF16) for _ in range(2)])
            Vp = RR([s2.sb([128, 17, 65], BF16) for _ in range(2)])
            OBp = RR([s2.sb([64, NPR], BF16) for _ in range(2)])
            tmp_r = RR([s2.sb([96, 512]) for _ in range(2)])
            tmp_r2 = RR([s2.sb([96, 512]) for _ in range(2)])
            PTp = RR([s2.sb([128, 4, 128], BF16) for _ in range(12)])
            otp = RR([s2.sb([128, 64], BF16) for _ in range(2)])
            rlp = RR([s2.sb([128, 1]) for _ in range(2)])
            for h in range(16):
                Qh, Kh, Vh, OBh = Qp.next(), Kp.next(), Vp.next(), OBp.next()
                for (t0, tn) in GROUPS:
                    psA, psB = psq.next(), psq.next()
                    for kc in range(4):
                        P(lambda E: E.matmul(psA[0:96, 0:tn], lhsT=WQ[:, kc, h * 96:(h + 1) * 96], rhs=CQ[:, kc, t0:t0 + tn],
                                             start=(kc == 0), stop=(kc == 3)), [WQ, CQ], [psA])
                    for kc in range(4):
                        P(lambda E: E.matmul(psB[0:32, 0:tn], lhsT=WQS[:, kc, h, :], rhs=CQ[:, kc, t0:t0 + tn],
                                             start=(kc == 0), stop=(kc == 3)), [WQS, CQ], [psB])
                    A(lambda E: E.copy(out=Qh[0:64, t0:t0 + tn], in_=psA[0:64, 0:tn]), [psA], [Qh])
                    t1, t2 = tmp_r.next(), tmp_r2.next()
                    V(lambda E: E.tensor_tensor(out=t1[64:96, 0:tn], in0=psB[0:32, 0:tn], in1=ropes[0:32, t0:t0 + tn], op=ALU.mult),
                      [psB, ropes], [t1])
                    V(lambda E: E.tensor_tensor(out=t2[64:96, 0:tn], in0=psA[64:96, 0:tn], in1=ropec[64:96, t0:t0 + tn], op=ALU.mult),
                      [psA, ropec], [t2])
                    G(lambda E: E.tensor_tensor(out=Qh[64:96, t0:t0 + tn], in0=t1[64:96, 0:tn], in1=t2[64:96, 0:tn], op=ALU.add),
                      [t1, t2], [Qh])
                    if t0 < NPR:
                        kn = min(tn, NPR - t0)
                        psK = psq.next()
                        for kc in range(2):
                            P(lambda E: E.matmul(psK[0:64, 0:kn], lhsT=WUKb[:, kc, h * 64:(h + 1) * 64], rhs=CKVb[:, kc, t0:t0 + kn],
                                                 start=(kc == 0), stop=(kc == 1)), [WUKb, CKVb], [psK])
                        A(lambda E: E.copy(out=Kh[0:64, t0:t0 + kn], in_=psK[0:64, 0:kn]), [psK], [Kh])
                G(lambda E: E.tensor_copy(out=Kh[64:96, :], in_=KRb[64:96, 0:NPR]), [KRb], [Kh])
                G(lambda E: E.memset(Vh[:, :, 64:65], 1.0), (), [Vh])
                for ti in range(17):
                    g0, n = TT[ti]
                    psV = psq.next()
                    for kc in range(2):
                        P(lambda E: E.matmul(psV[0:n, 0:64], lhsT=CKVb[:, kc, g0:g0 + n], rhs=WUVb[:, kc, h * 64:(h + 1) * 64],
                                             start=(kc == 0), stop=(kc == 1)), [CKVb, WUVb], [psV])
                    A(lambda E: E.copy(out=Vh[0:n, ti, 0:64], in_=psV[0:n, 0:64]), [psV], [Vh])
                for kc in range(2):
                    psL = psq.next()
                    P(lambda E: E.matmul(psL[:, 0:128], lhsT=WUKT[0:64, h, kc, :], rhs=Qh[0:64, NPR:NT], start=True, stop=True), [WUKT, Qh], [psL])
                    A(lambda E: E.copy(out=QLAT[:, kc, :, h, :], in_=psL[:, 0:128].rearrange("p (b s) -> p b s", s=8)), [psL], [QLAT])
                G(lambda E: E.tensor_copy(out=QR[64:96, :, h, :], in_=Qh[64:96, NPR:NT].rearrange("p (b s) -> p b s", s=8)), [Qh], [QR])
                def scores(qi):
                    q0, nq = TT[qi]
                    klist = list(range(qi + 1))
                    groups = [[0]] + [klist[1:][i:i + 4] for i in range(0, len(klist) - 1, 4)]
                    items = []
                    for grp in groups:
                        psS = pss_.next()
                        PT = PTp.next()
                        nk = TT[grp[0]][1]
                        for a, kt in enumerate(grp):
                            k0, _ = TT[kt]
                            P(lambda E: E.matmul(psS[0:nk, a * 128:a * 128 + nq], lhsT=Kh[:, k0:k0 + nk], rhs=Qh[:, q0:q0 + nq],
                                                 start=True, stop=True), [Kh, Qh], [psS])
                        na = len(grp)
                        A(lambda E: E.activation(out=PT[0:nk, 0:na, 0:nq], in_=psS[0:nk, 0:na * 128].rearrange("p (a q) -> p a q", a=na)[:, :, 0:nq],
                                                 func=AF.Exp, scale=SCALE), [psS], [PT])
                        if grp[-1] == qi:
                            a = na - 1
                            G(lambda E: E.tensor_tensor(out=PT[0:nk, a, 0:nq], in0=PT[0:nk, a, 0:nq], in1=mleb[0:nk, 0:nq], op=ALU.mult),
                              [PT, mleb], [PT])
                        items.append((PT, grp, nk))
                    return items

                def pv(qi, items):
                    q0, nq = TT[qi]
                    po = pso.next()
                    nk_total = qi + 1
                    done = 0
                    for (PT, grp, nk) in items:
                        for a, kt in enumerate(grp):
                            P(lambda E: E.matmul(po[0:nq, 0:65], lhsT=PT[0:nk, a, 0:nq], rhs=Vh[0:nk, kt, :], start=(done == 0),
                                                 stop=(done == nk_total - 1)), [PT, Vh], [po])
                            done += 1
                    rl = rlp.next()
                    V(lambda E: E.reciprocal(out=rl[0:nq, :], in_=po[0:nq, 64:65]), [po], [rl])
                    ot = otp.next()
                    V(lambda E: E.tensor_scalar(out=ot[0:nq, :], in0=po[0:nq, 0:64], scalar1=rl[0:nq, 0:1], scalar2=None, op0=ALU.mult),
                      [po, rl], [ot])
                    pt_ = psb.next()
                    P(lambda E: E.transpose(out=pt_[0:64, 0:nq], in_=ot[0:nq, :], identity=identb[0:nq, 0:nq]), [ot, identb], [pt_])
                    A(lambda E: E.copy(out=OBh[:, q0:q0 + nq], in_=pt_[0:64, 0:nq]), [pt_], [OBh])

                pend = {}
                for step in range(18):
                    if step < 17:
                        pend[step] = scores(step)
                    if step >= 1:
                        pv(step - 1, pend.pop(step - 1))
                DS(lambda E: E.dma_start(out=X.OBT[h // 2, (h % 2) * 64:(h % 2) * 64 + 64, 0:NPR], in_=OBh[:, :]), [OBh], [X.OBT])
        with Stage(X) as s3:
            ptb = s3.sb([128, 16], I32)
            DS(lambda E: E.dma_start(out=ptb[:], in_=I["page_table"].rearrange("b j -> j b")), (), [ptb])
            idx = s3.sb([128, 16, 16], I32)
            for g in range(16):
                V(lambda E: E.tensor_scalar(out=idx[:, :, g], in0=ptb[:], scalar1=16, scalar2=g, op0=ALU.mult, op1=ALU.add), [ptb], [idx])
            ckv_v = I["cache_ckv"].rearrange("n (g r) c -> (n g) (r c)", g=16)
            kr_v = I["cache_krope"].rearrange("n (g r) c -> (n g) (r c)", g=16)
            Gk = RR([s3.sb([128, 8, 256]) for _ in range(4)])
            Gr = RR([s3.sb([128, 8, 32]) for _ in range(4)])
            KCb = RR([s3.sb([128, 8, 257], BF16) for _ in range(3)])
            KRc = RR([s3.sb([128, 8, 96], BF16) for _ in range(3)])
            KCh = {}
            for t in KCb.items:
                G(lambda E: E.memset(t[:, :, 256:257], 1.0), (), [t])
                KCh[id(t)] = (T(t.t), T(t.t))
            for t in KRc.items:
                G(lambda E: E.memset(t[:, :, 0:64], 0.0), (), [t])
            KT = RR([s3.sb([128, 2, 4, 128], BF16) for _ in range(2)])
            KTh = {id(t): (T(t.t), T(t.t)) for t in KT.items}
            KTR = RR([s3.sb([96, 4, 128], BF16) for _ in range(3)])
            PTs = RR([s3.sb([128, 4, 128], BF16) for _ in range(3)])
            PN = RR([s3.sb([8, 128], BF16) for _ in range(2)])
            OL = RR([s3.sb([128, 256], BF16) for _ in range(2)])
            rlp = RR([s3.sb([128, 1]) for _ in range(2)])
            OLT = s3.sb([128, 2, 16, 16, 8], BF16)
            psA = RR([s3.ps([128, 512]) for _ in range(2)])
            psS = RR([s3.ps([128, 512]) for _ in range(2)])
            psT = RR([s3.ps([128, 2, 4, 128], BF16) for _ in range(2)])
            psR = RR([s3.ps([128, 4, 128], BF16) for _ in range(2)])
            KT0 = RR([s3.sb([128, 4, 128], BF16) for _ in range(3)])
            KT1 = RR([s3.sb([128, 4, 128], BF16) for _ in range(3)])
            NIT = 16 * 16 * 2
            stt = {}
            pas = {}

            def stage1(it):
                b, g, r4 = it // 32, (it % 32) // 2, it % 2
                if r4 == 0:
                    gk, gr = Gk.next(), Gr.next()
                    DG(lambda E: E.indirect_dma_start(out=gk[:].rearrange("p r c -> p (r c)"), out_offset=None, in_=ckv_v,
                                                      in_offset=bass.IndirectOffsetOnAxis(ap=idx[:, b, g:g + 1], axis=0)), [idx], [gk])
                    DG(lambda E: E.indirect_dma_start(out=gr[:].rearrange("p r c -> p (r c)"), out_offset=None, in_=kr_v,
                                                      in_offset=bass.IndirectOffsetOnAxis(ap=idx[:, b, g:g + 1], axis=0)), [idx], [gr])
                    kcb, krc = KCb.next(), KRc.next()
                    kh = KCh[id(kcb)]
                    G(lambda E: E.tensor_copy(out=kcb[:, 0:4, 0:256], in_=gk[:, 0:4, :]), [gk, kcb], [kh[0]])
                    A(lambda E: E.copy(out=kcb[:, 4:8, 0:256], in_=gk[:, 4:8, :]), [gk, kcb], [kh[1]])
                    V(lambda E: E.tensor_copy(out=krc[:, :, 64:96], in_=gr[:]), [gr], [krc])
                    stt[(b, g)] = (kcb, krc, kh)
                kcb, krc, kh = stt[(b, g)]
                pT, pR = psT.next(), psR.next()
                for rr in range(4):
                    r = r4 * 4 + rr
                    for kc in range(2):
                        P(lambda E: E.transpose(out=pT[:, kc, rr, :], in_=kcb[:, r, kc * 128:(kc + 1) * 128], identity=identb[:, :]),
                          [kh[r4], identb], [pT])
                    P(lambda E: E.transpose(out=pR[0:96, rr, :], in_=krc[:, r, :], identity=identb[:, :]), [krc, identb], [pR])
                kt0, kt1, ktr = KT0.next(), KT1.next(), KTR.next()
                A(lambda E: E.copy(out=kt0[:], in_=pT[:, 0]), [pT], [kt0])
                V(lambda E: E.tensor_copy(out=kt1[:], in_=pT[:, 1]), [pT, kt0], [kt1])
                V(lambda E: E.tensor_copy(out=ktr[64:96], in_=pR[64:96]), [pR], [ktr])
                stt[it] = (kt0, kt1, ktr)

            def stage2(it):
                b = it // 32
                kt0, kt1, ktr = stt.pop(it)
                pS = psS.next()
                for rr in range(4):
                    P(lambda E: E.matmul(pS[:, rr * 128:(rr + 1) * 128], lhsT=kt0[:, rr, :],
                                         rhs=QLAT[:, 0, b].rearrange("p h s -> p (h s)"), start=True, stop=False), [kt0, QLAT], [pS])
                    P(lambda E: E.matmul(pS[:, rr * 128:(rr + 1) * 128], lhsT=kt1[:, rr, :],
                                         rhs=QLAT[:, 1, b].rearrange("p h s -> p (h s)"), start=False, stop=False), [kt1, QLAT], [pS])
                    P(lambda E: E.matmul(pS[:, rr * 128:(rr + 1) * 128], lhsT=ktr[64:96, rr, :],
                                         rhs=QR[64:96, b].rearrange("p h s -> p (h s)"), start=False, stop=True), [ktr, QR], [pS])
                pts = PTs.next()
                A(lambda E: E.activation(out=pts[:].rearrange("p a q -> p (a q)"), in_=pS[:, :], func=AF.Exp, scale=SCALE), [pS], [pts])
                stt[("p", it)] = pts

            def stage3(it):
                b, g, r4 = it // 32, (it % 32) // 2, it % 2
                pts = stt.pop(("p", it))
                kcb, krc, kh = stt[(b, g)]
                if it % 32 == 0:
                    pas[b] = psA.next()
                pa = pas[b]
                for rr in range(4):
                    r = r4 * 4 + rr
                    P(lambda E: E.matmul(pa[:, 0:257], lhsT=pts[:, rr, :], rhs=kcb[:, r, :], start=(it % 32 == 0 and rr == 0), stop=False),
                      [pts, kh[r4]], [pa])
                if r4 == 1:
                    del stt[(b, g)]
                if it % 32 == 31:
                    finalize(b)

            def finalize(b):
                pa = pas.pop(b)
                pS = psS.next()
                c0 = NPR + 8 * b
                for kc in range(2):
                    P(lambda E: E.matmul(pS[0:8, 0:128], lhsT=CKVb[:, kc, c0:c0 + 8], rhs=QLAT[:, kc, b].rearrange("p h s -> p (h s)"),
                                         start=(kc == 0), stop=False), [CKVb, QLAT], [pS])
                P(lambda E: E.matmul(pS[0:8, 0:128], lhsT=KRb[64:96, c0:c0 + 8], rhs=QR[64:96, b].rearrange("p h s -> p (h s)"),
                                     start=False, stop=True), [KRb, QR], [pS])
                pn = PN.next()
                A(lambda E: E.activation(out=pn[:], in_=pS[0:8, 0:128], func=AF.Exp, scale=SCALE), [pS], [pn])
                V(lambda E: E.tensor_tensor(out=pn[:], in0=pn[:], in1=newmask[:], op=ALU.mult), [pn, newmask], [pn])
                P(lambda E: E.matmul(pa[:, 0:257], lhsT=pn[:], rhs=KCN[:, b, :], start=False, stop=True), [pn, KCN], [pa])
                rl = rlp.next()
                V(lambda E: E.reciprocal(out=rl[:], in_=pa[:, 256:257]), [pa], [rl])
                ol = OL.next()
                V(lambda E: E.tensor_scalar(out=ol[:], in0=pa[:, 0:256], scalar1=rl[:, 0:1], scalar2=None, op0=ALU.mult), [pa, rl], [ol])
                pR = psR.next()
                for kc in range(2):
                    P(lambda E: E.transpose(out=pR[:, kc, :], in_=ol[:, kc * 128:(kc + 1) * 128], identity=identb[:, :]), [ol, identb], [pR])
                A(lambda E: E.copy(out=OLT[:, :, b].rearrange("p k h s -> p k (h s)"), in_=pR[:, 0:2, :]), [pR], [OLT])

            for step in range(NIT + 2):
                if step < NIT:
                    stage1(step)
                if 0 <= step - 1 < NIT:
                    stage2(step - 1)
                if 0 <= step - 2 < NIT:
                    stage3(step - 2)
            obs = RR([s3.sb([64, 128], BF16) for _ in range(2)])
            for h in range(16):
                pS = psS.next()
                for kc in range(2):
                    P(lambda E: E.matmul(pS[0:64, 0:128], lhsT=WUVb[:, kc, h * 64:(h + 1) * 64], rhs=OLT[:, kc, :, h, :],
                                         start=(kc == 0), stop=(kc == 1)), [WUVb, OLT], [pS])
                ob = obs.next()
                A(lambda E: E.copy(out=ob[:], in_=pS[0:64, 0:128]), [pS], [ob])
                DS(lambda E: E.dma_start(out=X.OBT[h // 2, (h % 2) * 64:(h % 2) * 64 + 64, NPR:NT], in_=ob[:]), [ob], [X.OBT])


EGROUPS = [(16, [1, 2, 3, 4]), (528, [5, 6, 7, 8]), (1040, [9, 10, 11, 12]), (1552, [13, 14, 15, 16]), (2064, [17])]


def x_rows(X, ti):
    return X.I["xp"][(ti - 1) * 128: ti * 128, :] if ti <= 16 else X.I["xs"]


def stage_e(X):
    I, O, V, A, G, P, DS, DG = X.I, X.O, X.V, X.A, X.G, X.P, X.DS, X.DG
    with Stage(X) as st:
        WUA = st.sb([128, 8, D], BF16)
        WUB = st.sb([128, 8, D], BF16)
        for kc in range(8):
            DG(lambda E: E.dma_start(out=WUA[:, kc, :], in_=I["w_up_a"][kc * 128:(kc + 1) * 128, :]), (), [WUA])
            DG(lambda E: E.dma_start(out=WUB[:, kc, :], in_=I["w_up_b"][kc * 128:(kc + 1) * 128, :]), (), [WUB])
        OAg = RR([st.sb([128, 8, 512], BF16) for _ in range(2)])
        OBg = RR([st.sb([128, 8, 512], BF16) for _ in range(2)])
        MTg = RR([st.sb([128, 16, 512], BF16) for _ in range(2)])
        gat = RR([st.sb([128, 2, 512]) for _ in range(3)])
        mtmp = RR([st.sb([128, 2, 512]) for _ in range(2)])
        psm = RR([st.ps([128, 512]) for _ in range(4)])
        for (t0, tiles) in EGROUPS:
            tn = 128 * len(tiles)
            oa, ob, mt = OAg.next(), OBg.next(), MTg.next()
            DS(lambda E: E.dma_start(out=oa[:, :, 0:tn], in_=X.OAT[:, :, t0:t0 + tn].rearrange("j p t -> p j t")), [X.OAT], [oa])
            DS(lambda E: E.dma_start(out=ob[:, :, 0:tn], in_=X.OBT[:, :, t0:t0 + tn].rearrange("j p t -> p j t")), [X.OBT], [ob])
            for dt in range(16):
                gt = gat.next()
                DS(lambda E: E.dma_start(out=gt[:, 0, 0:tn], in_=X.PFM[CTI[f"ga{dt}"], :, t0:t0 + tn]), [X.PFM], [gt])
                DS(lambda E: E.dma_start(out=gt[:, 1, 0:tn], in_=X.PFM[CTI[f"gb{dt}"], :, t0:t0 + tn]), [X.PFM], [gt])
                pa, pb = psm.next(), psm.next()
                for kc in range(8):
                    P(lambda E: E.matmul(pa[:, 0:tn], lhsT=WUA[:, kc, dt * 128:(dt + 1) * 128], rhs=oa[:, kc, 0:tn], start=(kc == 0), stop=(kc == 7)),
                      [WUA, oa], [pa])
                for kc in range(8):
                    P(lambda E: E.matmul(pb[:, 0:tn], lhsT=WUB[:, kc, dt * 128:(dt + 1) * 128], rhs=ob[:, kc, 0:tn], start=(kc == 0), stop=(kc == 7)),
                      [WUB, ob], [pb])
                m_ = mtmp.next()
                V(lambda E: E.scalar_tensor_tensor(out=m_[:, 0, 0:tn], in0=gt[:, 0, 0:tn], scalar=1.0, in1=pa[:, 0:tn], op0=ALU.add, op1=ALU.mult),
                  [gt, pa], [m_])
                V(lambda E: E.scalar_tensor_tensor(out=m_[:, 1, 0:tn], in0=gt[:, 1, 0:tn], scalar=1.0, in1=pb[:, 0:tn], op0=ALU.add, op1=ALU.mult),
                  [gt, pb], [m_])
                G(lambda E: E.tensor_tensor(out=mt[:, dt, 0:tn], in0=m_[:, 0, 0:tn], in1=m_[:, 1, 0:tn], op=ALU.add), [m_], [mt])
            DS(lambda E: E.dma_start(out=X.MTD[:, :, t0:t0 + tn], in_=mt[:, :, 0:tn]), [mt], [X.MTD])
    with Stage(X) as st:
        identf = st.load(I["ident_f"], [128, 128])
        WO = st.sb([128, 16, D], BF16)
        for kc in range(16):
            DG(lambda E: E.dma_start(out=WO[:, kc, :], in_=I["w_o"][kc * 128:(kc + 1) * 128, :]), (), [WO])
        RWt = st.load(I["router_w"].rearrange("(kc p) c -> p kc c", p=128), [128, 16, 36])
        RB = st.load(I["router_b"].to_broadcast([128, 36]), [128, 36])
        g2 = st.load(I["norm_ffn_g"].to_broadcast([128, D]), [128, D])
        MTt = RR([st.sb([128, 16, 128], BF16) for _ in range(2)])
        xin = RR([st.sb([128, D]) for _ in range(2)])
        x2p = RR([st.sb([128, D]) for _ in range(2)])
        xn = RR([st.sb([128, D]) for _ in range(2)])
        junk = st.sb([128, D])
        XT32 = RR([st.sb([128, 16, 128]) for _ in range(2)])
        XTb = RR([st.sb([128, 16, 128], BF16) for _ in range(2)])
        sm = RR([st.sb([128, 64]) for _ in range(24)])
        cmb = RR([st.sb([128, 32]) for _ in range(2)])
        psm = RR([st.ps([128, 512]) for _ in range(4)])
        pst = RR([st.ps([128, 512]) for _ in range(4)])
        for (t0, tiles) in EGROUPS:
            for li, ti in enumerate(tiles):
                g0, n = TT[ti]
                mt = MTt.next()
                DS(lambda E: E.dma_start(out=mt[:], in_=X.MTD[:, :, g0:g0 + n]), [X.MTD], [mt])
                li = 0
                x = xin.next()
                DS(lambda E: E.dma_start(out=x[:], in_=x_rows(X, ti)), (), [x])
                x2 = x2p.next()
                for cc in range(4):
                    ps = psm.next()
                    for kc in range(16):
                        P(lambda E: E.matmul(ps[:, :], lhsT=mt[:, kc, li * 128:(li + 1) * 128], rhs=WO[:, kc, cc * 512:(cc + 1) * 512],
                                             start=(kc == 0), stop=(kc == 15)), [mt, WO], [ps])
                    V(lambda E: E.scalar_tensor_tensor(out=x2[:, cc * 512:(cc + 1) * 512], in0=ps[:, :], scalar=0.5,
                                                       in1=x[:, cc * 512:(cc + 1) * 512], op0=ALU.mult, op1=ALU.add), [ps, x], [x2])
                DS(lambda E: E.dma_start(out=X.X2[g0:g0 + n, :], in_=x2[:]), [x2], [X.X2])
                sq = sm.next()
                G(lambda E: E.tensor_tensor(out=junk[:], in0=x2[:], in1=x2[:], op=ALU.mult), [x2], [junk])
                V(lambda E: E.tensor_reduce(out=sq[:, 0:1], in_=junk[:], axis=AX.X, op=ALU.add), [junk], [sq])
                A(lambda E: E.activation(out=sq[:, 0:1], in_=sq[:, 0:1], func=AF.Sqrt, bias=1e-6, scale=1.0 / D), [sq], [sq])
                V(lambda E: E.reciprocal(out=sq[:, 0:1], in_=sq[:, 0:1]), [sq], [sq])
                xn2 = xn.next()
                V(lambda E: E.scalar_tensor_tensor(out=xn2[:], in0=x2[:], scalar=sq[:, 0:1], in1=g2[:], op0=ALU.mult, op1=ALU.mult),
                  [x2, sq, g2], [xn2])
                xt32, xtb = XT32.next(), XTb.next()
                for q4 in range(4):
                    ps = pst.next()
                    for a in range(4):
                        kc = q4 * 4 + a
                        P(lambda E: E.transpose(out=ps[:, a * 128:(a + 1) * 128], in_=xn2[:, kc * 128:(kc + 1) * 128], identity=identf[:, :]),
                          [xn2, identf], [ps])
                    A(lambda E: E.copy(out=xt32[:, q4 * 4:q4 * 4 + 4, :], in_=ps[:, :].rearrange("p (a t) -> p a t", a=4)), [ps], [xt32])
                G(lambda E: E.tensor_copy(out=xtb[:], in_=xt32[:]), [xt32], [xtb])
                DS(lambda E: E.dma_start(out=X.XN2T.t.rearrange("(kc p) t -> p kc t", p=128)[:, :, g0:g0 + n], in_=xtb[:]), [xtb], [X.XN2T])
                ps = psm.next()
                for kc in range(16):
                    P(lambda E: E.matmul(ps[:, 0:36], lhsT=xt32[:, kc, :], rhs=RWt[:, kc, :], start=(kc == 0), stop=(kc == 15)), [xt32, RWt], [ps])
                lg = sm.next()
                A(lambda E: E.copy(out=lg[:, 0:36], in_=ps[:, 0:36]), [ps], [lg])
                gl = lg[:, 0:4]
                el = lg[:, 4:36].rearrange("p (g e) -> p g e", g=4)
                mx, e4, s4, pr = sm.next(), sm.next(), sm.next(), sm.next()
                V(lambda E: E.tensor_reduce(out=mx[:, 0:1], in_=gl, axis=AX.X, op=ALU.max), [lg], [mx])
                V(lambda E: E.tensor_scalar(out=e4[:, 0:4], in0=gl, scalar1=mx[:, 0:1], scalar2=None, op0=ALU.subtract), [lg, mx], [e4])
                A(lambda E: E.activation(out=e4[:, 0:4], in_=e4[:, 0:4], func=AF.Exp), [e4], [e4])
                V(lambda E: E.tensor_reduce(out=s4[:, 0:1], in_=e4[:, 0:4], axis=AX.X, op=ALU.add), [e4], [s4])
                V(lambda E: E.reciprocal(out=s4[:, 0:1], in_=s4[:, 0:1]), [s4], [s4])
                V(lambda E: E.tensor_scalar(out=pr[:, 0:4], in0=e4[:, 0:4], scalar1=s4[:, 0:1], scalar2=None, op0=ALU.mult), [e4, s4], [pr])
                glb, mxb, oh = sm.next(), sm.next(), sm.next()
                V(lambda E: E.tensor_tensor(out=glb[:, 0:4], in0=gl, in1=RB[:, 0:4], op=ALU.add), [lg, RB], [glb])
                V(lambda E: E.tensor_reduce(out=mxb[:, 0:1], in_=glb[:, 0:4], axis=AX.X, op=ALU.max), [glb], [mxb])
                V(lambda E: E.tensor_scalar(out=oh[:, 0:4], in0=glb[:, 0:4], scalar1=mxb[:, 0:1], scalar2=None, op0=ALU.is_ge), [glb, mxb], [oh])
                pg, t4 = sm.next(), sm.next()
                V(lambda E: E.tensor_tensor(out=t4[:, 0:4], in0=pr[:, 0:4], in1=oh[:, 0:4], op=ALU.mult), [pr, oh], [t4])
                V(lambda E: E.tensor_reduce(out=pg[:, 0:1], in_=t4[:, 0:4], axis=AX.X, op=ALU.add), [t4], [pg])
                ohb = oh[:, 0:4].unsqueeze(2).to_broadcast([128, 4, 8])
                t32, ein, eb = sm.next(), sm.next(), sm.next()
                V(lambda E: E.tensor_tensor(out=t32[:, 0:32].rearrange("p (g e) -> p g e", g=4), in0=el, in1=ohb, op=ALU.mult), [lg, oh], [t32])
                V(lambda E: E.tensor_reduce(out=ein[:, 0:8], in_=t32[:, 0:32].rearrange("p (g e) -> p e g", g=4), axis=AX.X, op=ALU.add), [t32], [ein])
                t32b = sm.next()
                V(lambda E: E.tensor_tensor(out=t32b[:, 0:32].rearrange("p (g e) -> p g e", g=4),
                                            in0=RB[:, 4:36].rearrange("p (g e) -> p g e", g=4), in1=ohb, op=ALU.mult), [RB, oh], [t32b])
                V(lambda E: E.tensor_reduce(out=eb[:, 0:8], in_=t32b[:, 0:32].rearrange("p (g e) -> p e g", g=4), axis=AX.X, op=ALU.add), [t32b], [eb])
                V(lambda E: E.tensor_tensor(out=eb[:, 0:8], in0=eb[:, 0:8], in1=ein[:, 0:8], op=ALU.add), [eb, ein], [eb])
                m1, oh1, eb2, m2, oh2 = sm.next(), sm.next(), sm.next(), sm.next(), sm.next()
                V(lambda E: E.tensor_reduce(out=m1[:, 0:1], in_=eb[:, 0:8], axis=AX.X, op=ALU.max), [eb], [m1])
                V(lambda E: E.tensor_scalar(out=oh1[:, 0:8], in0=eb[:, 0:8], scalar1=m1[:, 0:1], scalar2=None, op0=ALU.is_ge), [eb, m1], [oh1])
                V(lambda E: E.scalar_tensor_tensor(out=eb2[:, 0:8], in0=oh1[:, 0:8], scalar=-1e30, in1=eb[:, 0:8], op0=ALU.mult, op1=ALU.add),
                  [oh1, eb], [eb2])
                V(lambda E: E.tensor_reduce(out=m2[:, 0:1], in_=eb2[:, 0:8], axis=AX.X, op=ALU.max), [eb2], [m2])
                V(lambda E: E.tensor_scalar(out=oh2[:, 0:8], in0=eb2[:, 0:8], scalar1=m2[:, 0:1], scalar2=None, op0=ALU.is_ge), [eb2, m2], [oh2])
                v1, v2, tt8 = sm.next(), sm.next(), sm.next()
                V(lambda E: E.tensor_tensor(out=tt8[:, 0:8], in0=ein[:, 0:8], in1=oh1[:, 0:8], op=ALU.mult), [ein, oh1], [tt8])
                V(lambda E: E.tensor_reduce(out=v1[:, 0:1], in_=tt8[:, 0:8], axis=AX.X, op=ALU.add), [tt8], [v1])
                V(lambda E: E.tensor_tensor(out=tt8[:, 8:16], in0=ein[:, 0:8], in1=oh2[:, 0:8], op=ALU.mult), [ein, oh2], [tt8])
                V(lambda E: E.tensor_reduce(out=v2[:, 0:1], in_=tt8[:, 8:16], axis=AX.X, op=ALU.add), [tt8], [v2])
                V(lambda E: E.tensor_tensor(out=v2[:, 0:1], in0=v2[:, 0:1], in1=v1[:, 0:1], op=ALU.subtract), [v2, v1], [v2])
                A(lambda E: E.activation(out=v2[:, 0:1], in_=v2[:, 0:1], func=AF.Exp), [v2], [v2])
                V(lambda E: E.tensor_scalar(out=v1[:, 0:1], in0=v2[:, 0:1], scalar1=1.0, scalar2=None, op0=ALU.add), [v2], [v1])
                V(lambda E: E.reciprocal(out=v1[:, 0:1], in_=v1[:, 0:1]), [v1], [v1])
                V(lambda E: E.tensor_tensor(out=v1[:, 0:1], in0=v1[:, 0:1], in1=pg[:, 0:1], op=ALU.mult), [v1, pg], [v1])
                V(lambda E: E.tensor_tensor(out=v2[:, 0:1], in0=v2[:, 0:1], in1=v1[:, 0:1], op=ALU.mult), [v2, v1], [v2])
                c8 = sm.next()
                V(lambda E: E.tensor_scalar(out=c8[:, 0:8], in0=oh1[:, 0:8], scalar1=v1[:, 0:1], scalar2=None, op0=ALU.mult), [oh1, v1], [c8])
                V(lambda E: E.scalar_tensor_tensor(out=c8[:, 0:8], in0=oh2[:, 0:8], scalar=v2[:, 0:1], in1=c8[:, 0:8], op0=ALU.mult, op1=ALU.add),
                  [oh2, v2, c8], [c8])
                cb = cmb.next()
                V(lambda E: E.tensor_tensor(out=cb[:, :].rearrange("p (g e) -> p g e", g=4), in0=c8[:, 0:8].unsqueeze(1).to_broadcast([128, 4, 8]),
                                            in1=ohb, op=ALU.mult), [c8, oh], [cb])
                DS(lambda E: E.dma_start(out=X.COMB[g0:g0 + n, :], in_=cb[:]), [cb], [X.COMB])


FGROUPS = [[1, 2, 3, 4, 5, 6], [7, 8, 9, 10, 11, 12], [13, 14, 15, 16, 17]]


def stage_f(X):
    I, O, V, A, G, P, DS, DG = X.I, X.O, X.V, X.A, X.G, X.P, X.DS, X.DG
    with Stage(X) as st:
        gf = st.load(I["norm_final_g"].to_broadcast([128, D]), [128, D])
        XTg = st.sb([128, 16, 768], BF16)
        YACC = [st.sb([128, D]) for _ in range(6)]
        CMB = st.sb([128, 6, 32])
        Wg = RR([st.sb([128, 16, 512], BF16) for _ in range(2)])
        Wu = RR([st.sb([128, 16, 512], BF16) for _ in range(2)])
        Wd = RR([st.sb([128, 4, D], BF16) for _ in range(2)])
        act = RR([st.sb([128, 4, 768], BF16) for _ in range(2)])
        sgp = RR([st.sb([128, 384]) for _ in range(3)])
        small = RR([st.sb([128, 1]) for _ in range(2)])
        x2f = RR([st.sb([128, D]) for _ in range(1)])
        psg = RR([st.ps([128, 512]) for _ in range(4)])
        psd = RR([st.ps([128, 512]) for _ in range(4)])
        for tiles in FGROUPS:
            t0 = TT[tiles[0]][0]
            nt_ = len(tiles)
            tn = 128 * nt_
            hn = tn // 2
            DS(lambda E: E.dma_start(out=XTg[:, :, 0:tn], in_=X.XN2T.t.rearrange("(p k) t -> p k t", k=16)[:, :, t0:t0 + tn]), [X.XN2T], [XTg])
            for li in range(nt_):
                DS(lambda E: E.dma_start(out=CMB[:, li, :], in_=X.COMB[t0 + li * 128:t0 + (li + 1) * 128, :]), [X.COMB], [CMB])
            for e in range(32):
                wg, wu, wd = Wg.next(), Wu.next(), Wd.next()
                DG(lambda E: E.dma_start(out=wg[:], in_=I["expert_w_gate"][e].rearrange("(p k) f -> p k f", k=16)), (), [wg])
                DG(lambda E: E.dma_start(out=wu[:], in_=I["expert_w_up"][e].rearrange("(p k) f -> p k f", k=16)), (), [wu])
                DG(lambda E: E.dma_start(out=wd[:], in_=I["expert_w_down"][e].rearrange("(p k) d -> p k d", k=4)), (), [wd])
                ac = act.next()
                for half in range(2):
                    hs = slice(half * hn, (half + 1) * hn)
                    for ft in range(4):
                        pg_, pu_ = psg.next(), psg.next()
                        for kc in range(16):
                            P(lambda E: E.matmul(pg_[:, 0:hn], lhsT=wg[:, kc, ft:512:4], rhs=XTg[:, kc, hs], start=(kc == 0), stop=(kc == 15)),
                              [wg, XTg], [pg_])
                        for kc in range(16):
                            P(lambda E: E.matmul(pu_[:, 0:hn], lhsT=wu[:, kc, ft:512:4], rhs=XTg[:, kc, hs], start=(kc == 0), stop=(kc == 15)),
                              [wu, XTg], [pu_])
                        sg = sgp.next()
                        A(lambda E: E.activation(out=sg[:, 0:hn], in_=pg_[:, 0:hn], func=AF.Silu), [pg_], [sg])
                        V(lambda E: E.tensor_tensor(out=ac[:, ft, hs], in0=sg[:, 0:hn], in1=pu_[:, 0:hn], op=ALU.mult), [sg, pu_], [ac])
                for li in range(nt_):
                    for cc in range(4):
                        pd = psd.next()
                        for ft in range(4):
                            P(lambda E: E.matmul(pd[:, :], lhsT=ac[:, ft, li * 128:(li + 1) * 128], rhs=wd[:, ft, cc * 512:(cc + 1) * 512],
                                                 start=(ft == 0), stop=(ft == 3)), [ac, wd], [pd])
                        ya = YACC[li]
                        if e == 0:
                            V(lambda E: E.tensor_scalar(out=ya[:, cc * 512:(cc + 1) * 512], in0=pd[:, :], scalar1=CMB[:, li, e:e + 1], scalar2=None,
                                                        op0=ALU.mult), [pd, CMB], [ya])
                        else:
                            V(lambda E: E.scalar_tensor_tensor(out=ya[:, cc * 512:(cc + 1) * 512], in0=pd[:, :], scalar=CMB[:, li, e:e + 1],
                                                               in1=ya[:, cc * 512:(cc + 1) * 512], op0=ALU.mult, op1=ALU.add), [pd, CMB, ya], [ya])
            for li, ti in enumerate(tiles):
                g0, n = TT[ti]
                ya = YACC[li]
                x2t = x2f.next()
                DS(lambda E: E.dma_start(out=x2t[:], in_=X.X2[g0:g0 + n, :]), [X.X2], [x2t])
                G(lambda E: E.tensor_tensor(out=ya[:], in0=ya[:], in1=x2t[:], op=ALU.add), [ya, x2t], [ya])
                G(lambda E: E.tensor_tensor(out=x2t[:], in0=ya[:], in1=ya[:], op=ALU.mult), [ya], [x2t])
                sq = small.next()
                V(lambda E: E.tensor_reduce(out=sq[:, 0:1], in_=x2t[:], axis=AX.X, op=ALU.add), [x2t], [sq])
                A(lambda E: E.activation(out=sq[:, 0:1], in_=sq[:, 0:1], func=AF.Sqrt, bias=1e-6, scale=1.0 / D), [sq], [sq])
                V(lambda E: E.reciprocal(out=sq[:, 0:1], in_=sq[:, 0:1]), [sq], [sq])
                V(lambda E: E.scalar_tensor_tensor(out=x2t[:], in0=ya[:], scalar=sq[:, 0:1], in1=gf[:], op0=ALU.mult, op1=ALU.mult),
                  [ya, sq, gf], [x2t])
                dst = O["y_prompt"][(ti - 1) * 128: ti * 128, :] if ti <= 16 else O["y_sample"]
                DS(lambda E: E.dma_start(out=dst, in_=x2t[:]), [x2t], [])


def shard_inputs(inputs, n_cores=8, n_pool=None):
    f = lambda a: np.ascontiguousarray(np.asarray(a))
    g = {k: np.asarray(v) for k, v in inputs.items()}
    col = lambda a: f(a.reshape(-1, 1))
    shared = {
        "meta": f(g["meta_tokens"]),
        "norm_mix_g": f(g["norm_mix_g"].reshape(1, D)), "w_in": f(g["w_in"][0]),
        "rwkv_mu": col(g["rwkv_mu"][0]), "rwkv_w0": col(g["rwkv_w0"][0]), "rwkv_w2": f(g["rwkv_w2"][0]),
        "rwkv_a0": col(g["rwkv_a0"][0]), "rwkv_a2": f(g["rwkv_a2"][0]), "rwkv_g2": f(g["rwkv_g2"][0]),
        "rwkv_k_k": col(g["rwkv_k_k"][0]), "rwkv_k_a": col(g["rwkv_k_a"][0]), "rwkv_r_k": col(g["rwkv_r_k"][0]),
        "rwkv_ln_g": col(g["rwkv_ln_g"][0]), "rwkv_ln_b": col(g["rwkv_ln_b"][0]),
        "mla_q_norm_g": col(g["mla_q_norm_g"][0]), "mla_w_uq": f(g["mla_w_uq"][0]),
        "mla_kv_norm_g": col(g["mla_kv_norm_g"][0]), "mla_w_uk": f(g["mla_w_uk"][0].reshape(256, 1024)),
        "mla_w_uv": f(g["mla_w_uv"][0].reshape(256, 1024)),
        "w_up_a": f(g["w_up_a"][0]), "w_up_b": f(g["w_up_b"][0]), "w_o": f(g["w_o"][0]),
        "norm_ffn_g": f(g["norm_ffn_g"].reshape(1, D)),
        "router_w": f(np.concatenate([g["router_group_w"][0], g["router_expert_w"][0]], axis=1)),
        "router_b": f(np.concatenate([g["router_group_b"][0], g["router_expert_b"][0]]).reshape(1, 36)),
        "expert_w_gate": f(g["expert_w_gate"][0]), "expert_w_up": f(g["expert_w_up"][0]),
        "expert_w_down": f(g["expert_w_down"][0]), "norm_final_g": f(g["norm_final_g"].reshape(1, D)),
        "cache_ckv": f(g["cache_ckv"][0]), "cache_krope": f(g["cache_krope"][0]),
    }
    shared.update(make_consts())
    maps = []
    for c in range(n_cores):
        m = dict(shared)
        m["xp"] = f(g["x_prompt"][c])
        m["xs"] = f(g["x_sample"][16 * c:16 * c + 16].reshape(NS, D))
        m["state_wkv"] = f(g["state_wkv"][0, 16 * c:16 * c + 16])
        m["state_shift"] = f(g["state_shift"][0, 16 * c:16 * c + 16])
        m["page_table"] = f(g["page_table"][16 * c:16 * c + 16]).astype(np.int32)
        maps.append(m)
    return maps


def kernel(**inputs):
    n_pool = int(np.asarray(inputs["cache_ckv"]).shape[1])
    nc, _ = build(n_pool=n_pool)
    maps = shard_inputs(inputs)
    res = run_bass_kernel_spmd(nc, maps, core_ids=list(range(8)))
    R = res.results
    cat = lambda k: np.stack([r[k] for r in R], axis=0)
    y_prompt = cat("y_prompt")
    y_sample = np.concatenate([r["y_sample"].reshape(16, 8, D) for r in R], axis=0)
    ckv_p = cat("ckv_p")[None]
    kr_p = cat("kr_p")[None]
    wkv_p = cat("wkv_p")[None]
    sh_p = np.concatenate([r["sh_p"] for r in R], axis=0)[None]
    ckv_s = np.concatenate([r["ckv_s"].reshape(16, 8, 256) for r in R], axis=0)[None]
    kr_s = np.concatenate([r["kr_s"].reshape(16, 8, 32) for r in R], axis=0)[None]
    wkv_s = np.concatenate([r["wkv_s"] for r in R], axis=0)[None]
    sh_s = np.concatenate([r["sh_s"] for r in R], axis=0)[None]
    return (y_prompt, y_sample, ckv_p, kr_p, wkv_p, sh_p, ckv_s, kr_s, wkv_s, sh_s)
```
